# Optimizing a Trainium2 kernel written in Bass

```python
import math
import jax, jax.numpy as jnp
from jax import lax
import numpy as np

D_MODEL = 1024
BATCH = 8
SEQ = 4096
DEPTH = 4

RET_HEADS = 4
RET_DK = 128
RET_DV = 128
RET_CHUNK = 128
RET_THETA = 10000.0
DIL_HEADS = 8
DIL_DH = 64
DIL_PATTERNS = ((128, 1), (512, 4), (2048, 16))
DIL_BLOCK = 128
ROPE_THETA = 500000.0
ROT_DIM = DIL_DH // 4
GLA_HEADS = 4
GLA_DK = (D_MODEL // 2) // GLA_HEADS
GLA_DV = D_MODEL // GLA_HEADS
GLA_LOWRANK = 16
GLA_TAU = 16.0
GLA_CHUNK = 64
FFN_HIDDEN = 2816
EPS = 1e-6

EVEN_SIZES = (RET_HEADS * RET_DK, RET_HEADS * RET_DK, RET_HEADS * RET_DV, RET_HEADS * RET_DV,
              DIL_HEADS * DIL_DH, DIL_HEADS * DIL_DH, DIL_HEADS * DIL_DH)
EVEN_IN = sum(EVEN_SIZES)
EVEN_MIX = RET_HEADS * RET_DV + DIL_HEADS * DIL_DH
GLA_SIZES = (GLA_HEADS * GLA_DK, GLA_HEADS * GLA_DK, GLA_HEADS * GLA_DV, GLA_HEADS * GLA_DV, GLA_LOWRANK)
GLA_IN = sum(GLA_SIZES)
GLA_MIX = GLA_HEADS * GLA_DV

kernel_name = "hybrid_retention_dilated_gla_macaron"


def _split(p, sizes):
    out, start = [], 0
    for s in sizes:
        out.append(p[..., start:start + s])
        start += s
    return out


def _rmsnorm(x, g):
    xf = x.astype(jnp.float32)
    y = xf * lax.rsqrt(jnp.mean(xf * xf, axis=-1, keepdims=True) + EPS)
    return (y * g.astype(jnp.float32)).astype(x.dtype)


def _head_norm(o):
    mu = jnp.mean(o, axis=-1, keepdims=True)
    var = jnp.mean(jnp.square(o - mu), axis=-1, keepdims=True)
    return (o - mu) * lax.rsqrt(var + EPS)


def _swiglu(h, wg, wu, wd):
    return (jax.nn.silu(h @ wg) * (h @ wu)) @ wd


def _rotary(x, inv_freq):
    rot = 2 * inv_freq.shape[0]
    pos = jnp.arange(x.shape[1], dtype=jnp.float32)
    ang = pos[:, None] * inv_freq[None, :]
    cos = jnp.cos(ang)[None, :, None, :].astype(x.dtype)
    sin = jnp.sin(ang)[None, :, None, :].astype(x.dtype)
    x1 = x[..., :rot // 2]
    x2 = x[..., rot // 2:rot]
    return jnp.concatenate([x1 * cos - x2 * sin, x2 * cos + x1 * sin, x[..., rot:]], axis=-1)


def _retention(q, k, v):
    B, S, H, Dk = q.shape
    Dv = v.shape[-1]
    C = RET_CHUNK
    N = S // C
    log_g = jnp.log(1.0 - 2.0 ** (-5.0 - jnp.arange(H, dtype=jnp.float32)))
    to_chunks = lambda t: t.reshape(B, N, C, H, t.shape[-1]).transpose(0, 3, 1, 2, 4)
    qc, kc, vc = to_chunks(q), to_chunks(k), to_chunks(v)
    i = jnp.arange(C, dtype=jnp.float32)
    rel = i[:, None] - i[None, :]
    dmask = jnp.where(rel >= 0, jnp.exp(log_g[:, None, None] * jnp.maximum(rel, 0.0)), 0.0)
    scores = jnp.einsum('bhnid,bhnjd->bhnij', qc, kc) * dmask[None, :, None]
    inner = jnp.einsum('bhnij,bhnje->bhnie', scores, vc)
    k_dec = kc * jnp.exp(log_g[:, None] * (C - 1 - i))[None, :, None, :, None]
    kv = jnp.einsum('bhnjd,bhnje->nbhde', k_dec, vc)
    chunk_decay = jnp.exp(log_g * C)[None, :, None, None]

    def step(s, kv_n):
        return s * chunk_decay + kv_n, s

    _, s_prev = lax.scan(step, jnp.zeros((B, H, Dk, Dv), jnp.float32), kv)
    q_dec = qc * jnp.exp(log_g[:, None] * (i + 1.0))[None, :, None, :, None]
    cross = jnp.einsum('bhnid,nbhde->bhnie', q_dec, s_prev)
    o = inner + cross
    return o.transpose(0, 2, 3, 1, 4).reshape(B, S, H, Dv)


def _dilated_branch(q, k, v, window, dil):
    B, S, H, Dh = q.shape
    L = S // dil
    W = window // dil
    c = DIL_BLOCK
    nb = -(-L // c)
    Lp = nb * c

    def split(t):
        return t.reshape(B, L, dil, H, Dh).transpose(0, 2, 3, 1, 4).reshape(B * dil, H, L, Dh)

    qs = jnp.pad(split(q), ((0, 0), (0, 0), (0, Lp - L), (0, 0))).reshape(B * dil, H, nb, c, Dh)

    def kblocks(t):
        t = jnp.pad(split(t), ((0, 0), (0, 0), (c, Lp - L), (0, 0))).reshape(B * dil, H, nb + 1, c, Dh)
        return jnp.concatenate([t[:, :, :-1], t[:, :, 1:]], axis=3)

    kb, vb = kblocks(k), kblocks(v)
    s = jnp.einsum('zhnqd,zhnkd->zhnqk', qs, kb).astype(jnp.float32) * (Dh ** -0.5)
    qi = jnp.arange(c)[:, None]
    kj = jnp.arange(2 * c)[None, :]
    dist = qi + c - kj
    blk = jnp.arange(nb)[:, None, None]
    valid = (dist >= 0) & (dist <= W) & (blk * c + kj - c >= 0)
    s = jnp.where(valid, s, -jnp.inf)
    m = jnp.max(s, axis=-1, keepdims=True)
    p = jnp.exp(s - m)
    l = jnp.sum(p, axis=-1, keepdims=True)
    o = jnp.einsum('zhnqk,zhnkd->zhnqd', p, vb.astype(jnp.float32)) / l
    lse = (m + jnp.log(l))[..., 0]
    o = o.reshape(B, dil, H, Lp, Dh)[:, :, :, :L].transpose(0, 3, 1, 2, 4).reshape(B, S, H, Dh)
    lse = lse.reshape(B, dil, H, Lp)[..., :L].transpose(0, 3, 1, 2).reshape(B, S, H)
    return o, lse


def _dilated_attention(q, k, v):
    outs, lses = [], []
    for window, dil in DIL_PATTERNS:
        o, lse = _dilated_branch(q, k, v, window, dil)
        outs.append(o)
        lses.append(lse)
    w = jax.nn.softmax(jnp.stack(lses, axis=0), axis=0)
    return jnp.sum(w[..., None] * jnp.stack(outs, axis=0), axis=0)


def _retention_dilated_mixer(h, w_in, w_out):
    B, S, _ = h.shape
    rq, rk, rv, rg, dq, dk, dv = _split(h @ w_in, EVEN_SIZES)
    ret_freq = RET_THETA ** (-jnp.linspace(0.0, 1.0, RET_DK // 2, dtype=jnp.float32))
    rq = _rotary(rq.reshape(B, S, RET_HEADS, RET_DK), ret_freq).astype(jnp.float32)
    rk = _rotary(rk.reshape(B, S, RET_HEADS, RET_DK), ret_freq).astype(jnp.float32) * (RET_DK ** -0.5)
    rv = rv.reshape(B, S, RET_HEADS, RET_DV).astype(jnp.float32)
    o_r = _head_norm(_retention(rq, rk, rv)).reshape(B, S, RET_HEADS * RET_DV)
    o_r = o_r * jax.nn.silu(rg.astype(jnp.float32))
    rope_freq = ROPE_THETA ** (-jnp.arange(0, ROT_DIM, 2, dtype=jnp.float32) / ROT_DIM)
    dq = _rotary(dq.reshape(B, S, DIL_HEADS, DIL_DH), rope_freq)
    dk = _rotary(dk.reshape(B, S, DIL_HEADS, DIL_DH), rope_freq)
    dv = dv.reshape(B, S, DIL_HEADS, DIL_DH)
    o_d = _dilated_attention(dq, dk, dv).reshape(B, S, DIL_HEADS * DIL_DH)
    cat = jnp.concatenate([o_r.astype(h.dtype), o_d.astype(h.dtype)], axis=-1)
    return cat @ w_out


def _gla(q, k, v, log_a):
    B, S, H, Dk = q.shape
    Dv = v.shape[-1]
    C = GLA_CHUNK
    N = S // C
    to_chunks = lambda t: t.reshape(B, N, C, H, t.shape[-1]).transpose(0, 3, 1, 2, 4)
    qc, kc, vc, lac = to_chunks(q), to_chunks(k), to_chunks(v), to_chunks(log_a)
    b = jnp.cumsum(lac, axis=3)
    b_last = b[:, :, :, -1:]
    q_t = qc * jnp.exp(b)
    k_t = kc * jnp.exp(-b)
    causal = jnp.tril(jnp.ones((C, C), dtype=bool))
    scores = jnp.where(causal, jnp.einsum('bhnid,bhnjd->bhnij', q_t, k_t), 0.0)
    inner = jnp.einsum('bhnij,bhnje->bhnie', scores, vc)
    kv = jnp.einsum('bhnjd,bhnje->nbhde', kc * jnp.exp(b_last - b), vc)
    decay = jnp.exp(b_last[:, :, :, 0]).transpose(2, 0, 1, 3)

    def step(s, inp):
        kv_n, d_n = inp
        return s * d_n[..., None] + kv_n, s

    _, s_prev = lax.scan(step, jnp.zeros((B, H, Dk, Dv), jnp.float32), (kv, decay))
    cross = jnp.einsum('bhnid,nbhde->bhnie', q_t, s_prev)
    o = inner + cross
    return o.transpose(0, 2, 3, 1, 4).reshape(B, S, H, Dv)


def _gla_mixer(h, w_in, w_a2, b_a, w_out):
    B, S, _ = h.shape
    q, k, v, r, a_low = _split(h @ w_in, GLA_SIZES)
    log_a = jax.nn.log_sigmoid((a_low @ w_a2 + b_a).astype(jnp.float32)) / GLA_TAU
    q = q.reshape(B, S, GLA_HEADS, GLA_DK).astype(jnp.float32) * (GLA_DK ** -0.5)
    k = k.reshape(B, S, GLA_HEADS, GLA_DK).astype(jnp.float32)
    v = v.reshape(B, S, GLA_HEADS, GLA_DV).astype(jnp.float32)
    log_a = log_a.reshape(B, S, GLA_HEADS, GLA_DK)
    o = _head_norm(_gla(q, k, v, log_a)).reshape(B, S, GLA_MIX)
    o = o * jax.nn.silu(r.astype(jnp.float32))
    return o.astype(h.dtype) @ w_out


def setup_inputs(seed: int = 0) -> dict:
    key = jax.random.key(seed)
    ks = jax.random.split(key, 24)
    n_even = (DEPTH + 1) // 2
    n_odd = DEPTH // 2
    f32 = jnp.float32

    def w(k, shape, fan_in):
        return jax.random.normal(k, shape, f32) * (fan_in ** -0.5)

    def gain(k, shape):
        return 1.0 + 0.05 * jax.random.normal(k, shape, f32)

    return {
        "x": jax.random.normal(ks[0], (BATCH, SEQ, D_MODEL), f32),
        "ffn_pre_norm": gain(ks[1], (DEPTH, D_MODEL)),
        "ffn_pre_w_gate": w(ks[2], (DEPTH, D_MODEL, FFN_HIDDEN), D_MODEL),
        "ffn_pre_w_up": w(ks[3], (DEPTH, D_MODEL, FFN_HIDDEN), D_MODEL),
        "ffn_pre_w_down": w(ks[4], (DEPTH, FFN_HIDDEN, D_MODEL), FFN_HIDDEN),
        "mix_norm": gain(ks[5], (DEPTH, D_MODEL)),
        "ab_w_in": w(ks[6], (n_even, D_MODEL, EVEN_IN), D_MODEL),
        "ab_w_out": w(ks[7], (n_even, EVEN_MIX, D_MODEL), EVEN_MIX),
        "gla_w_in": w(ks[8], (n_odd, D_MODEL, GLA_IN), D_MODEL),
        "gla_w_a2": w(ks[9], (n_odd, GLA_LOWRANK, GLA_HEADS * GLA_DK), GLA_LOWRANK),
        "gla_b_a": 0.1 * jax.random.normal(ks[10], (n_odd, GLA_HEADS * GLA_DK), f32),
        "gla_w_out": w(ks[11], (n_odd, GLA_MIX, D_MODEL), GLA_MIX),
        "ffn_post_norm": gain(ks[12], (DEPTH, D_MODEL)),
        "ffn_post_w_gate": w(ks[13], (DEPTH, D_MODEL, FFN_HIDDEN), D_MODEL),
        "ffn_post_w_up": w(ks[14], (DEPTH, D_MODEL, FFN_HIDDEN), D_MODEL),
        "ffn_post_w_down": w(ks[15], (DEPTH, FFN_HIDDEN, D_MODEL), FFN_HIDDEN),
        "final_norm": gain(ks[16], (D_MODEL,)),
    }


def reference(x, ffn_pre_norm, ffn_pre_w_gate, ffn_pre_w_up, ffn_pre_w_down, mix_norm,
              ab_w_in, ab_w_out, gla_w_in, gla_w_a2, gla_b_a, gla_w_out,
              ffn_post_norm, ffn_post_w_gate, ffn_post_w_up, ffn_post_w_down, final_norm):
    for l in range(DEPTH):
        h = _rmsnorm(x, ffn_pre_norm[l])
        x = x + 0.5 * _swiglu(h, ffn_pre_w_gate[l], ffn_pre_w_up[l], ffn_pre_w_down[l])
        h = _rmsnorm(x, mix_norm[l])
        if l % 2 == 0:
            x = x + _retention_dilated_mixer(h, ab_w_in[l // 2], ab_w_out[l // 2])
        else:
            x = x + _gla_mixer(h, gla_w_in[l // 2], gla_w_a2[l // 2], gla_b_a[l // 2], gla_w_out[l // 2])
        h = _rmsnorm(x, ffn_post_norm[l])
        x = x + 0.5 * _swiglu(h, ffn_post_w_gate[l], ffn_post_w_up[l], ffn_post_w_down[l])
    return _rmsnorm(x, final_norm)
```

```python
import math
from contextlib import ExitStack

import numpy as np
import ml_dtypes
import concourse.bass as bass
import concourse.mybir as mybir
from concourse.bass_utils import run_bass_kernel_spmd

F32 = mybir.dt.float32
BF16 = mybir.dt.bfloat16
AF = mybir.ActivationFunctionType
ALU = mybir.AluOpType
AX = mybir.AxisListType

D_MODEL = 1024
FFN_HIDDEN = 2816
EPS = 1e-6
DEPTH = 4


class Res:
    __slots__ = ("name", "last_w", "readers", "dma_readers", "excl")

    def __init__(self, name, excl=False):
        self.name = name
        self.excl = excl
        self.last_w = None
        self.readers = {}
        self.dma_readers = []


class Op:
    __slots__ = ("eng", "fn", "deps", "sig", "val", "is_dma", "chan", "sem")

    def __init__(self, eng, fn, is_dma=False, chan=None):
        self.eng = eng
        self.fn = fn
        self.deps = []
        self.sig = False
        self.val = None
        self.is_dma = is_dma
        self.chan = chan
        self.sem = None


class Prog:
    ENGS = ("pe", "act", "dve", "pool", "sp")

    def __init__(self, nc):
        self.nc = nc
        self.ops = {e: [] for e in self.ENGS}
        self.chan_count = {}
        self.n_ops = 0

    def _add_dep(self, op, d, kind):
        if d is None or d is op:
            return
        if not d.is_dma and not op.is_dma and d.eng == op.eng:
            if op.eng == "pe":
                return
        if d.is_dma and op.is_dma and d.eng == op.eng and kind == "WAR":
            pass
        d.sig = True
        op.deps.append(d)

    def op(self, eng, fn, reads=(), writes=(), is_dma=False, chan=None):
        o = Op(eng, fn, is_dma, chan)
        writes = list(writes) + [r for r in reads if r.excl and r not in writes]
        reads = [r for r in reads if not r.excl]
        for r in reads:
            self._add_dep(o, r.last_w, "RAW")
        for w in writes:
            self._add_dep(o, w.last_w, "WAW")
            for rd in w.readers.values():
                self._add_dep(o, rd, "WAR")
            for rd in w.dma_readers:
                self._add_dep(o, rd, "WAR")
        for r in reads:
            if is_dma:
                r.dma_readers.append(o)
            else:
                r.readers[eng] = o
        for w in writes:
            w.last_w = o
            w.readers = {}
            w.dma_readers = []
        if is_dma:
            c = self.chan_count.get(chan, 0) + 1
            self.chan_count[chan] = c
            o.val = 16 * c
        self.ops[eng].append(o)
        self.n_ops += 1
        return o

    def wait_all(self, eng, resources):
        o = Op(eng, None)
        for w in resources:
            for d in [w.last_w] + list(w.readers.values()) + list(w.dma_readers):
                if d is None or (not d.is_dma and d.eng == eng and eng == "pe"):
                    continue
                d.sig = True
                o.deps.append(d)
        self.ops[eng].append(o)
        self.n_ops += 1
        return o

    def dma(self, queue, out_ap, in_ap, reads, writes, chan):
        def fn(e, out_ap=out_ap, in_ap=in_ap):
            return e.dma_start(out=out_ap, in_=in_ap)

        return self.op(queue, fn, reads, writes, is_dma=True, chan=chan)

    def emit(self):
        nc = self.nc
        with ExitStack() as es:
            eng_sem = {e: es.enter_context(nc.semaphore("s_" + e)) for e in self.ENGS}
            chan_sem = {}
            for c in self.chan_count:
                chan_sem[c] = es.enter_context(nc.semaphore("c_%d" % len(chan_sem)))
            for e in self.ENGS:
                cnt = 0
                for o in self.ops[e]:
                    if o.is_dma:
                        o.sem = chan_sem[o.chan]
                    else:
                        o.sem = eng_sem[e]
                        if o.sig:
                            cnt += 1
                            o.val = cnt
            block = es.enter_context(nc.Block())

            def emit_eng(eng_name, e):
                waited = {}
                for o in self.ops[eng_name]:
                    need = {}
                    for d in o.deps:
                        k = id(d.sem)
                        if waited.get(k, 0) >= d.val:
                            continue
                        if k not in need or need[k][1] < d.val:
                            need[k] = (d.sem, d.val)
                    for k, (sem, val) in need.items():
                        e.wait_ge(sem, val)
                        waited[k] = val
                    if o.fn is None:
                        assert not o.sig
                    else:
                        ins = o.fn(e)
                        if o.is_dma:
                            ins.then_inc(o.sem, 16)
                        elif o.sig:
                            ins.then_inc(o.sem, 1)

            @block.tensor
            def _(e):
                emit_eng("pe", e)

            @block.scalar
            def _(e):
                emit_eng("act", e)

            @block.vector
            def _(e):
                emit_eng("dve", e)

            @block.gpsimd
            def _(e):
                emit_eng("pool", e)

            @block.sync
            def _(e):
                emit_eng("sp", e)


class Ctx:
    def __init__(self, nc, S):
        self.nc = nc
        self.S = S
        self.P = Prog(nc)
        self.NT = S // 128
        self.res_cache = {}
        self.dbg32 = None
        self.dbg16 = None
        self.dbg_off = {False: 0, True: 0}
        self.dbg_map = {}

    def dbg(self, tile, ap2d, name):
        if self.dbg32 is None:
            return
        is16 = ap2d.dtype == BF16
        d = self.dbg16 if is16 else self.dbg32
        off = self.dbg_off[is16]
        p, n = ap2d.shape
        self.dbg_map[name] = (is16, off, p, n)
        self.dbg_off[is16] = off + n
        self.P.dma("sp", d[0:p, off:off + n], ap2d, [tile.r], [self.R("dbgout")], ("dbg", 0))

    def R(self, name):
        r = self.res_cache.get(name)
        if r is None:
            r = Res(name)
            self.res_cache[name] = r
        return r


class Tile:
    def __init__(self, t, name, excl=False):
        self.t = t
        self.r = Res(name, excl)

    def __getitem__(self, k):
        return self.t[k]


def phase_alloc(cx, es):
    nc = cx.nc
    cnt = [0]

    def sb(name, shape, dt):
        cnt[0] += 1
        return Tile(es.enter_context(nc.sbuf_tensor("%s_%d" % (name, cx.P.n_ops), shape, dt)), name)

    def ps(name, shape, dt):
        cnt[0] += 1
        return Tile(es.enter_context(nc.psum_tensor("%s_%d" % (name, cx.P.n_ops), shape, dt)), name, excl=True)

    return sb, ps


def barrier(cx, tiles):
    for eng in ("pe", "act", "dve", "pool", "sp"):
        cx.P.wait_all(eng, [t.r for t in tiles])


def load_gain_bc(cx, g_bc, g_row_ap):
    cx.P.dma("sp", g_bc[:], g_row_ap.partition_broadcast(128), [], [g_bc.r], ("g", g_bc.r.name))


def rms_prep(cx, xt, junk, ssq, rstd, xn, g_bc):
    P = cx.P
    P.op("act", lambda e: e.activation(out=junk[:], in_=xt[:], func=AF.Square, accum_out=ssq[:]),
         [xt.r], [junk.r, ssq.r])
    P.op("act", lambda e: e.activation(out=rstd[:], in_=ssq[:], func=AF.Sqrt, scale=1.0 / D_MODEL, bias=EPS),
         [ssq.r], [rstd.r])
    P.op("dve", lambda e: e.reciprocal(out=rstd[:], in_=rstd[:]), [rstd.r], [rstd.r])
    P.op("dve", lambda e: e.scalar_tensor_tensor(out=xn[:], in0=xt[:], scalar=rstd[:], in1=g_bc[:],
                                                 op0=ALU.mult, op1=ALU.mult),
         [xt.r, rstd.r, g_bc.r], [xn.r])


def phase_ffn(cx, X, g_row, wg_d, wu_d, wd_d, ident, Xin=None):
    nc, P, S = cx.nc, cx.P, cx.S
    if Xin is None:
        Xin = X
    TT = 256
    nt = S // TT
    NFC = FFN_HIDDEN // 128
    with ExitStack() as es:
        sb, ps = phase_alloc(cx, es)
        wg = sb("wg", [128, 8, FFN_HIDDEN], BF16)
        wu = sb("wu", [128, 8, FFN_HIDDEN], BF16)
        wd = sb("wd", [128, NFC, D_MODEL], BF16)
        g_bc = sb("g_bc", [128, D_MODEL], F32)
        xt = [[sb("xt%d%d" % (b, a), [128, D_MODEL], F32) for a in range(2)] for b in range(2)]
        junk = sb("junk", [128, D_MODEL], BF16)
        ssq = [[sb("ssq%d%d" % (b, a), [128, 1], F32) for a in range(2)] for b in range(2)]
        rstd = [[sb("rstd%d%d" % (b, a), [128, 1], F32) for a in range(2)] for b in range(2)]
        xn = [sb("xn%d" % a, [128, D_MODEL], BF16) for a in range(2)]
        xnT = [sb("xnT%d" % b, [128, 8, TT], BF16) for b in range(2)]
        sg = [sb("sg%d" % i, [128, TT], F32) for i in range(2)]
        hT = [sb("hT%d" % i, [128, TT], BF16) for i in range(3)]
        xo = [sb("xo%d" % a, [128, D_MODEL], F32) for a in range(2)]
        pT = [ps("pT%d" % a, [128, 8, 128], BF16) for a in range(2)]
        pGU = [ps("pGU%d" % i, [128, 512], F32) for i in range(2)]
        pO = [[ps("pO%d%d" % (a, h), [128, 512], F32) for h in range(2)] for a in range(2)]
        idt = sb("idt", [128, 128], BF16)
        tiles_extra = []
        all_tiles = ([wg, wu, wd, g_bc, junk, idt] + sum(xt, []) + sum(ssq, []) + sum(rstd, []) + xn + xnT + sg
                     + hT + xo + pT + pGU + sum(pO, []))

        P.dma("sp", idt[:], ident, [], [idt.r], ("id", 0))
        load_gain_bc(cx, g_bc, g_row)
        wgv = wg_d.rearrange("(kc p) f -> p kc f", p=128)
        wuv = wu_d.rearrange("(kc p) f -> p kc f", p=128)
        wdv = wd_d.rearrange("(fc p) d -> p fc d", p=128)
        FH = FFN_HIDDEN // 2
        wres = {}
        for fh in range(2):
            for (dst, src, nm) in ((wg, wgv, "wg"), (wu, wuv, "wu")):
                for kh in range(2):
                    r_ = Res("%s_%d_%d" % (nm, fh, kh))
                    wres[(nm, fh, kh)] = r_
                    tiles_extra.append(r_)
                    P.dma("pool", dst[:, kh * 4:(kh + 1) * 4, fh * FH:(fh + 1) * FH],
                          src[:, kh * 4:(kh + 1) * 4, fh * FH:(fh + 1) * FH], [], [r_], ("w", nm, fh, kh))
            r_ = Res("wd_%d" % fh)
            wres[("wd", fh)] = r_
            tiles_extra.append(r_)
            P.dma("pool", wd[:, fh * 11:(fh + 1) * 11, :], wdv[:, fh * 11:(fh + 1) * 11, :], [], [r_], ("w", "wd", fh))

        def xres(t, a):
            return cx.R("X%d" % (t * 2 + a))

        def prep_load(t):
            b = t % 2
            for a in range(2):
                r0 = t * TT + a * 128
                P.dma("sp", xt[b][a][:], Xin[r0:r0 + 128, :], [xres(t, a)], [xt[b][a].r], ("xt", b, a))

        def prep_norm(t):
            b = t % 2
            for a in range(2):
                rms_prep(cx, xt[b][a], junk, ssq[b][a], rstd[b][a], xn[a], g_bc)

        def prep_T(t):
            b = t % 2
            for a in range(2):
                for kc in range(8):
                    P.op("pe", lambda e, a=a, kc=kc: e.transpose(out=pT[a][:, kc, :],
                                                                 in_=xn[a][:, kc * 128:(kc + 1) * 128],
                                                                 identity=idt[:]),
                         [xn[a].r, idt.r], [pT[a].r])
                P.op("act", lambda e, a=a, b=b: e.activation(out=xnT[b][:, :, a * 128:(a + 1) * 128], in_=pT[a][:],
                                                             func=AF.Copy),
                     [pT[a].r], [xnT[b].r])

        def gu(t, fc):
            b = t % 2
            p = pGU[fc % 2]
            for (w_, off, nm) in ((wg, 0, "wg"), (wu, TT, "wu")):
                for kc in range(8):
                    P.op("pe", lambda e, w_=w_, off=off, kc=kc, p=p, b=b, fc=fc: e.matmul(
                        out=p[:, off:off + TT], lhsT=w_[:, kc, fc * 128:(fc + 1) * 128], rhs=xnT[b][:, kc, :],
                        start=(kc == 0), stop=(kc == 7)), [wres[(nm, fc // 11, kc // 4)], xnT[b].r], [p.r])

        def actmul(t, fc):
            p = pGU[fc % 2]
            s_ = sg[fc % 2]
            h_ = hT[fc % 3]
            P.op("act", lambda e, p=p, s_=s_: e.activation(out=s_[:], in_=p[:, 0:TT], func=AF.Silu), [p.r], [s_.r])
            P.op("dve", lambda e, p=p, s_=s_, h_=h_: e.tensor_tensor(out=h_[:], in0=s_[:], in1=p[:, TT:2 * TT],
                                                                    op=ALU.mult), [s_.r, p.r], [h_.r])

        def down(t, fc):
            h_ = hT[fc % 3]
            for a in range(2):
                for hf in range(2):
                    P.op("pe", lambda e, a=a, hf=hf, h_=h_, fc=fc: e.matmul(
                        out=pO[a][hf][:], lhsT=h_[:, a * 128:(a + 1) * 128], rhs=wd[:, fc, hf * 512:(hf + 1) * 512],
                        start=(fc == 0), stop=(fc == NFC - 1)), [h_.r, wres[("wd", fc // 11)]], [pO[a][hf].r])

        def finish(t):
            b = t % 2
            for a in range(2):
                for hf in range(2):
                    P.op("dve", lambda e, a=a, hf=hf, b=b: e.scalar_tensor_tensor(
                        out=xo[a][:, hf * 512:(hf + 1) * 512], in0=pO[a][hf][:], scalar=0.5,
                        in1=xt[b][a][:, hf * 512:(hf + 1) * 512], op0=ALU.mult, op1=ALU.add),
                        [pO[a][hf].r, xt[b][a].r], [xo[a].r])
                r0 = t * TT + a * 128
                P.dma("sp", X[r0:r0 + 128, :], xo[a][:], [xo[a].r], [xres(t, a)], ("xo", a))

        prep_load(0)
        prep_norm(0)
        prep_T(0)
        for t in range(nt):
            for fc in range(NFC):
                gu(t, fc)
                if t + 1 < nt:
                    if fc == 0:
                        prep_load(t + 1)
                    if fc == 5:
                        prep_norm(t + 1)
                    if fc == 13:
                        prep_T(t + 1)
                actmul(t, fc)
                if fc >= 1:
                    down(t, fc - 1)
            down(t, NFC - 1)
            finish(t)
        for eng in ("pe", "act", "dve", "pool", "sp"):
            cx.P.wait_all(eng, [t_.r for t_ in all_tiles] + tiles_extra)


def phase_final(cx, X, g_row, OUT):
    nc, P, S = cx.nc, cx.P, cx.S
    with ExitStack() as es:
        sb, ps = phase_alloc(cx, es)
        g_bc = sb("g_bc", [128, D_MODEL], F32)
        xt = [sb("xt%d" % b, [128, D_MODEL], F32) for b in range(2)]
        junk = sb("junk", [128, D_MODEL], BF16)
        ssq = [sb("ssq%d" % b, [128, 1], F32) for b in range(2)]
        rstd = [sb("rstd%d" % b, [128, 1], F32) for b in range(2)]
        xo = [sb("xo%d" % b, [128, D_MODEL], F32) for b in range(2)]
        all_tiles = [g_bc, junk] + xt + ssq + rstd + xo
        load_gain_bc(cx, g_bc, g_row)
        outs = []
        for t in range(cx.NT):
            b = t % 2
            P.dma("sp", xt[b][:], X[t * 128:(t + 1) * 128, :], [cx.R("X%d" % t)], [xt[b].r], ("xt", b))
            rms_prep(cx, xt[b], junk, ssq[b], rstd[b], xo[b], g_bc)
            ro = cx.R("OUT%d" % t)
            outs.append(ro)
            P.dma("sp", OUT[t * 128:(t + 1) * 128, :], xo[b][:], [xo[b].r], [ro], ("xo", b))
        P.op("sp", None, outs, [])
        barrier(cx, all_tiles)


INPUT_SHAPES = {
    "ffn_pre_norm": [4, 1024], "ffn_pre_w_gate": [4, 1024, 2816], "ffn_pre_w_up": [4, 1024, 2816],
    "ffn_pre_w_down": [4, 2816, 1024], "mix_norm": [4, 1024], "ab_w_in": [2, 1024, 3584],
    "ab_w_out": [2, 1024, 1024], "gla_w_in": [2, 1024, 3088], "gla_w_a2": [2, 16, 512], "gla_b_a": [2, 512],
    "gla_w_out": [2, 1024, 1024], "ffn_post_norm": [4, 1024], "ffn_post_w_gate": [4, 1024, 2816],
    "ffn_post_w_up": [4, 1024, 2816], "ffn_post_w_down": [4, 2816, 1024], "final_norm": [1024],
}


def build_program(S, phases, names):
    return _build_program(S, phases, names)[0]


def _build_program(S, phases, names):
    nc = bass.Bass("TRN2", target_bir_lowering=False)
    x_in = nc.dram_tensor("x", [S, D_MODEL], F32, kind="ExternalInput").ap()
    ident = nc.dram_tensor("ident", [128, 128], BF16, kind="ExternalInput").ap()
    cf_d = nc.dram_tensor("cf32", list(CF32.shape), F32, kind="ExternalInput").ap()
    ce_d = nc.dram_tensor("ce32", list(CE32.shape), F32, kind="ExternalInput").ap()
    ropeR_d = nc.dram_tensor("ropeR", [S, 512], F32, kind="ExternalInput").ap()
    ropeD_d = nc.dram_tensor("ropeD", [S, 128], F32, kind="ExternalInput").ap()
    scr = {
        "QD": nc.dram_tensor("qd", [S, 512], BF16, kind="Internal").ap(),
        "KD": nc.dram_tensor("kd", [S, 512], BF16, kind="Internal").ap(),
        "VD": nc.dram_tensor("vd", [S, 520], BF16, kind="Internal").ap(),
        "ORD": nc.dram_tensor("ord", [S, 512], BF16, kind="Internal").ap(),
        "UB": [nc.dram_tensor("ub%d" % i, [S, 520], F32, kind="Internal").ap() for i in range(3)],
    }
    W = {n: nc.dram_tensor(n, INPUT_SHAPES[n], F32, kind="ExternalInput").ap() for n in names}
    OUT = nc.dram_tensor("out", [S, D_MODEL], F32, kind="ExternalOutput").ap()
    cx = Ctx(nc, S)
    import os
    if os.environ.get("KDEBUG"):
        cx.dbg32 = nc.dram_tensor("dbg32", [128, 16384], F32, kind="ExternalOutput").ap()
        cx.dbg16 = nc.dram_tensor("dbg16", [128, 16384], BF16, kind="ExternalOutput").ap()
    X = x_in
    Xs = nc.dram_tensor("xs", [S, D_MODEL], F32, kind="Internal").ap()
    X = Xs
    first = [True]
    for ph in phases:
        if ph[0] == "ffn":
            l, which = ph[1], ph[2]
            phase_ffn(cx, X, W["ffn_%s_norm" % which][l], W["ffn_%s_w_gate" % which][l],
                      W["ffn_%s_w_up" % which][l], W["ffn_%s_w_down" % which][l], ident,
                      Xin=(x_in if first[0] else None))
            first[0] = False
        elif ph[0] == "copy":
            prev = None
            for t in range(cx.NT):
                cx.P.dma("sp", Xs[t * 128:(t + 1) * 128, :], x_in[t * 128:(t + 1) * 128, :],
                         [cx.R("xcopy_chain")], [cx.R("X%d" % t), cx.R("xcopy_chain")], ("xcopy", 0))
        elif ph[0] == "mixA":
            i = ph[1]
            phase_even(cx, X, W["mix_norm"][2 * i], W["ab_w_in"][i], W["ab_w_out"][i], ident, ce_d, ropeR_d, ropeD_d, scr)
        elif ph[0] == "gla":
            i = ph[1]
            l = 2 * i + 1
            phase_gla(cx, X, W["mix_norm"][l], W["gla_w_in"][i], W["gla_w_a2"][i], W["gla_b_a"][i], W["gla_w_out"][i],
                      ident, cf_d)
        elif ph[0] == "final":
            phase_final(cx, X, W["final_norm"], OUT)
        else:
            raise ValueError(ph)
    cx.P.emit()
    return nc, cx


def make_consts():
    cf = {}
    tok = np.arange(128)
    same64 = (tok[:, None] // 64) == (tok[None, :] // 64)
    cf["mc"] = np.where(same64 & (tok[:, None] <= tok[None, :]), -1.0 / 16, 0.0)
    cf["ms"] = np.where(same64 & (tok[:, None] > tok[None, :]), -1.0 / 16, 0.0)
    cf["mch"] = np.stack([np.where(tok < 64, -1.0 / 16, 0.0), np.where(tok >= 64, -1.0 / 16, 0.0)], 1)
    mT = np.where(same64 & (tok[None, :] >= tok[:, None]), 1.0, 0.0)
    cf["gmaskT"] = np.repeat(mT[:, None, :], 4, axis=1).reshape(128, 512)
    cf["mab"] = np.stack([np.where(tok < 64, 1.0, 0.0), np.where(tok >= 64, 1.0, 0.0)], 1)
    off = {}
    cols = []
    c0 = 0
    for k, v in cf.items():
        v = np.asarray(v, np.float32).reshape(128, -1)
        off[k] = (c0, v.shape[1])
        cols.append(v)
        c0 += v.shape[1]
    return np.ascontiguousarray(np.concatenate(cols, 1)), off


CF32, CF_OFF = make_consts()


GLA_LEAD = 10 ** 9


def run_interleaved(bodies, lead):
    active = []
    it = iter(bodies)
    nxt = next(it, None)
    while active or nxt is not None:
        if nxt is not None and (len(active) == 0 or (len(active) == 1 and active[0][1] >= lead)):
            active.append([nxt, 0])
            nxt = next(it, None)
        for a_ in list(active):
            try:
                next(a_[0])
                a_[1] += 1
            except StopIteration:
                active.remove(a_)


def phase_gla(cx, X, g_row, w_in_d, w_a2_d, b_a_d, w_out_d, ident, cf_d):
    nc, P, S = cx.nc, cx.P, cx.S
    H, DK, DV = 4, 128, 256
    GIN = 3088
    with ExitStack() as es:
        sb, ps = phase_alloc(cx, es)
        tiles = []

        def SB(name, shape, dt):
            t = sb(name, shape, dt)
            tiles.append(t)
            return t

        def PS(name, shape, dt):
            t = ps(name, shape, dt)
            tiles.append(t)
            return t

        w_in = SB("w_in", [128, 8, GIN], BF16)
        w_out = SB("w_out", [128, 8, D_MODEL], BF16)
        wa2 = SB("wa2", [17, 512], F32)
        cf = SB("cf", [128, CF32.shape[1]], F32)
        idt = SB("idt", [128, 128], BF16)
        g_bc = SB("g_bc", [128, D_MODEL], F32)
        xt = [SB("xt%d" % b, [128, D_MODEL], F32) for b in range(2)]
        junk = SB("junk", [128, D_MODEL], BF16)
        ssq_2 = [SB("ssq_%d" % i_, [128, 1], F32) for i_ in range(2)]
        rstd_2 = [SB("rstd_%d" % i_, [128, 1], F32) for i_ in range(2)]
        xn_2 = [SB("xn_%d" % i_, [128, D_MODEL], BF16) for i_ in range(2)]
        xnT_2 = [SB("xnT_%d" % i_, [128, 8, 128], BF16) for i_ in range(2)]
        alT_2 = [SB("alT_%d" % i_, [17, 128], F32) for i_ in range(2)]
        e1_2 = [SB("e1_%d" % i_, [128, 512], F32) for i_ in range(2)]
        l1_2 = [SB("l1_%d" % i_, [128, 512], F32) for i_ in range(2)]
        E1_2 = [SB("E1_%d" % i_, [128, 512], F32) for i_ in range(2)]
        E2_2 = [SB("E2_%d" % i_, [128, 512], F32) for i_ in range(2)]
        E3_2 = [SB("E3_%d" % i_, [128, 512], F32) for i_ in range(2)]
        qt_2 = [SB("qt_%d" % i_, [128, 512], BF16) for i_ in range(2)]
        kt_2 = [SB("kt_%d" % i_, [128, 512], BF16) for i_ in range(2)]
        kd_2 = [[SB("kd%d_%d" % (c, i_), [128, 512], BF16) for c in range(2)] for i_ in range(2)]
        vb_2 = [SB("vb_%d" % i_, [128, 1024], BF16) for i_ in range(2)]
        sr_2 = [SB("sr_%d" % i_, [128, 1024], F32) for i_ in range(2)]
        qTf_2 = [SB("qTf_%d" % i_, [128, 4, 128], BF16) for i_ in range(2)]
        qT2_2 = [SB("qT2_%d" % i_, [128, 4, 2, 128], BF16) for i_ in range(2)]
        kT_2 = [SB("kT_%d" % i_, [128, 4, 128], BF16) for i_ in range(2)]
        sT_2 = [SB("sT_%d" % i_, [128, 4, 128], BF16) for i_ in range(2)]
        st = [SB("st%d" % i, [128, 4, 256], F32) for i in range(2)]
        sbf = [SB("sbf%d" % i, [128, 4, 256], BF16) for i in range(3)]
        dec_2 = [SB("dec_%d" % i_, [128, 4, 2], F32) for i_ in range(2)]
        bst_2 = [SB("bst_%d" % i_, [128, 4, 6], F32) for i_ in range(2)]
        mv_2 = [SB("mv_%d" % i_, [128, 4, 2], F32) for i_ in range(2)]
        hr_2 = [SB("hr_%d" % i_, [128, 4], F32) for i_ in range(2)]
        tmp_2 = [SB("tmp_%d" % i_, [128, 1024], F32) for i_ in range(2)]
        og_2 = [SB("og_%d" % i_, [128, 1024], BF16) for i_ in range(2)]
        ogT_2 = [SB("ogT_%d" % i_, [128, 8, 128], BF16) for i_ in range(2)]
        xo_2 = [SB("xo_%d" % i_, [128, D_MODEL], F32) for i_ in range(2)]

        pT = PS("pT", [128, 8, 128], BF16)
        pP = [PS("pP%d" % i, [128, 512], F32) for i in range(2)]
        pB = [PS("pB%d" % i, [128, 512], F32) for i in range(2)]
        pSm = PS("pSm", [128, 512], F32)
        pTq = PS("pTq", [128, 2, 4, 128], BF16)
        pS = PS("pS", [128, 4, 128], F32)

        def cfv(name):
            o, w = CF_OFF[name]
            return cf[:, o:o + w]

        P.dma("sp", idt[:], ident, [], [idt.r], ("id", 0))
        P.dma("sp", cf[:], cf_d, [], [cf.r], ("cf", 0))
        load_gain_bc(cx, g_bc, g_row)
        P.dma("sp", wa2[0:16, :], w_a2_d, [], [wa2.r], ("wa2", 0))
        P.dma("sp", wa2[16:17, :], b_a_d.rearrange("(o f) -> o f", o=1), [], [wa2.r], ("wa2", 0))
        wv = w_in_d.rearrange("(kc p) f -> p kc f", p=128)
        for kh in range(2):
            for (c0, c1) in ((0, 1544), (1544, GIN)):
                P.dma("pool", w_in[:, kh * 4:(kh + 1) * 4, c0:c1], wv[:, kh * 4:(kh + 1) * 4, c0:c1], [], [w_in.r],
                      ("w", "w_in"))
        wov = w_out_d.rearrange("(kc p) f -> p kc f", p=128)
        for kh in range(2):
            P.dma("pool", w_out[:, kh * 4:(kh + 1) * 4, :], wov[:, kh * 4:(kh + 1) * 4, :], [], [w_out.r], ("w", "w_out"))
        for alT in alT_2:
            P.op("dve", lambda e, alT=alT: e.memset(alT[:], 1.0), [], [alT.r])
        P.op("dve", lambda e: e.memset(st[0][:], 0.0), [], [st[0].r])
        P.op("dve", lambda e: e.memset(sbf[0][:], 0.0), [], [sbf[0].r])
        for qT2 in qT2_2:
            P.op("pool", lambda e, qT2=qT2: e.memset(qT2[:], 0.0), [], [qT2.r])

        sc = DK ** -0.5
        def body(t):
            b = t % 2
            k_ = t % 2
            ssq = ssq_2[k_]
            rstd = rstd_2[k_]
            xn = xn_2[k_]
            xnT = xnT_2[k_]
            alT = alT_2[k_]
            e1 = e1_2[k_]
            l1 = l1_2[k_]
            E1 = E1_2[k_]
            E2 = E2_2[k_]
            E3 = E3_2[k_]
            qt = qt_2[k_]
            kt = kt_2[k_]
            vb = vb_2[k_]
            sr = sr_2[k_]
            qTf = qTf_2[k_]
            qT2 = qT2_2[k_]
            kT = kT_2[k_]
            sT = sT_2[k_]
            dec = dec_2[k_]
            bst = bst_2[k_]
            mv = mv_2[k_]
            hr = hr_2[k_]
            tmp = tmp_2[k_]
            og = og_2[k_]
            ogT = ogT_2[k_]
            xo = xo_2[k_]
            kd = kd_2[k_]
            xr = cx.R("X%d" % t)
            P.dma("sp", xt[b][:], X[t * 128:(t + 1) * 128, :], [xr], [xt[b].r], ("xt", b))
            rms_prep(cx, xt[b], junk, ssq, rstd, xn, g_bc)
            for kc in range(8):
                P.op("pe", lambda e, kc=kc: e.transpose(out=pT[:, kc, :], in_=xn[:, kc * 128:(kc + 1) * 128],
                                                        identity=idt[:]), [xn.r, idt.r], [pT.r])
            P.op("act", lambda e: e.activation(out=xnT[:], in_=pT[:], func=AF.Copy), [pT.r], [xnT.r])
            yield
            for kc in range(8):
                P.op("pe", lambda e, kc=kc: e.matmul(out=pSm[0:16, 0:128], lhsT=w_in[:, kc, 3072:3088], rhs=xnT[:, kc, :],
                                                     start=(kc == 0), stop=(kc == 7)), [w_in.r, xnT.r], [pSm.r])
            P.op("act", lambda e: e.activation(out=alT[0:16, :], in_=pSm[0:16, 0:128], func=AF.Copy), [pSm.r], [alT.r])
            yield
            P.op("pe", lambda e: e.matmul(out=pB[0][:], lhsT=alT[:], rhs=wa2[:], start=True, stop=True),
                 [alT.r, wa2.r], [pB[0].r])
            P.op("act", lambda e: e.activation(out=e1[:], in_=pB[0][:], func=AF.Exp, scale=-1.0), [pB[0].r], [e1.r])
            P.op("act", lambda e: e.activation(out=l1[:], in_=e1[:], func=AF.Ln, bias=1.0), [e1.r], [l1.r])
            yield
            P.op("pe", lambda e: e.matmul(out=pB[0][:], lhsT=cfv("mc"), rhs=l1[:], start=True, stop=True),
                 [cf.r, l1.r], [pB[0].r])
            P.op("pe", lambda e: e.matmul(out=pB[1][:], lhsT=cfv("ms"), rhs=l1[:], start=True, stop=True),
                 [cf.r, l1.r], [pB[1].r])
            for h in range(H):
                P.op("pe", lambda e, h=h: e.matmul(out=pSm[:, 128 + 2 * h:130 + 2 * h], lhsT=l1[:, h * 128:(h + 1) * 128],
                                                   rhs=cfv("mch"), start=True, stop=True), [cf.r, l1.r], [pSm.r])
            P.op("act", lambda e: e.activation(out=E1[:], in_=pB[0][:], func=AF.Exp), [pB[0].r], [E1.r])
            P.op("act", lambda e: e.activation(out=E2[:], in_=pB[0][:], func=AF.Exp, scale=-1.0), [pB[0].r], [E2.r])
            P.op("act", lambda e: e.activation(out=E3[:], in_=pB[1][:], func=AF.Exp), [pB[1].r], [E3.r])
            P.op("act", lambda e: e.activation(out=dec[:].rearrange("p h c -> p (h c)"), in_=pSm[:, 128:136], func=AF.Exp),
                 [pSm.r], [dec.r])
            yield
            def proj(gi, pp):
                for kc in range(8):
                    P.op("pe", lambda e, kc=kc, gi=gi, pp=pp: e.matmul(out=pp[:], lhsT=xnT[:, kc, :],
                                                                       rhs=w_in[:, kc, gi * 512:(gi + 1) * 512],
                                                                       start=(kc == 0), stop=(kc == 7)),
                         [xnT.r, w_in.r], [pp.r])
            proj(0, pP[0])
            P.op("dve", lambda e: e.scalar_tensor_tensor(out=qt[:], in0=pP[0][:], scalar=sc, in1=E1[:], op0=ALU.mult,
                                                         op1=ALU.mult), [pP[0].r, E1.r], [qt.r])
            yield
            proj(1, pP[1])
            P.op("dve", lambda e: e.tensor_tensor(out=kt[:], in0=pP[1][:], in1=E2[:], op=ALU.mult), [pP[1].r, E2.r], [kt.r])
            for c in range(2):
                P.op("dve", lambda e, c=c: e.scalar_tensor_tensor(out=kd[c][:], in0=pP[1][:], scalar=cfv("mab")[:, c:c + 1],
                                                                  in1=E3[:], op0=ALU.mult, op1=ALU.mult),
                     [pP[1].r, E3.r, cf.r], [kd[c].r])
            for gi in (2, 3):
                yield
                pp = pP[gi % 2]
                proj(gi, pp)
                P.op("act", lambda e, gi=gi, pp=pp: e.activation(out=vb[:, (gi - 2) * 512:(gi - 1) * 512], in_=pp[:],
                                                                 func=AF.Copy), [pp.r], [vb.r])
            for gi in (4, 5):
                yield
                pp = pP[gi % 2]
                proj(gi, pp)
                P.op("act", lambda e, gi=gi, pp=pp: e.activation(out=sr[:, (gi - 4) * 512:(gi - 3) * 512], in_=pp[:],
                                                                 func=AF.Silu), [pp.r], [sr.r])
            yield
            for h in range(H):
                P.op("pe", lambda e, h=h: e.transpose(out=pTq[:, 0, h, :], in_=qt[:, h * 128:(h + 1) * 128], identity=idt[:]),
                     [qt.r, idt.r], [pTq.r])
            for h in range(H):
                P.op("pe", lambda e, h=h: e.transpose(out=pTq[:, 1, h, :], in_=kt[:, h * 128:(h + 1) * 128], identity=idt[:]),
                     [kt.r, idt.r], [pTq.r])
            P.op("act", lambda e: e.activation(out=qTf[:], in_=pTq[:, 0, :, :], func=AF.Copy), [pTq.r], [qTf.r])
            P.op("dve", lambda e: e.tensor_copy(out=kT[:], in_=pTq[:, 1, :, :]), [pTq.r], [kT.r])
            P.op("pool", lambda e: e.tensor_copy(out=qT2[:, :, 0, 0:64], in_=qTf[:, :, 0:64]), [qTf.r], [qT2.r])
            P.op("pool", lambda e: e.tensor_copy(out=qT2[:, :, 1, 64:128], in_=qTf[:, :, 64:128]), [qTf.r], [qT2.r])
            yield
            for h in range(H):
                P.op("pe", lambda e, h=h: e.matmul(out=pS[:, h, :], lhsT=kT[:, h, :], rhs=qTf[:, h, :], start=True, stop=True),
                     [kT.r, qTf.r], [pS.r])
            P.op("dve", lambda e: e.tensor_tensor(out=sT[:].rearrange("p h i -> p (h i)"),
                                                  in0=pS[:].rearrange("p h i -> p (h i)"), in1=cfv("gmaskT"), op=ALU.mult),
                 [pS.r, cf.r], [sT.r])
            yield
            b0, b1, b2 = (2 * t) % 3, (2 * t + 1) % 3, (2 * t + 2) % 3
            s0, s1 = 0, 1
            for h in range(H):
                pk = pB[h % 2]
                for c in range(2):
                    P.op("pe", lambda e, h=h, c=c, pk=pk: e.matmul(out=pk[:, c * 256:(c + 1) * 256],
                                                                  lhsT=kd[c][:, h * 128:(h + 1) * 128],
                                                                  rhs=vb[:, h * 256:(h + 1) * 256], start=True, stop=True),
                         [kd[c].r, vb.r], [pk.r])
                P.op("dve", lambda e, h=h, pk=pk, s0=s0, s1=s1: e.scalar_tensor_tensor(out=st[s1][:, h, :], in0=st[s0][:, h, :],
                                                                         scalar=dec[:, h, 0:1], in1=pk[:, 0:256],
                                                                         op0=ALU.mult, op1=ALU.add),
                     [st[s0].r, dec.r, pk.r], [st[s1].r])
                P.op("pool", lambda e, h=h, b1=b1, s1=s1: e.tensor_copy(out=sbf[b1][:, h, :], in_=st[s1][:, h, :]), [st[s1].r], [sbf[b1].r])
                P.op("dve", lambda e, h=h, pk=pk, s0=s0, s1=s1: e.scalar_tensor_tensor(out=st[s0][:, h, :], in0=st[s1][:, h, :],
                                                                         scalar=dec[:, h, 1:2], in1=pk[:, 256:512],
                                                                         op0=ALU.mult, op1=ALU.add),
                     [st[s1].r, dec.r, pk.r], [st[s0].r])
                P.op("pool", lambda e, h=h, b2=b2, s0=s0: e.tensor_copy(out=sbf[b2][:, h, :], in_=st[s0][:, h, :]), [st[s0].r], [sbf[b2].r])
            for h in range(H):
                po = pP[h // 2]
                osl = slice((h % 2) * 256, (h % 2 + 1) * 256)
                P.op("pe", lambda e, h=h, po=po, osl=osl, b0=b0, b1=b1: e.matmul(out=po[:, osl], lhsT=sT[:, h, :],
                                                                  rhs=vb[:, h * 256:(h + 1) * 256], start=True, stop=False),
                     [sT.r, vb.r], [po.r])
                P.op("pe", lambda e, h=h, po=po, osl=osl, b0=b0, b1=b1: e.matmul(out=po[:, osl], lhsT=qT2[:, h, 0, :],
                                                                  rhs=sbf[b0][:, h, :], start=False, stop=False),
                     [qT2.r, sbf[b0].r], [po.r])
                P.op("pe", lambda e, h=h, po=po, osl=osl, b0=b0, b1=b1: e.matmul(out=po[:, osl], lhsT=qT2[:, h, 1, :],
                                                                  rhs=sbf[b1][:, h, :], start=False, stop=True),
                     [qT2.r, sbf[b1].r], [po.r])
            for h in range(H):
                po = pP[h // 2]
                osl = slice((h % 2) * 256, (h % 2 + 1) * 256)
                P.op("dve", lambda e, h=h, po=po, osl=osl: e.bn_stats(out=bst[:, h, :], in_=po[:, osl]), [po.r], [bst.r])
                P.op("dve", lambda e, h=h: e.bn_aggr(out=mv[:, h, :], in_=bst[:, h, :]), [bst.r], [mv.r])
            P.op("act", lambda e: e.activation(out=hr[:], in_=mv[:, :, 1], func=AF.Sqrt, bias=EPS), [mv.r], [hr.r])
            P.op("dve", lambda e: e.reciprocal(out=hr[:], in_=hr[:]), [hr.r], [hr.r])
            for h in range(H):
                po = pP[h // 2]
                osl = slice((h % 2) * 256, (h % 2 + 1) * 256)
                P.op("dve", lambda e, h=h, po=po, osl=osl: e.tensor_scalar(out=tmp[:, h * 256:(h + 1) * 256], in0=po[:, osl],
                                                                          scalar1=mv[:, h, 0:1], scalar2=hr[:, h:h + 1],
                                                                          op0=ALU.subtract, op1=ALU.mult),
                     [po.r, mv.r, hr.r], [tmp.r])
            P.op("pool", lambda e: e.tensor_tensor(out=og[:], in0=tmp[:], in1=sr[:], op=ALU.mult), [tmp.r, sr.r], [og.r])
            yield
            for ec in range(8):
                P.op("pe", lambda e, ec=ec: e.transpose(out=pT[:, ec, :], in_=og[:, ec * 128:(ec + 1) * 128], identity=idt[:]),
                     [og.r, idt.r], [pT.r])
            P.op("act", lambda e: e.activation(out=ogT[:], in_=pT[:], func=AF.Copy), [pT.r], [ogT.r])
            for hf in range(2):
                for ec in range(8):
                    P.op("pe", lambda e, hf=hf, ec=ec: e.matmul(out=pP[hf][:], lhsT=ogT[:, ec, :],
                                                               rhs=w_out[:, ec, hf * 512:(hf + 1) * 512],
                                                               start=(ec == 0), stop=(ec == 7)), [ogT.r, w_out.r], [pP[hf].r])
                P.op("dve", lambda e, hf=hf, b=b: e.tensor_tensor(out=xo[:, hf * 512:(hf + 1) * 512], in0=pP[hf][:],
                                                                  in1=xt[b][:, hf * 512:(hf + 1) * 512], op=ALU.add),
                     [pP[hf].r, xt[b].r], [xo.r])
            P.dma("sp", X[t * 128:(t + 1) * 128, :], xo[:], [xo.r], [xr], ("xo", k_))
        run_interleaved([body(t) for t in range(cx.NT)], lead=GLA_LEAD)
        barrier(cx, tiles)


RET_G = [1.0 - 2.0 ** (-5.0 - h) for h in range(4)]
DIL_PATTERNS = ((128, 1), (512, 4), (2048, 16))


def make_consts_even(S):
    tok = np.arange(128)
    cf = {}
    lg = [math.log(g) for g in RET_G]
    dm = np.zeros((128, 4, 128))
    gq = np.zeros((128, 4, 128))
    gk = np.zeros((128, 4, 128))
    for h in range(4):
        rel = tok[None, :] - tok[:, None]
        dm[:, h, :] = np.where(rel >= 0, np.exp(lg[h] * np.maximum(rel, 0)), 0.0)
        gq[:, h, :] = np.exp(lg[h] * (tok + 1.0))[:, None]
        gk[:, h, :] = np.exp(lg[h] * (127.0 - tok))[:, None]
    cf["dmT"] = dm.reshape(128, 512)
    cf["gq"] = gq.reshape(128, 512)
    cf["gk"] = gk.reshape(128, 512)
    prev = np.where(tok[:, None] >= tok[None, :], 1.0, 0.0)
    same = np.where(tok[None, :] >= tok[:, None], 1.0, 0.0)
    cf["dilm"] = np.stack([prev, same, prev, same], 1).reshape(128, 512)
    off = {}
    cols = []
    c0 = 0
    for k, v in cf.items():
        v = np.asarray(v, np.float32).reshape(128, -1)
        off[k] = (c0, v.shape[1])
        cols.append(v)
        c0 += v.shape[1]
    ce = np.ascontiguousarray(np.concatenate(cols, 1))
    pos = np.arange(S, dtype=np.float32)
    fr = (np.float32(10000.0) ** (-np.linspace(0.0, 1.0, 64, dtype=np.float32))).astype(np.float32)
    ang = pos[:, None] * fr[None, :]
    ropeR = np.concatenate([np.tile(np.cos(ang), (1, 4)), np.tile(np.sin(ang), (1, 4))], 1).astype(np.float32)
    fd = (np.float32(500000.0) ** (-np.arange(0, 16, 2, dtype=np.float32) / np.float32(16))).astype(np.float32)
    angd = pos[:, None] * fd[None, :]
    ropeD = np.concatenate([np.tile(np.cos(angd), (1, 8)), np.tile(np.sin(angd), (1, 8))], 1).astype(np.float32)
    return ce, off, np.ascontiguousarray(ropeR), np.ascontiguousarray(ropeD)


CE32, CE_OFF, _, _ = make_consts_even(128)


def phase_even(cx, X, g_row, w_in_d, w_out_d, ident, ce_d, ropeR_d, ropeD_d, scr):
    nc, P, S = cx.nc, cx.P, cx.S
    NT = cx.NT
    EIN = 3584
    QD, KD, VD, ORD, UB = scr["QD"], scr["KD"], scr["VD"], scr["ORD"], scr["UB"]
    def stageA():
        with ExitStack() as es:
            sb, ps = phase_alloc(cx, es)
            tiles = []

            def SB(name, shape, dt):
                t = sb(name, shape, dt)
                tiles.append(t)
                return t

            def PS(name, shape, dt):
                t = ps(name, shape, dt)
                tiles.append(t)
                return t

            w_in = SB("w_in", [128, 8, EIN], BF16)
            ce = SB("ce", [128, CE32.shape[1]], F32)
            idt = SB("idt", [128, 128], BF16)
            g_bc = SB("g_bc", [128, D_MODEL], F32)
            xt = [SB("xt%d" % b, [128, D_MODEL], F32) for b in range(2)]
            rR = [SB("rR%d" % b, [128, 512], F32) for b in range(2)]
            rD = [SB("rD%d" % b, [128, 128], F32) for b in range(2)]
            junk = SB("junk", [128, D_MODEL], BF16)
            ssq_2 = [SB("ssq_%d" % i_, [128, 1], F32) for i_ in range(2)]
            rstd_2 = [SB("rstd_%d" % i_, [128, 1], F32) for i_ in range(2)]
            xn_2 = [SB("xn_%d" % i_, [128, D_MODEL], BF16) for i_ in range(2)]
            xnT_2 = [SB("xnT_%d" % i_, [128, 8, 128], BF16) for i_ in range(2)]
            ta_2 = [SB("ta_%d" % i_, [128, 256], F32) for i_ in range(2)]
            tb_2 = [SB("tb_%d" % i_, [128, 256], F32) for i_ in range(2)]
            qr_2 = [SB("qr_%d" % i_, [128, 512], F32) for i_ in range(2)]
            kr_2 = [SB("kr_%d" % i_, [128, 512], F32) for i_ in range(2)]
            qb_2 = [SB("qb_%d" % i_, [128, 3, 512], BF16) for i_ in range(2)]
            kdec_2 = [SB("kdec_%d" % i_, [128, 512], BF16) for i_ in range(2)]
            vb_2 = [SB("vb_%d" % i_, [128, 512], BF16) for i_ in range(2)]
            sg_2 = [SB("sg_%d" % i_, [128, 512], F32) for i_ in range(2)]
            qkT_2 = [SB("qkT_%d" % i_, [128, 3, 4, 128], BF16) for i_ in range(2)]
            sT_2 = [SB("sT_%d" % i_, [128, 4, 128], BF16) for i_ in range(2)]
            st = SB("st", [128, 4, 128], F32)
            sbf = [SB("sbf%d" % i, [128, 4, 128], BF16) for i in range(2)]
            bst_2 = [SB("bst_%d" % i_, [128, 4, 6], F32) for i_ in range(2)]
            mv_2 = [SB("mv_%d" % i_, [128, 4, 2], F32) for i_ in range(2)]
            hr_2 = [SB("hr_%d" % i_, [128, 4], F32) for i_ in range(2)]
            tmp_2 = [SB("tmp_%d" % i_, [128, 512], F32) for i_ in range(2)]
            orb_2 = [SB("orb_%d" % i_, [128, 512], BF16) for i_ in range(2)]
            dq_2 = [SB("dq_%d" % i_, [128, 512], BF16) for i_ in range(2)]
            dk_2 = [SB("dk_%d" % i_, [128, 512], BF16) for i_ in range(2)]
            va_2 = [SB("va_%d" % i_, [128, 8, 65], BF16) for i_ in range(2)]
            da_2 = [SB("da_%d" % i_, [128, 64], F32) for i_ in range(2)]
            db_2 = [SB("db_%d" % i_, [128, 64], F32) for i_ in range(2)]
            prq_2 = [SB("prq_%d" % i_, [128, 512], F32) for i_ in range(2)]
            prk_2 = [SB("prk_%d" % i_, [128, 512], F32) for i_ in range(2)]
            drq_2 = [SB("drq_%d" % i_, [128, 128], F32) for i_ in range(2)]
            drk_2 = [SB("drk_%d" % i_, [128, 128], F32) for i_ in range(2)]

            pT = PS("pT", [128, 8, 128], BF16)
            pP = [PS("pP%d" % i, [128, 512], F32) for i in range(3)]
            pT3 = [PS("pT3%d" % i, [128, 8, 128], BF16) for i in range(1)]
            pS = PS("pS", [128, 4, 128], F32)
            pKV = PS("pKV", [128, 4, 128], F32)
            pO = PS("pO", [128, 4, 128], F32)

            def cev(name):
                o, w = CE_OFF[name]
                return ce[:, o:o + w]

            P.dma("sp", idt[:], ident, [], [idt.r], ("id", 0))
            P.dma("sp", ce[:], ce_d, [], [ce.r], ("ce", 0))
            load_gain_bc(cx, g_bc, g_row)
            wv = w_in_d.rearrange("(kc p) f -> p kc f", p=128)
            for kh in range(2):
                for (c0, c1) in ((0, 1792), (1792, EIN)):
                    P.dma("pool", w_in[:, kh * 4:(kh + 1) * 4, c0:c1], wv[:, kh * 4:(kh + 1) * 4, c0:c1], [], [w_in.r],
                          ("w", "w_in"))
            P.op("dve", lambda e: e.memset(st[:], 0.0), [], [st.r])
            P.op("dve", lambda e: e.memset(sbf[0][:], 0.0), [], [sbf[0].r])
            for va in va_2:
                P.op("pool", lambda e, va=va: e.memset(va[:], 1.0), [], [va.r])
            scK = 128 ** -0.5

            def proj(gi, pp, xnT):
                for kc in range(8):
                    P.op("pe", lambda e, kc=kc, gi=gi, pp=pp, xnT=xnT: e.matmul(out=pp[:], lhsT=xnT[:, kc, :],
                                                                       rhs=w_in[:, kc, gi * 512:(gi + 1) * 512],
                                                                       start=(kc == 0), stop=(kc == 7)),
                         [xnT.r, w_in.r], [pp.r])

            def rot(pp, rt, nh, hd, half, dst, scale, scratch):
                pv = pp[:].rearrange("p (h d) -> p h d", h=nh)
                x1 = pv[:, :, 0:half]
                x2 = pv[:, :, half:2 * half]
                n = nh * half
                cosv = rt[:, 0:n].rearrange("p (h d) -> p h d", h=nh)
                sinv = rt[:, n:2 * n].rearrange("p (h d) -> p h d", h=nh)
                A, B = scratch
                Av = A[:, 0:n].rearrange("p (h d) -> p h d", h=nh)
                Bv = B[:, 0:n].rearrange("p (h d) -> p h d", h=nh)
                dv = dst.rearrange("p (h d) -> p h d", h=nh)
                for (u, w_, sgn, lo) in ((x1, x2, ALU.subtract, 0), (x2, x1, ALU.add, half)):
                    P.op("dve", lambda e, u=u: e.scalar_tensor_tensor(out=Av, in0=u, scalar=scale, in1=cosv, op0=ALU.mult,
                                                                      op1=ALU.mult), [pp.r, rt_res[0]], [A.r])
                    P.op("dve", lambda e, w_=w_: e.scalar_tensor_tensor(out=Bv, in0=w_, scalar=scale, in1=sinv, op0=ALU.mult,
                                                                        op1=ALU.mult), [pp.r, rt_res[0]], [B.r])
                    P.op("pool", lambda e, sgn=sgn, lo=lo: e.tensor_tensor(out=dv[:, :, lo:lo + half], in0=Av, in1=Bv, op=sgn),
                         [A.r, B.r], [dst_res[0]])

            rt_res = [None]
            dst_res = [None]
            def body(t):
                b = t % 2
                k_ = t % 2
                ssq = ssq_2[k_]
                rstd = rstd_2[k_]
                xn = xn_2[k_]
                xnT = xnT_2[k_]
                ta = ta_2[k_]
                tb = tb_2[k_]
                qr = qr_2[k_]
                kr = kr_2[k_]
                qb = qb_2[k_]
                kdec = kdec_2[k_]
                vb = vb_2[k_]
                sg = sg_2[k_]
                qkT = qkT_2[k_]
                sT = sT_2[k_]
                bst = bst_2[k_]
                mv = mv_2[k_]
                hr = hr_2[k_]
                tmp = tmp_2[k_]
                orb = orb_2[k_]
                dq = dq_2[k_]
                dk = dk_2[k_]
                va = va_2[k_]
                da = da_2[k_]
                db = db_2[k_]
                prq = prq_2[k_]
                prk = prk_2[k_]
                drq = drq_2[k_]
                drk = drk_2[k_]
                xr = cx.R("X%d" % t)
                P.dma("sp", xt[b][:], X[t * 128:(t + 1) * 128, :], [xr], [xt[b].r], ("xt", b))
                P.dma("sp", rR[b][:], ropeR_d[t * 128:(t + 1) * 128, :], [], [rR[b].r], ("rR", b))
                P.dma("sp", rD[b][:], ropeD_d[t * 128:(t + 1) * 128, :], [], [rD[b].r], ("rD", b))
                rms_prep(cx, xt[b], junk, ssq, rstd, xn, g_bc)
                for kc in range(8):
                    P.op("pe", lambda e, kc=kc: e.transpose(out=pT[:, kc, :], in_=xn[:, kc * 128:(kc + 1) * 128],
                                                            identity=idt[:]), [xn.r, idt.r], [pT.r])
                P.op("act", lambda e: e.activation(out=xnT[:], in_=pT[:], func=AF.Copy), [pT.r], [xnT.r])
                yield
                proj(0, pP[0], xnT)
                rt_res[0], dst_res[0] = rR[b].r, qr.r
                P.op("act", lambda e: e.activation(out=prq[:], in_=pP[0][:], func=AF.Copy), [pP[0].r], [prq.r])
                rot(prq, rR[b], 4, 128, 64, qr[:], 1.0, (ta, tb))
                P.op("act", lambda e: e.activation(out=qb[:, 0, :], in_=qr[:], func=AF.Copy), [qr.r], [qb.r])
                P.op("dve", lambda e: e.tensor_tensor(out=qb[:, 1, :], in0=qr[:], in1=cev("gq"), op=ALU.mult), [qr.r, ce.r], [qb.r])
                yield
                proj(1, pP[1], xnT)
                rt_res[0], dst_res[0] = rR[b].r, kr.r
                P.op("act", lambda e: e.activation(out=prk[:], in_=pP[1][:], func=AF.Copy), [pP[1].r], [prk.r])
                rot(prk, rR[b], 4, 128, 64, kr[:], scK, (ta, tb))
                P.op("act", lambda e: e.activation(out=qb[:, 2, :], in_=kr[:], func=AF.Copy), [kr.r], [qb.r])
                P.op("dve", lambda e: e.tensor_tensor(out=kdec[:], in0=kr[:], in1=cev("gk"), op=ALU.mult), [kr.r, ce.r], [kdec.r])
                yield
                proj(2, pP[2], xnT)
                P.op("act", lambda e: e.activation(out=vb[:], in_=pP[2][:], func=AF.Copy), [pP[2].r], [vb.r])
                proj(3, pP[0], xnT)
                P.op("act", lambda e: e.activation(out=sg[:], in_=pP[0][:], func=AF.Silu), [pP[0].r], [sg.r])
                yield
                for (gi, pp, dst, dram) in ((4, pP[1], dq, QD), (5, pP[2], dk, KD)):
                    if gi == 5:
                        yield
                    proj(gi, pp, xnT)
                    P.op("act", lambda e, pp=pp, dst=dst: e.activation(out=dst[:], in_=pp[:], func=AF.Copy), [pp.r], [dst.r])
                    rt_res[0], dst_res[0] = rD[b].r, dst.r
                    drw = drq if gi == 4 else drk
                    P.op("act", lambda e, pp=pp, drw=drw: e.activation(
                        out=drw[:].rearrange("p (h d) -> p h d", h=8),
                        in_=pp[:].rearrange("p (h d) -> p h d", h=8)[:, :, 0:16], func=AF.Copy), [pp.r], [drw.r])
                    rot(drw, rD[b], 8, 64, 8, dst[:], 1.0, (da, db))
                    P.dma("sp", dram[t * 128:(t + 1) * 128, :], dst[:], [dst.r], [cx.R("%s%d" % (dram.tensor.name, t))],
                          ("st", dst.r.name, k_))
                yield
                proj(6, pP[0], xnT)
                P.op("act", lambda e: e.activation(out=va[:, :, 0:64], in_=pP[0][:].rearrange("p (h d) -> p h d", h=8),
                                                   func=AF.Copy), [pP[0].r], [va.r])
                P.dma("sp", VD[t * 128:(t + 1) * 128, :], va[:].rearrange("p h d -> p (h d)"), [va.r], [cx.R("VD%d" % t)],
                      ("st", "va", k_))
                yield
                for j in range(3):
                    for h in range(4):
                        P.op("pe", lambda e, j=j, h=h: e.transpose(out=pT3[0][:, h, :], in_=qb[:, j, h * 128:(h + 1) * 128],
                                                                   identity=idt[:]), [qb.r, idt.r], [pT3[0].r])
                    P.op("act", lambda e, j=j: e.activation(out=qkT[:, j, :, :], in_=pT3[0][:, 0:4, :], func=AF.Copy), [pT3[0].r], [qkT.r])
                for h in range(4):
                    P.op("pe", lambda e, h=h: e.matmul(out=pS[:, h, :], lhsT=qkT[:, 2, h, :], rhs=qkT[:, 0, h, :], start=True,
                                                       stop=True), [qkT.r], [pS.r])
                P.op("dve", lambda e: e.tensor_tensor(out=sT[:].rearrange("p h i -> p (h i)"),
                                                      in0=pS[:].rearrange("p h i -> p (h i)"), in1=cev("dmT"), op=ALU.mult),
                     [pS.r, ce.r], [sT.r])
                yield
                sp, sn = sbf[t % 2], sbf[(t + 1) % 2]
                for h in range(4):
                    P.op("pe", lambda e, h=h: e.matmul(out=pKV[:, h, :], lhsT=kdec[:, h * 128:(h + 1) * 128],
                                                       rhs=vb[:, h * 128:(h + 1) * 128], start=True, stop=True),
                         [kdec.r, vb.r], [pKV.r])
                for h in range(4):
                    P.op("pe", lambda e, h=h: e.matmul(out=pO[:, h, :], lhsT=sT[:, h, :], rhs=vb[:, h * 128:(h + 1) * 128],
                                                       start=True, stop=False), [sT.r, vb.r], [pO.r])
                    P.op("pe", lambda e, h=h, sp=sp: e.matmul(out=pO[:, h, :], lhsT=qkT[:, 1, h, :], rhs=sp[:, h, :],
                                                              start=False, stop=True), [qkT.r, sp.r], [pO.r])
                for h in range(4):
                    P.op("dve", lambda e, h=h: e.scalar_tensor_tensor(out=st[:, h, :], in0=st[:, h, :], scalar=RET_G[h] ** 128,
                                                                      in1=pKV[:, h, :], op0=ALU.mult, op1=ALU.add),
                         [st.r, pKV.r], [st.r])
                P.op("pool", lambda e, sn=sn: e.tensor_copy(out=sn[:], in_=st[:]), [st.r], [sn.r])
                for h in range(4):
                    P.op("dve", lambda e, h=h: e.bn_stats(out=bst[:, h, :], in_=pO[:, h, :]), [pO.r], [bst.r])
                    P.op("dve", lambda e, h=h: e.bn_aggr(out=mv[:, h, :], in_=bst[:, h, :]), [bst.r], [mv.r])
                P.op("act", lambda e: e.activation(out=hr[:], in_=mv[:, :, 1], func=AF.Sqrt, bias=EPS), [mv.r], [hr.r])
                P.op("dve", lambda e: e.reciprocal(out=hr[:], in_=hr[:]), [hr.r], [hr.r])
                for h in range(4):
                    P.op("dve", lambda e, h=h: e.tensor_scalar(out=tmp[:, h * 128:(h + 1) * 128], in0=pO[:, h, :],
                                                               scalar1=mv[:, h, 0:1], scalar2=hr[:, h:h + 1],
                                                               op0=ALU.subtract, op1=ALU.mult), [pO.r, mv.r, hr.r], [tmp.r])
                P.op("pool", lambda e: e.tensor_tensor(out=orb[:], in0=tmp[:], in1=sg[:], op=ALU.mult), [tmp.r, sg.r], [orb.r])
                P.dma("sp", ORD[t * 128:(t + 1) * 128, :], orb[:], [orb.r], [cx.R("ORD%d" % t)], ("st", "orb", k_))
            run_interleaved([body(t) for t in range(NT)], lead=5)
            barrier(cx, tiles)

    import os
    _st = os.environ.get('EVEN_STAGES', 'ABC')
    if 'A' in _st:
        stageA()
    def stageB():
        with ExitStack() as es:
            sb, ps = phase_alloc(cx, es)
            tiles = []

            def SB(name, shape, dt):
                t = sb(name, shape, dt)
                tiles.append(t)
                return t

            def PS(name, shape, dt):
                t = ps(name, shape, dt)
                tiles.append(t)
                return t

            ce = SB("ce", [128, CE32.shape[1]], F32)
            idt = SB("idt", [128, 128], BF16)
            mk = SB("mk", [128, 512], BF16)
            qt_ = [SB("qt%d" % i, [128, 512], BF16) for i in range(2)]
            kt_ = [SB("kt%d" % i, [128, 512], BF16) for i in range(2)]
            vt_ = [SB("vt%d" % i, [128, 8, 65], BF16) for i in range(3)]
            qT_2 = [SB("qT_%d" % i_, [128, 4, 128], BF16) for i_ in range(2)]
            kT = [SB("kT%d" % i, [128, 4, 128], BF16) for i in range(3)]
            pe_ = [SB("pe%d" % i, [128, 512], BF16) for i in range(4)]
            pm = [SB("pm%d" % i, [128, 4, 128], BF16) for i in range(4)]
            us = [SB("us%d" % i, [128, 520], F32) for i in range(2)]
            pTq = PS("pTq", [128, 8, 128], BF16)
            pTk = PS("pTk", [128, 8, 128], BF16)
            pS = [PS("pS%d" % i, [128, 4, 128], F32) for i in range(4)]
            pU = [PS("pU%d" % i, [128, 512], F32) for i in range(2)]

            P.dma("sp", idt[:], ident, [], [idt.r], ("id", 0))
            P.dma("sp", ce[:], ce_d, [], [ce.r], ("ce", 0))
            o_, w_ = CE_OFF["dilm"]
            P.op("act", lambda e: e.activation(out=mk[:], in_=ce[:, o_:o_ + w_], func=AF.Copy), [ce.r], [mk.r])
            mk4 = mk[:].rearrange("p (a c i) -> p a c i", a=2, c=2)
            sc = 64 ** -0.5
            def body(bi, r, rho, n, cnt):
                Qv = QD.rearrange("(l r) f -> r l f", r=r)
                Kv = KD.rearrange("(l r) f -> r l f", r=r)
                Vv = VD.rearrange("(l r) f -> r l f", r=r)
                Uv = UB[bi].rearrange("(l r) f -> r l f", r=r)
                i2 = cnt % 2
                i3 = cnt % 3
                ip = (cnt - 1) % 3
                qT = qT_2[i2]
                rows = slice(n * 128, (n + 1) * 128)
                src_q = [cx.R("%s%d" % (QD.tensor.name, t)) for t in range(NT)]
                src_k = [cx.R("%s%d" % (KD.tensor.name, t)) for t in range(NT)]
                src_v = [cx.R("VD%d" % t) for t in range(NT)]
                P.dma("sp", qt_[i2][:], Qv[rho, rows, :], src_q, [qt_[i2].r], ("ld", "q", i2))
                P.dma("sp", kt_[i2][:], Kv[rho, rows, :], src_k, [kt_[i2].r], ("ld", "k", i2))
                P.dma("sp", vt_[i3][:].rearrange("p h d -> p (h d)"), Vv[rho, rows, :], src_v, [vt_[i3].r], ("ld", "v", i3))
                for hp in range(4):
                    P.op("pe", lambda e, hp=hp, i2=i2: e.transpose(out=pTq[:, hp, :], in_=qt_[i2][:, hp * 128:(hp + 1) * 128],
                                                                   identity=idt[:]), [qt_[i2].r, idt.r], [pTq.r])
                P.op("act", lambda e: e.activation(out=qT[:], in_=pTq[:, 0:4, :], func=AF.Copy), [pTq.r], [qT.r])
                for hp in range(4):
                    P.op("pe", lambda e, hp=hp, i2=i2: e.transpose(out=pTk[:, hp, :], in_=kt_[i2][:, hp * 128:(hp + 1) * 128],
                                                                   identity=idt[:]), [kt_[i2].r, idt.r], [pTk.r])
                P.op("dve", lambda e, i3=i3: e.tensor_copy(out=kT[i3][:], in_=pTk[:, 0:4, :]), [pTk.r], [kT[i3].r])
                yield
                for hq in range(2):
                    psb = [pS[hq * 2 + 0], pS[hq * 2 + 1]]
                    peb = [pe_[hq * 2 + 0], pe_[hq * 2 + 1]]
                    pmb = [pm[hq * 2 + 0], pm[hq * 2 + 1]]
                    for hpi in range(2):
                        hp = hq * 2 + hpi
                        for hh in range(2):
                            prt = slice(hh * 64, (hh + 1) * 64)
                            ps_ = psb[hh]
                            if n > 0:
                                P.op("pe", lambda e, hp=hp, hpi=hpi, prt=prt, ps_=ps_, ip=ip: e.matmul(
                                    out=ps_[:, hpi * 2 + 0, :], lhsT=kT[ip][prt, hp, :], rhs=qT[prt, hp, :], start=True,
                                    stop=True), [kT[ip].r, qT.r], [ps_.r])
                            P.op("pe", lambda e, hp=hp, hpi=hpi, prt=prt, ps_=ps_, i3=i3: e.matmul(
                                out=ps_[:, hpi * 2 + 1, :], lhsT=kT[i3][prt, hp, :], rhs=qT[prt, hp, :], start=True,
                                stop=True), [kT[i3].r, qT.r], [ps_.r])
                    for hh in range(2):
                        ps_, pe__, pm_ = psb[hh], peb[hh], pmb[hh]
                        pe4 = pe__[:].rearrange("p (a c i) -> p a c i", a=2, c=2)
                        ps4 = ps_[:].rearrange("p (a c) i -> p a c i", a=2)
                        pm4 = pm_[:].rearrange("p (a c) i -> p a c i", a=2)
                        if n > 0:
                            P.op("act", lambda e, ps_=ps_, pe__=pe__: e.activation(
                                out=pe__[:], in_=ps_[:].rearrange("p a i -> p (a i)"), func=AF.Exp, scale=sc),
                                [ps_.r], [pe__.r])
                            P.op("pool", lambda e, pe__=pe__, pm_=pm_: e.tensor_tensor(
                                out=pm_[:].rearrange("p a i -> p (a i)"), in0=pe__[:], in1=mk[:], op=ALU.mult),
                                [pe__.r, mk.r], [pm_.r])
                        else:
                            P.op("act", lambda e, pe4=pe4, ps4=ps4: e.activation(
                                out=pe4[:, :, 1, :], in_=ps4[:, :, 1, :], func=AF.Exp, scale=sc), [ps_.r], [pe__.r])
                            P.op("pool", lambda e, pe4=pe4, pm4=pm4: e.tensor_tensor(
                                out=pm4[:, :, 1, :], in0=pe4[:, :, 1, :], in1=mk4[:, :, 1, :], op=ALU.mult),
                                [pe__.r, mk.r], [pm_.r])
                    for hpi in range(2):
                        hp = hq * 2 + hpi
                        for hh in range(2):
                            h = hp * 2 + hh
                            pu = pU[h // 4]
                            pm_ = pmb[hh]
                            usl = slice((h % 4) * 65, (h % 4 + 1) * 65)
                            if n > 0:
                                P.op("pe", lambda e, h=h, hpi=hpi, pu=pu, pm_=pm_, ip=ip, usl=usl: e.matmul(
                                    out=pu[:, usl], lhsT=pm_[:, hpi * 2 + 0, :], rhs=vt_[ip][:, h, :], start=True,
                                    stop=False), [pm_.r, vt_[ip].r], [pu.r])
                            P.op("pe", lambda e, h=h, hpi=hpi, pu=pu, pm_=pm_, i3=i3, n=n, usl=usl: e.matmul(
                                out=pu[:, usl], lhsT=pm_[:, hpi * 2 + 1, :], rhs=vt_[i3][:, h, :], start=(n == 0),
                                stop=True), [pm_.r, vt_[i3].r], [pu.r])
                    P.op("act", lambda e, hq=hq, i2=i2: e.activation(out=us[i2][:, hq * 260:(hq + 1) * 260],
                                                                     in_=pU[hq][:, 0:260], func=AF.Copy),
                         [pU[hq].r], [us[i2].r])
                    yield
                dst_res = []
                for tt in range(NT):
                    dst_res.append(cx.R("UB%d_%d" % (bi, tt)))
                P.dma("sp", Uv[rho, rows, :], us[i2][:], [us[i2].r], dst_res, ("st", "us", i2))
            bodies = []
            cnt = 0
            for bi, (window, r) in enumerate(DIL_PATTERNS):
                nb = (S // r) // 128
                for rho in range(r):
                    for n in range(nb):
                        bodies.append(body(bi, r, rho, n, cnt))
                        cnt += 1
            run_interleaved(bodies, lead=2)
            barrier(cx, tiles)

    if 'B' in _st:
        stageB()
    def stageC():
        with ExitStack() as es:
            sb, ps = phase_alloc(cx, es)
            tiles = []

            def SB(name, shape, dt):
                t = sb(name, shape, dt)
                tiles.append(t)
                return t

            def PS(name, shape, dt):
                t = ps(name, shape, dt)
                tiles.append(t)
                return t

            w_out = SB("w_out", [128, 8, D_MODEL], BF16)
            idt = SB("idt", [128, 128], BF16)
            xt = [SB("xt%d" % b, [128, D_MODEL], F32) for b in range(2)]
            u = [[SB("u%d%d" % (b, i), [128, 8, 65], F32) for i in range(3)] for b in range(2)]
            cat = [SB("cat%d" % b, [128, 1024], BF16) for b in range(2)]
            rc_2 = [SB("rc_%d" % i_, [128, 8], F32) for i_ in range(2)]
            catT_2 = [SB("catT_%d" % i_, [128, 8, 128], BF16) for i_ in range(2)]
            xo = [SB("xo%d" % b, [128, D_MODEL], F32) for b in range(2)]
            pT = PS("pT", [128, 8, 128], BF16)
            pY = [PS("pY%d" % i, [128, 512], F32) for i in range(2)]
            P.dma("sp", idt[:], ident, [], [idt.r], ("id", 0))
            wov = w_out_d.rearrange("(kc p) f -> p kc f", p=128)
            for kh in range(2):
                P.dma("pool", w_out[:, kh * 4:(kh + 1) * 4, :], wov[:, kh * 4:(kh + 1) * 4, :], [], [w_out.r], ("w", "w_out"))
            def body(t):
                b = t % 2
                rc = rc_2[b]
                catT = catT_2[b]
                xr = cx.R("X%d" % t)
                rows = slice(t * 128, (t + 1) * 128)
                P.dma("sp", xt[b][:], X[rows, :], [xr], [xt[b].r], ("xt", b))
                P.dma("sp", cat[b][:, 0:512], ORD[rows, :], [cx.R("ORD%d" % t)], [cat[b].r], ("ld", "cat", b))
                for i in range(3):
                    P.dma("sp", u[b][i][:].rearrange("p h d -> p (h d)"), UB[i][rows, :], [cx.R("UB%d_%d" % (i, t))], [u[b][i].r],
                          ("ld", "u", b, i))
                P.op("dve", lambda e, b=b: e.tensor_tensor(out=u[b][0][:], in0=u[b][0][:], in1=u[b][1][:], op=ALU.add),
                     [u[b][0].r, u[b][1].r], [u[b][0].r])
                P.op("dve", lambda e, b=b: e.tensor_tensor(out=u[b][0][:], in0=u[b][0][:], in1=u[b][2][:], op=ALU.add),
                     [u[b][0].r, u[b][2].r], [u[b][0].r])
                P.op("dve", lambda e, b=b: e.reciprocal(out=rc[:], in_=u[b][0][:, :, 64]), [u[b][0].r], [rc.r])
                for h in range(8):
                    P.op("dve", lambda e, b=b, h=h: e.tensor_scalar(out=cat[b][:, 512 + h * 64:512 + (h + 1) * 64],
                                                                    in0=u[b][0][:, h, 0:64], scalar1=rc[:, h:h + 1], scalar2=None,
                                                                    op0=ALU.mult), [u[b][0].r, rc.r], [cat[b].r])
                yield
                for ec in range(8):
                    P.op("pe", lambda e, ec=ec, b=b: e.transpose(out=pT[:, ec, :], in_=cat[b][:, ec * 128:(ec + 1) * 128],
                                                                 identity=idt[:]), [cat[b].r, idt.r], [pT.r])
                P.op("act", lambda e: e.activation(out=catT[:], in_=pT[:], func=AF.Copy), [pT.r], [catT.r])
                for hf in range(2):
                    for ec in range(8):
                        P.op("pe", lambda e, hf=hf, ec=ec: e.matmul(out=pY[hf][:], lhsT=catT[:, ec, :],
                                                                   rhs=w_out[:, ec, hf * 512:(hf + 1) * 512],
                                                                   start=(ec == 0), stop=(ec == 7)), [catT.r, w_out.r], [pY[hf].r])
                    P.op("dve", lambda e, hf=hf, b=b: e.tensor_tensor(out=xo[b][:, hf * 512:(hf + 1) * 512], in0=pY[hf][:],
                                                                      in1=xt[b][:, hf * 512:(hf + 1) * 512], op=ALU.add),
                         [pY[hf].r, xt[b].r], [xo[b].r])
                P.dma("sp", X[rows, :], xo[b][:], [xo[b].r], [xr], ("xo", b))
            run_interleaved([body(t) for t in range(NT)], lead=1)
            barrier(cx, tiles)

    if 'C' in _st:
        stageC()

def extra_inputs(S=4096):
    ce, off, ropeR, ropeD = make_consts_even(S)
    return {"ident": np.eye(128).astype(ml_dtypes.bfloat16), "cf32": CF32, "ce32": ce, "ropeR": ropeR, "ropeD": ropeD}


ALL_NAMES = list(INPUT_SHAPES.keys())


def kernel(**inputs):
    S = 4096
    phases = []
    for l in range(DEPTH):
        phases.append(("ffn", l, "pre"))
        phases.append(("mixA", l // 2) if l % 2 == 0 else ("gla", l // 2))
        phases.append(("ffn", l, "post"))
    phases.append(("final",))
    nc = build_program(S, phases, ALL_NAMES)
    x = np.ascontiguousarray(np.asarray(inputs["x"], dtype=np.float32))
    B = x.shape[0]
    shared = {n: np.ascontiguousarray(np.asarray(inputs[n], dtype=np.float32)) for n in ALL_NAMES}
    shared.update(extra_inputs(S))
    in_maps = []
    for b in range(B):
        m = {"x": x[b]}
        m.update(shared)
        in_maps.append(m)
    res = run_bass_kernel_spmd(nc, in_maps, core_ids=list(range(B)))
    return np.stack([np.asarray(r["out"], dtype=np.float32) for r in res.results], axis=0)
```

```python
import math
from contextlib import ExitStack

import numpy as np
import ml_dtypes
import concourse.bass as bass
import concourse.mybir as mybir
from concourse.bass_utils import run_bass_kernel_spmd

F32 = mybir.dt.float32
BF16 = mybir.dt.bfloat16
AF = mybir.ActivationFunctionType
ALU = mybir.AluOpType
AX = mybir.AxisListType

D_MODEL = 1024
FFN_HIDDEN = 2816
EPS = 1e-6
DEPTH = 4


class Res:
    __slots__ = ("name", "last_w", "readers", "dma_readers", "excl")

    def __init__(self, name, excl=False):
        self.name = name
        self.excl = excl
        self.last_w = None
        self.readers = {}
        self.dma_readers = []


class Op:
    __slots__ = ("eng", "fn", "deps", "sig", "val", "is_dma", "chan", "sem")

    def __init__(self, eng, fn, is_dma=False, chan=None):
        self.eng = eng
        self.fn = fn
        self.deps = []
        self.sig = False
        self.val = None
        self.is_dma = is_dma
        self.chan = chan
        self.sem = None


class Prog:
    ENGS = ("pe", "act", "dve", "pool", "sp")

    def __init__(self, nc):
        self.nc = nc
        self.ops = {e: [] for e in self.ENGS}
        self.chan_count = {}
        self.n_ops = 0

    def _add_dep(self, op, d, kind):
        if d is None or d is op:
            return
        if not d.is_dma and not op.is_dma and d.eng == op.eng:
            if op.eng == "pe":
                return
        if d.is_dma and op.is_dma and d.eng == op.eng and kind == "WAR":
            pass
        d.sig = True
        op.deps.append(d)

    def op(self, eng, fn, reads=(), writes=(), is_dma=False, chan=None):
        o = Op(eng, fn, is_dma, chan)
        writes = list(writes) + [r for r in reads if r.excl and r not in writes]
        reads = [r for r in reads if not r.excl]
        for r in reads:
            self._add_dep(o, r.last_w, "RAW")
        for w in writes:
            self._add_dep(o, w.last_w, "WAW")
            for rd in w.readers.values():
                self._add_dep(o, rd, "WAR")
            for rd in w.dma_readers:
                self._add_dep(o, rd, "WAR")
        for r in reads:
            if is_dma:
                r.dma_readers.append(o)
            else:
                r.readers[eng] = o
        for w in writes:
            w.last_w = o
            w.readers = {}
            w.dma_readers = []
        if is_dma:
            c = self.chan_count.get(chan, 0) + 1
            self.chan_count[chan] = c
            o.val = 16 * c
        self.ops[eng].append(o)
        self.n_ops += 1
        return o

    def wait_all(self, eng, resources):
        o = Op(eng, None)
        for w in resources:
            for d in [w.last_w] + list(w.readers.values()) + list(w.dma_readers):
                if d is None or (not d.is_dma and d.eng == eng and eng == "pe"):
                    continue
                d.sig = True
                o.deps.append(d)
        self.ops[eng].append(o)
        self.n_ops += 1
        return o

    def dma(self, queue, out_ap, in_ap, reads, writes, chan):
        def fn(e, out_ap=out_ap, in_ap=in_ap):
            return e.dma_start(out=out_ap, in_=in_ap)

        return self.op(queue, fn, reads, writes, is_dma=True, chan=chan)

    def emit(self):
        nc = self.nc
        with ExitStack() as es:
            eng_sem = {e: es.enter_context(nc.semaphore("s_" + e)) for e in self.ENGS}
            chan_sem = {}
            for c in self.chan_count:
                chan_sem[c] = es.enter_context(nc.semaphore("c_%d" % len(chan_sem)))
            for e in self.ENGS:
                cnt = 0
                for o in self.ops[e]:
                    if o.is_dma:
                        o.sem = chan_sem[o.chan]
                    else:
                        o.sem = eng_sem[e]
                        if o.sig:
                            cnt += 1
                            o.val = cnt
            block = es.enter_context(nc.Block())

            def emit_eng(eng_name, e):
                waited = {}
                for o in self.ops[eng_name]:
                    need = {}
                    for d in o.deps:
                        k = id(d.sem)
                        if waited.get(k, 0) >= d.val:
                            continue
                        if k not in need or need[k][1] < d.val:
                            need[k] = (d.sem, d.val)
                    for k, (sem, val) in need.items():
                        e.wait_ge(sem, val)
                        waited[k] = val
                    if o.fn is None:
                        assert not o.sig
                    else:
                        ins = o.fn(e)
                        if o.is_dma:
                            ins.then_inc(o.sem, 16)
                        elif o.sig:
                            ins.then_inc(o.sem, 1)

            @block.tensor
            def _(e):
                emit_eng("pe", e)

            @block.scalar
            def _(e):
                emit_eng("act", e)

            @block.vector
            def _(e):
                emit_eng("dve", e)

            @block.gpsimd
            def _(e):
                emit_eng("pool", e)

            @block.sync
            def _(e):
                emit_eng("sp", e)


class Ctx:
    def __init__(self, nc, S):
        self.nc = nc
        self.S = S
        self.P = Prog(nc)
        self.NT = S // 128
        self.res_cache = {}
        self.dbg32 = None
        self.dbg16 = None
        self.dbg_off = {False: 0, True: 0}
        self.dbg_map = {}

    def dbg(self, tile, ap2d, name):
        if self.dbg32 is None:
            return
        is16 = ap2d.dtype == BF16
        d = self.dbg16 if is16 else self.dbg32
        off = self.dbg_off[is16]
        p, n = ap2d.shape
        self.dbg_map[name] = (is16, off, p, n)
        self.dbg_off[is16] = off + n
        self.P.dma("sp", d[0:p, off:off + n], ap2d, [tile.r], [self.R("dbgout")], ("dbg", 0))

    def R(self, name):
        r = self.res_cache.get(name)
        if r is None:
            r = Res(name)
            self.res_cache[name] = r
        return r


class Tile:
    def __init__(self, t, name, excl=False):
        self.t = t
        self.r = Res(name, excl)

    def __getitem__(self, k):
        return self.t[k]


def phase_alloc(cx, es):
    nc = cx.nc
    cnt = [0]

    def sb(name, shape, dt):
        cnt[0] += 1
        return Tile(es.enter_context(nc.sbuf_tensor("%s_%d" % (name, cx.P.n_ops), shape, dt)), name)

    def ps(name, shape, dt):
        cnt[0] += 1
        return Tile(es.enter_context(nc.psum_tensor("%s_%d" % (name, cx.P.n_ops), shape, dt)), name, excl=True)

    return sb, ps


def barrier(cx, tiles):
    for eng in ("pe", "act", "dve", "pool", "sp"):
        cx.P.wait_all(eng, [t.r for t in tiles])


def load_gain_bc(cx, g_bc, g_row_ap):
    cx.P.dma("sp", g_bc[:], g_row_ap.partition_broadcast(128), [], [g_bc.r], ("g", g_bc.r.name))


def rms_prep(cx, xt, junk, ssq, rstd, xn, g_bc):
    P = cx.P
    P.op("act", lambda e: e.activation(out=junk[:], in_=xt[:], func=AF.Square, accum_out=ssq[:]),
         [xt.r], [junk.r, ssq.r])
    P.op("act", lambda e: e.activation(out=rstd[:], in_=ssq[:], func=AF.Sqrt, scale=1.0 / D_MODEL, bias=EPS),
         [ssq.r], [rstd.r])
    P.op("dve", lambda e: e.reciprocal(out=rstd[:], in_=rstd[:]), [rstd.r], [rstd.r])
    P.op("dve", lambda e: e.scalar_tensor_tensor(out=xn[:], in0=xt[:], scalar=rstd[:], in1=g_bc[:],
                                                 op0=ALU.mult, op1=ALU.mult),
         [xt.r, rstd.r, g_bc.r], [xn.r])


def phase_ffn(cx, X, g_row, wg_d, wu_d, wd_d, ident, Xin=None):
    nc, P, S = cx.nc, cx.P, cx.S
    if Xin is None:
        Xin = X
    TT = 256
    nt = S // TT
    NFC = FFN_HIDDEN // 128
    with ExitStack() as es:
        sb, ps = phase_alloc(cx, es)
        wg = sb("wg", [128, 8, FFN_HIDDEN], BF16)
        wu = sb("wu", [128, 8, FFN_HIDDEN], BF16)
        wd = sb("wd", [128, NFC, D_MODEL], BF16)
        g_bc = sb("g_bc", [128, D_MODEL], F32)
        xt = [[sb("xt%d%d" % (b, a), [128, D_MODEL], F32) for a in range(2)] for b in range(2)]
        junk = sb("junk", [128, D_MODEL], BF16)
        ssq = [[sb("ssq%d%d" % (b, a), [128, 1], F32) for a in range(2)] for b in range(2)]
        rstd = [[sb("rstd%d%d" % (b, a), [128, 1], F32) for a in range(2)] for b in range(2)]
        xn = [sb("xn%d" % a, [128, D_MODEL], BF16) for a in range(2)]
        xnT = [sb("xnT%d" % b, [128, 8, TT], BF16) for b in range(2)]
        sg = [sb("sg%d" % i, [128, TT], F32) for i in range(2)]
        hT = [sb("hT%d" % i, [128, TT], BF16) for i in range(3)]
        xo = [sb("xo%d" % a, [128, D_MODEL], F32) for a in range(2)]
        pT = [ps("pT%d" % a, [128, 8, 128], BF16) for a in range(2)]
        pGU = [ps("pGU%d" % i, [128, 512], F32) for i in range(2)]
        pO = [[ps("pO%d%d" % (a, h), [128, 512], F32) for h in range(2)] for a in range(2)]
        idt = sb("idt", [128, 128], BF16)
        tiles_extra = []
        all_tiles = ([wg, wu, wd, g_bc, junk, idt] + sum(xt, []) + sum(ssq, []) + sum(rstd, []) + xn + xnT + sg
                     + hT + xo + pT + pGU + sum(pO, []))

        P.dma("sp", idt[:], ident, [], [idt.r], ("id", 0))
        load_gain_bc(cx, g_bc, g_row)
        wgv = wg_d.rearrange("(kc p) f -> p kc f", p=128)
        wuv = wu_d.rearrange("(kc p) f -> p kc f", p=128)
        wdv = wd_d.rearrange("(fc p) d -> p fc d", p=128)
        FH = FFN_HIDDEN // 2
        wres = {}
        for fh in range(2):
            for (dst, src, nm) in ((wg, wgv, "wg"), (wu, wuv, "wu")):
                for kh in range(2):
                    r_ = Res("%s_%d_%d" % (nm, fh, kh))
                    wres[(nm, fh, kh)] = r_
                    tiles_extra.append(r_)
                    P.dma("pool", dst[:, kh * 4:(kh + 1) * 4, fh * FH:(fh + 1) * FH],
                          src[:, kh * 4:(kh + 1) * 4, fh * FH:(fh + 1) * FH], [], [r_], ("w", nm, fh, kh))
            r_ = Res("wd_%d" % fh)
            wres[("wd", fh)] = r_
            tiles_extra.append(r_)
            P.dma("pool", wd[:, fh * 11:(fh + 1) * 11, :], wdv[:, fh * 11:(fh + 1) * 11, :], [], [r_], ("w", "wd", fh))

        def xres(t, a):
            return cx.R("X%d" % (t * 2 + a))

        def prep_load(t):
            b = t % 2
            for a in range(2):
                r0 = t * TT + a * 128
                P.dma("sp", xt[b][a][:], Xin[r0:r0 + 128, :], [xres(t, a)], [xt[b][a].r], ("xt", b, a))

        def prep_norm(t):
            b = t % 2
            for a in range(2):
                rms_prep(cx, xt[b][a], junk, ssq[b][a], rstd[b][a], xn[a], g_bc)

        def prep_T(t):
            b = t % 2
            for a in range(2):
                for kc in range(8):
                    P.op("pe", lambda e, a=a, kc=kc: e.transpose(out=pT[a][:, kc, :],
                                                                 in_=xn[a][:, kc * 128:(kc + 1) * 128],
                                                                 identity=idt[:]),
                         [xn[a].r, idt.r], [pT[a].r])
                P.op("act", lambda e, a=a, b=b: e.activation(out=xnT[b][:, :, a * 128:(a + 1) * 128], in_=pT[a][:],
                                                             func=AF.Copy),
                     [pT[a].r], [xnT[b].r])

        def gu(t, fc):
            b = t % 2
            p = pGU[fc % 2]
            for (w_, off, nm) in ((wg, 0, "wg"), (wu, TT, "wu")):
                for kc in range(8):
                    P.op("pe", lambda e, w_=w_, off=off, kc=kc, p=p, b=b, fc=fc: e.matmul(
                        out=p[:, off:off + TT], lhsT=w_[:, kc, fc * 128:(fc + 1) * 128], rhs=xnT[b][:, kc, :],
                        start=(kc == 0), stop=(kc == 7)), [wres[(nm, fc // 11, kc // 4)], xnT[b].r], [p.r])

        def actmul(t, fc):
            p = pGU[fc % 2]
            s_ = sg[fc % 2]
            h_ = hT[fc % 3]
            P.op("act", lambda e, p=p, s_=s_: e.activation(out=s_[:], in_=p[:, 0:TT], func=AF.Silu), [p.r], [s_.r])
            P.op("dve", lambda e, p=p, s_=s_, h_=h_: e.tensor_tensor(out=h_[:], in0=s_[:], in1=p[:, TT:2 * TT],
                                                                    op=ALU.mult), [s_.r, p.r], [h_.r])

        def down(t, fc):
            h_ = hT[fc % 3]
            for a in range(2):
                for hf in range(2):
                    P.op("pe", lambda e, a=a, hf=hf, h_=h_, fc=fc: e.matmul(
                        out=pO[a][hf][:], lhsT=h_[:, a * 128:(a + 1) * 128], rhs=wd[:, fc, hf * 512:(hf + 1) * 512],
                        start=(fc == 0), stop=(fc == NFC - 1)), [h_.r, wres[("wd", fc // 11)]], [pO[a][hf].r])

        def finish(t):
            b = t % 2
            for a in range(2):
                for hf in range(2):
                    P.op("dve", lambda e, a=a, hf=hf, b=b: e.scalar_tensor_tensor(
                        out=xo[a][:, hf * 512:(hf + 1) * 512], in0=pO[a][hf][:], scalar=0.5,
                        in1=xt[b][a][:, hf * 512:(hf + 1) * 512], op0=ALU.mult, op1=ALU.add),
                        [pO[a][hf].r, xt[b][a].r], [xo[a].r])
                r0 = t * TT + a * 128
                P.dma("sp", X[r0:r0 + 128, :], xo[a][:], [xo[a].r], [xres(t, a)], ("xo", a))

        prep_load(0)
        prep_norm(0)
        prep_T(0)
        for t in range(nt):
            for fc in range(NFC):
                gu(t, fc)
                if t + 1 < nt:
                    if fc == 0:
                        prep_load(t + 1)
                    if fc == 5:
                        prep_norm(t + 1)
                    if fc == 13:
                        prep_T(t + 1)
                actmul(t, fc)
                if fc >= 1:
                    down(t, fc - 1)
            down(t, NFC - 1)
            finish(t)
        for eng in ("pe", "act", "dve", "pool", "sp"):
            cx.P.wait_all(eng, [t_.r for t_ in all_tiles] + tiles_extra)


def phase_final(cx, X, g_row, OUT):
    nc, P, S = cx.nc, cx.P, cx.S
    with ExitStack() as es:
        sb, ps = phase_alloc(cx, es)
        g_bc = sb("g_bc", [128, D_MODEL], F32)
        xt = [sb("xt%d" % b, [128, D_MODEL], F32) for b in range(2)]
        junk = sb("junk", [128, D_MODEL], BF16)
        ssq = [sb("ssq%d" % b, [128, 1], F32) for b in range(2)]
        rstd = [sb("rstd%d" % b, [128, 1], F32) for b in range(2)]
        xo = [sb("xo%d" % b, [128, D_MODEL], F32) for b in range(2)]
        all_tiles = [g_bc, junk] + xt + ssq + rstd + xo
        load_gain_bc(cx, g_bc, g_row)
        outs = []
        for t in range(cx.NT):
            b = t % 2
            P.dma("sp", xt[b][:], X[t * 128:(t + 1) * 128, :], [cx.R("X%d" % t)], [xt[b].r], ("xt", b))
            rms_prep(cx, xt[b], junk, ssq[b], rstd[b], xo[b], g_bc)
            ro = cx.R("OUT%d" % t)
            outs.append(ro)
            P.dma("sp", OUT[t * 128:(t + 1) * 128, :], xo[b][:], [xo[b].r], [ro], ("xo", b))
        P.op("sp", None, outs, [])
        barrier(cx, all_tiles)


INPUT_SHAPES = {
    "ffn_pre_norm": [4, 1024], "ffn_pre_w_gate": [4, 1024, 2816], "ffn_pre_w_up": [4, 1024, 2816],
    "ffn_pre_w_down": [4, 2816, 1024], "mix_norm": [4, 1024], "ab_w_in": [2, 1024, 3584],
    "ab_w_out": [2, 1024, 1024], "gla_w_in": [2, 1024, 3088], "gla_w_a2": [2, 16, 512], "gla_b_a": [2, 512],
    "gla_w_out": [2, 1024, 1024], "ffn_post_norm": [4, 1024], "ffn_post_w_gate": [4, 1024, 2816],
    "ffn_post_w_up": [4, 1024, 2816], "ffn_post_w_down": [4, 2816, 1024], "final_norm": [1024],
}


def build_program(S, phases, names):
    return _build_program(S, phases, names)[0]


def _build_program(S, phases, names):
    nc = bass.Bass("TRN2", target_bir_lowering=False)
    x_in = nc.dram_tensor("x", [S, D_MODEL], F32, kind="ExternalInput").ap()
    ident = nc.dram_tensor("ident", [128, 128], BF16, kind="ExternalInput").ap()
    cf_d = nc.dram_tensor("cf32", list(CF32.shape), F32, kind="ExternalInput").ap()
    ce_d = nc.dram_tensor("ce32", list(CE32.shape), F32, kind="ExternalInput").ap()
    ropeR_d = nc.dram_tensor("ropeR", [S, 512], F32, kind="ExternalInput").ap()
    ropeD_d = nc.dram_tensor("ropeD", [S, 128], F32, kind="ExternalInput").ap()
    scr = {
        "QD": nc.dram_tensor("qd", [S, 512], BF16, kind="Internal").ap(),
        "KD": nc.dram_tensor("kd", [S, 512], BF16, kind="Internal").ap(),
        "VD": nc.dram_tensor("vd", [S, 520], BF16, kind="Internal").ap(),
        "ORD": nc.dram_tensor("ord", [S, 512], BF16, kind="Internal").ap(),
        "UB": [nc.dram_tensor("ub%d" % i, [S, 520], F32, kind="Internal").ap() for i in range(3)],
    }
    W = {n: nc.dram_tensor(n, INPUT_SHAPES[n], F32, kind="ExternalInput").ap() for n in names}
    OUT = nc.dram_tensor("out", [S, D_MODEL], F32, kind="ExternalOutput").ap()
    cx = Ctx(nc, S)
    import os
    if os.environ.get("KDEBUG"):
        cx.dbg32 = nc.dram_tensor("dbg32", [128, 16384], F32, kind="ExternalOutput").ap()
        cx.dbg16 = nc.dram_tensor("dbg16", [128, 16384], BF16, kind="ExternalOutput").ap()
    X = x_in
    Xs = nc.dram_tensor("xs", [S, D_MODEL], F32, kind="Internal").ap()
    X = Xs
    first = [True]
    for ph in phases:
        if ph[0] == "ffn":
            l, which = ph[1], ph[2]
            phase_ffn(cx, X, W["ffn_%s_norm" % which][l], W["ffn_%s_w_gate" % which][l],
                      W["ffn_%s_w_up" % which][l], W["ffn_%s_w_down" % which][l], ident,
                      Xin=(x_in if first[0] else None))
            first[0] = False
        elif ph[0] == "copy":
            prev = None
            for t in range(cx.NT):
                cx.P.dma("sp", Xs[t * 128:(t + 1) * 128, :], x_in[t * 128:(t + 1) * 128, :],
                         [cx.R("xcopy_chain")], [cx.R("X%d" % t), cx.R("xcopy_chain")], ("xcopy", 0))
        elif ph[0] == "mixA":
            i = ph[1]
            phase_even(cx, X, W["mix_norm"][2 * i], W["ab_w_in"][i], W["ab_w_out"][i], ident, ce_d, ropeR_d, ropeD_d, scr)
        elif ph[0] == "gla":
            i = ph[1]
            l = 2 * i + 1
            phase_gla(cx, X, W["mix_norm"][l], W["gla_w_in"][i], W["gla_w_a2"][i], W["gla_b_a"][i], W["gla_w_out"][i],
                      ident, cf_d)
        elif ph[0] == "final":
            phase_final(cx, X, W["final_norm"], OUT)
        else:
            raise ValueError(ph)
    cx.P.emit()
    return nc, cx


def make_consts():
    cf = {}
    tok = np.arange(128)
    same64 = (tok[:, None] // 64) == (tok[None, :] // 64)
    cf["mc"] = np.where(same64 & (tok[:, None] <= tok[None, :]), -1.0 / 16, 0.0)
    cf["ms"] = np.where(same64 & (tok[:, None] > tok[None, :]), -1.0 / 16, 0.0)
    cf["mch"] = np.stack([np.where(tok < 64, -1.0 / 16, 0.0), np.where(tok >= 64, -1.0 / 16, 0.0)], 1)
    mT = np.where(same64 & (tok[None, :] >= tok[:, None]), 1.0, 0.0)
    cf["gmaskT"] = np.repeat(mT[:, None, :], 4, axis=1).reshape(128, 512)
    cf["mab"] = np.stack([np.where(tok < 64, 1.0, 0.0), np.where(tok >= 64, 1.0, 0.0)], 1)
    off = {}
    cols = []
    c0 = 0
    for k, v in cf.items():
        v = np.asarray(v, np.float32).reshape(128, -1)
        off[k] = (c0, v.shape[1])
        cols.append(v)
        c0 += v.shape[1]
    return np.ascontiguousarray(np.concatenate(cols, 1)), off


CF32, CF_OFF = make_consts()


GLA_LEAD = 10 ** 9


def run_interleaved(bodies, lead):
    active = []
    it = iter(bodies)
    nxt = next(it, None)
    while active or nxt is not None:
        if nxt is not None and (len(active) == 0 or (len(active) == 1 and active[0][1] >= lead)):
            active.append([nxt, 0])
            nxt = next(it, None)
        for a_ in list(active):
            try:
                next(a_[0])
                a_[1] += 1
            except StopIteration:
                active.remove(a_)


def phase_gla(cx, X, g_row, w_in_d, w_a2_d, b_a_d, w_out_d, ident, cf_d):
    nc, P, S = cx.nc, cx.P, cx.S
    H, DK, DV = 4, 128, 256
    GIN = 3088
    with ExitStack() as es:
        sb, ps = phase_alloc(cx, es)
        tiles = []

        def SB(name, shape, dt):
            t = sb(name, shape, dt)
            tiles.append(t)
            return t

        def PS(name, shape, dt):
            t = ps(name, shape, dt)
            tiles.append(t)
            return t

        w_in = SB("w_in", [128, 8, GIN], BF16)
        w_out = SB("w_out", [128, 8, D_MODEL], BF16)
        wa2 = SB("wa2", [17, 512], F32)
        cf = SB("cf", [128, CF32.shape[1]], F32)
        idt = SB("idt", [128, 128], BF16)
        g_bc = SB("g_bc", [128, D_MODEL], F32)
        xt = [SB("xt%d" % b, [128, D_MODEL], F32) for b in range(4)]
        junk = SB("junk", [128, D_MODEL], BF16)
        ssq_2 = [SB("ssq_%d" % i_, [128, 1], F32) for i_ in range(2)]
        rstd_2 = [SB("rstd_%d" % i_, [128, 1], F32) for i_ in range(2)]
        xn_2 = [SB("xn_%d" % i_, [128, D_MODEL], BF16) for i_ in range(2)]
        xnT_2 = [SB("xnT_%d" % i_, [128, 8, 128], BF16) for i_ in range(2)]
        alT_2 = [SB("alT_%d" % i_, [17, 128], F32) for i_ in range(2)]
        e1_2 = [SB("e1_%d" % i_, [128, 512], F32) for i_ in range(2)]
        l1_2 = [SB("l1_%d" % i_, [128, 512], F32) for i_ in range(2)]
        E1_2 = [SB("E1_%d" % i_, [128, 512], F32) for i_ in range(2)]
        E2_2 = [SB("E2_%d" % i_, [128, 512], F32) for i_ in range(2)]
        E3_2 = [SB("E3_%d" % i_, [128, 512], F32) for i_ in range(2)]
        qt_2 = [SB("qt_%d" % i_, [128, 512], BF16) for i_ in range(2)]
        kt_2 = [SB("kt_%d" % i_, [128, 512], BF16) for i_ in range(2)]
        kd_2 = [[SB("kd%d_%d" % (c, i_), [128, 512], BF16) for c in range(2)] for i_ in range(2)]
        vb_2 = [SB("vb_%d" % i_, [128, 1024], BF16) for i_ in range(2)]
        sr_2 = [SB("sr_%d" % i_, [128, 1024], F32) for i_ in range(2)]
        qTf_2 = [SB("qTf_%d" % i_, [128, 4, 128], BF16) for i_ in range(2)]
        qT2_2 = [SB("qT2_%d" % i_, [128, 4, 2, 128], BF16) for i_ in range(2)]
        kT_2 = [SB("kT_%d" % i_, [128, 4, 128], BF16) for i_ in range(2)]
        sT_2 = [SB("sT_%d" % i_, [128, 4, 128], BF16) for i_ in range(2)]
        st = [SB("st%d" % i, [128, 4, 256], F32) for i in range(2)]
        sbf = [SB("sbf%d" % i, [128, 4, 256], BF16) for i in range(3)]
        dec_2 = [SB("dec_%d" % i_, [128, 4, 2], F32) for i_ in range(2)]
        bst_2 = [SB("bst_%d" % i_, [128, 4, 6], F32) for i_ in range(2)]
        mv_2 = [SB("mv_%d" % i_, [128, 4, 2], F32) for i_ in range(2)]
        hr_2 = [SB("hr_%d" % i_, [128, 4], F32) for i_ in range(2)]
        tmp_2 = [SB("tmp_%d" % i_, [128, 1024], F32) for i_ in range(2)]
        og_2 = [SB("og_%d" % i_, [128, 1024], BF16) for i_ in range(2)]
        ogT_2 = [SB("ogT_%d" % i_, [128, 8, 128], BF16) for i_ in range(2)]
        xo_2 = [SB("xo_%d" % i_, [128, D_MODEL], F32) for i_ in range(2)]

        pT = PS("pT", [128, 8, 128], BF16)
        pP = [PS("pP%d" % i, [128, 512], F32) for i in range(2)]
        pB = [PS("pB%d" % i, [128, 512], F32) for i in range(2)]
        pSm = PS("pSm", [128, 512], F32)
        pTq = PS("pTq", [128, 2, 4, 128], BF16)
        pS = PS("pS", [128, 4, 128], F32)

        def cfv(name):
            o, w = CF_OFF[name]
            return cf[:, o:o + w]

        P.dma("sp", idt[:], ident, [], [idt.r], ("id", 0))
        P.dma("sp", cf[:], cf_d, [], [cf.r], ("cf", 0))
        load_gain_bc(cx, g_bc, g_row)
        P.dma("sp", wa2[0:16, :], w_a2_d, [], [wa2.r], ("wa2", 0))
        P.dma("sp", wa2[16:17, :], b_a_d.rearrange("(o f) -> o f", o=1), [], [wa2.r], ("wa2", 0))
        wv = w_in_d.rearrange("(kc p) f -> p kc f", p=128)
        for kh in range(2):
            for (c0, c1) in ((0, 1544), (1544, GIN)):
                P.dma("pool", w_in[:, kh * 4:(kh + 1) * 4, c0:c1], wv[:, kh * 4:(kh + 1) * 4, c0:c1], [], [w_in.r],
                      ("w", "w_in"))
        wov = w_out_d.rearrange("(kc p) f -> p kc f", p=128)
        for kh in range(2):
            P.dma("pool", w_out[:, kh * 4:(kh + 1) * 4, :], wov[:, kh * 4:(kh + 1) * 4, :], [], [w_out.r], ("w", "w_out"))
        for alT in alT_2:
            P.op("dve", lambda e, alT=alT: e.memset(alT[:], 1.0), [], [alT.r])
        P.op("dve", lambda e: e.memset(st[0][:], 0.0), [], [st[0].r])
        P.op("dve", lambda e: e.memset(sbf[0][:], 0.0), [], [sbf[0].r])
        for qT2 in qT2_2:
            P.op("pool", lambda e, qT2=qT2: e.memset(qT2[:], 0.0), [], [qT2.r])

        sc = DK ** -0.5
        def loads(tt):
            bb = tt % 4
            P.dma("sp", xt[bb][:], X[tt * 128:(tt + 1) * 128, :], [cx.R("X%d" % tt)], [xt[bb].r], ("xt", bb))

        def body(t):
            b = t % 4
            k_ = t % 2
            ssq = ssq_2[k_]
            rstd = rstd_2[k_]
            xn = xn_2[k_]
            xnT = xnT_2[k_]
            alT = alT_2[k_]
            e1 = e1_2[k_]
            l1 = l1_2[k_]
            E1 = E1_2[k_]
            E2 = E2_2[k_]
            E3 = E3_2[k_]
            qt = qt_2[k_]
            kt = kt_2[k_]
            vb = vb_2[k_]
            sr = sr_2[k_]
            qTf = qTf_2[k_]
            qT2 = qT2_2[k_]
            kT = kT_2[k_]
            sT = sT_2[k_]
            dec = dec_2[k_]
            bst = bst_2[k_]
            mv = mv_2[k_]
            hr = hr_2[k_]
            tmp = tmp_2[k_]
            og = og_2[k_]
            ogT = ogT_2[k_]
            xo = xo_2[k_]
            kd = kd_2[k_]
            xr = cx.R("X%d" % t)
            if t == 0:
                loads(0)
                loads(1)
            if t + 2 < cx.NT:
                loads(t + 2)
            rms_prep(cx, xt[b], junk, ssq, rstd, xn, g_bc)
            for kc in range(8):
                P.op("pe", lambda e, kc=kc: e.transpose(out=pT[:, kc, :], in_=xn[:, kc * 128:(kc + 1) * 128],
                                                        identity=idt[:]), [xn.r, idt.r], [pT.r])
            P.op("act", lambda e: e.activation(out=xnT[:], in_=pT[:], func=AF.Copy), [pT.r], [xnT.r])
            yield
            for kc in range(8):
                P.op("pe", lambda e, kc=kc: e.matmul(out=pSm[0:16, 0:128], lhsT=w_in[:, kc, 3072:3088], rhs=xnT[:, kc, :],
                                                     start=(kc == 0), stop=(kc == 7)), [w_in.r, xnT.r], [pSm.r])
            P.op("act", lambda e: e.activation(out=alT[0:16, :], in_=pSm[0:16, 0:128], func=AF.Copy), [pSm.r], [alT.r])
            yield
            P.op("pe", lambda e: e.matmul(out=pB[0][:], lhsT=alT[:], rhs=wa2[:], start=True, stop=True),
                 [alT.r, wa2.r], [pB[0].r])
            P.op("act", lambda e: e.activation(out=e1[:], in_=pB[0][:], func=AF.Exp, scale=-1.0), [pB[0].r], [e1.r])
            P.op("act", lambda e: e.activation(out=l1[:], in_=e1[:], func=AF.Ln, bias=1.0), [e1.r], [l1.r])
            yield
            P.op("pe", lambda e: e.matmul(out=pB[0][:], lhsT=cfv("mc"), rhs=l1[:], start=True, stop=True),
                 [cf.r, l1.r], [pB[0].r])
            P.op("pe", lambda e: e.matmul(out=pB[1][:], lhsT=cfv("ms"), rhs=l1[:], start=True, stop=True),
                 [cf.r, l1.r], [pB[1].r])
            for h in range(H):
                P.op("pe", lambda e, h=h: e.matmul(out=pSm[:, 128 + 2 * h:130 + 2 * h], lhsT=l1[:, h * 128:(h + 1) * 128],
                                                   rhs=cfv("mch"), start=True, stop=True), [cf.r, l1.r], [pSm.r])
            P.op("act", lambda e: e.activation(out=E1[:], in_=pB[0][:], func=AF.Exp), [pB[0].r], [E1.r])
            P.op("act", lambda e: e.activation(out=E2[:], in_=pB[0][:], func=AF.Exp, scale=-1.0), [pB[0].r], [E2.r])
            P.op("act", lambda e: e.activation(out=E3[:], in_=pB[1][:], func=AF.Exp), [pB[1].r], [E3.r])
            P.op("act", lambda e: e.activation(out=dec[:].rearrange("p h c -> p (h c)"), in_=pSm[:, 128:136], func=AF.Exp),
                 [pSm.r], [dec.r])
            yield
            def proj(gi, pp):
                for kc in range(8):
                    P.op("pe", lambda e, kc=kc, gi=gi, pp=pp: e.matmul(out=pp[:], lhsT=xnT[:, kc, :],
                                                                       rhs=w_in[:, kc, gi * 512:(gi + 1) * 512],
                                                                       start=(kc == 0), stop=(kc == 7)),
                         [xnT.r, w_in.r], [pp.r])
            proj(0, pP[0])
            P.op("dve", lambda e: e.scalar_tensor_tensor(out=qt[:], in0=pP[0][:], scalar=sc, in1=E1[:], op0=ALU.mult,
                                                         op1=ALU.mult), [pP[0].r, E1.r], [qt.r])
            yield
            proj(1, pP[1])
            P.op("dve", lambda e: e.tensor_tensor(out=kt[:], in0=pP[1][:], in1=E2[:], op=ALU.mult), [pP[1].r, E2.r], [kt.r])
            for c in range(2):
                P.op("dve", lambda e, c=c: e.scalar_tensor_tensor(out=kd[c][:], in0=pP[1][:], scalar=cfv("mab")[:, c:c + 1],
                                                                  in1=E3[:], op0=ALU.mult, op1=ALU.mult),
                     [pP[1].r, E3.r, cf.r], [kd[c].r])
            for gi in (2, 3):
                yield
                pp = pP[gi % 2]
                proj(gi, pp)
                P.op("act", lambda e, gi=gi, pp=pp: e.activation(out=vb[:, (gi - 2) * 512:(gi - 1) * 512], in_=pp[:],
                                                                 func=AF.Copy), [pp.r], [vb.r])
            for gi in (4, 5):
                yield
                pp = pP[gi % 2]
                proj(gi, pp)
                P.op("act", lambda e, gi=gi, pp=pp: e.activation(out=sr[:, (gi - 4) * 512:(gi - 3) * 512], in_=pp[:],
                                                                 func=AF.Silu), [pp.r], [sr.r])
            yield
            for h in range(H):
                P.op("pe", lambda e, h=h: e.transpose(out=pTq[:, 0, h, :], in_=qt[:, h * 128:(h + 1) * 128], identity=idt[:]),
                     [qt.r, idt.r], [pTq.r])
            for h in range(H):
                P.op("pe", lambda e, h=h: e.transpose(out=pTq[:, 1, h, :], in_=kt[:, h * 128:(h + 1) * 128], identity=idt[:]),
                     [kt.r, idt.r], [pTq.r])
            P.op("act", lambda e: e.activation(out=qTf[:], in_=pTq[:, 0, :, :], func=AF.Copy), [pTq.r], [qTf.r])
            P.op("dve", lambda e: e.tensor_copy(out=kT[:], in_=pTq[:, 1, :, :]), [pTq.r], [kT.r])
            P.op("pool", lambda e: e.tensor_copy(out=qT2[:, :, 0, 0:64], in_=qTf[:, :, 0:64]), [qTf.r], [qT2.r])
            P.op("pool", lambda e: e.tensor_copy(out=qT2[:, :, 1, 64:128], in_=qTf[:, :, 64:128]), [qTf.r], [qT2.r])
            yield
            for h in range(H):
                P.op("pe", lambda e, h=h: e.matmul(out=pS[:, h, :], lhsT=kT[:, h, :], rhs=qTf[:, h, :], start=True, stop=True),
                     [kT.r, qTf.r], [pS.r])
            P.op("dve", lambda e: e.tensor_tensor(out=sT[:].rearrange("p h i -> p (h i)"),
                                                  in0=pS[:].rearrange("p h i -> p (h i)"), in1=cfv("gmaskT"), op=ALU.mult),
                 [pS.r, cf.r], [sT.r])
            yield
            b0, b1, b2 = (2 * t) % 3, (2 * t + 1) % 3, (2 * t + 2) % 3
            s0, s1 = 0, 1
            for h in range(H):
                pk = pB[h % 2]
                for c in range(2):
                    P.op("pe", lambda e, h=h, c=c, pk=pk: e.matmul(out=pk[:, c * 256:(c + 1) * 256],
                                                                  lhsT=kd[c][:, h * 128:(h + 1) * 128],
                                                                  rhs=vb[:, h * 256:(h + 1) * 256], start=True, stop=True),
                         [kd[c].r, vb.r], [pk.r])
                P.op("dve", lambda e, h=h, pk=pk, s0=s0, s1=s1: e.scalar_tensor_tensor(out=st[s1][:, h, :], in0=st[s0][:, h, :],
                                                                         scalar=dec[:, h, 0:1], in1=pk[:, 0:256],
                                                                         op0=ALU.mult, op1=ALU.add),
                     [st[s0].r, dec.r, pk.r], [st[s1].r])
                P.op("pool", lambda e, h=h, b1=b1, s1=s1: e.tensor_copy(out=sbf[b1][:, h, :], in_=st[s1][:, h, :]), [st[s1].r], [sbf[b1].r])
                P.op("dve", lambda e, h=h, pk=pk, s0=s0, s1=s1: e.scalar_tensor_tensor(out=st[s0][:, h, :], in0=st[s1][:, h, :],
                                                                         scalar=dec[:, h, 1:2], in1=pk[:, 256:512],
                                                                         op0=ALU.mult, op1=ALU.add),
                     [st[s1].r, dec.r, pk.r], [st[s0].r])
                P.op("pool", lambda e, h=h, b2=b2, s0=s0: e.tensor_copy(out=sbf[b2][:, h, :], in_=st[s0][:, h, :]), [st[s0].r], [sbf[b2].r])
            for h in range(H):
                po = pP[h // 2]
                osl = slice((h % 2) * 256, (h % 2 + 1) * 256)
                P.op("pe", lambda e, h=h, po=po, osl=osl, b0=b0, b1=b1: e.matmul(out=po[:, osl], lhsT=sT[:, h, :],
                                                                  rhs=vb[:, h * 256:(h + 1) * 256], start=True, stop=False),
                     [sT.r, vb.r], [po.r])
                P.op("pe", lambda e, h=h, po=po, osl=osl, b0=b0, b1=b1: e.matmul(out=po[:, osl], lhsT=qT2[:, h, 0, :],
                                                                  rhs=sbf[b0][:, h, :], start=False, stop=False),
                     [qT2.r, sbf[b0].r], [po.r])
                P.op("pe", lambda e, h=h, po=po, osl=osl, b0=b0, b1=b1: e.matmul(out=po[:, osl], lhsT=qT2[:, h, 1, :],
                                                                  rhs=sbf[b1][:, h, :], start=False, stop=True),
                     [qT2.r, sbf[b1].r], [po.r])
            for h in range(H):
                po = pP[h // 2]
                osl = slice((h % 2) * 256, (h % 2 + 1) * 256)
                P.op("dve", lambda e, h=h, po=po, osl=osl: e.bn_stats(out=bst[:, h, :], in_=po[:, osl]), [po.r], [bst.r])
                P.op("dve", lambda e, h=h: e.bn_aggr(out=mv[:, h, :], in_=bst[:, h, :]), [bst.r], [mv.r])
            P.op("act", lambda e: e.activation(out=hr[:], in_=mv[:, :, 1], func=AF.Sqrt, bias=EPS), [mv.r], [hr.r])
            P.op("dve", lambda e: e.reciprocal(out=hr[:], in_=hr[:]), [hr.r], [hr.r])
            for h in range(H):
                po = pP[h // 2]
                osl = slice((h % 2) * 256, (h % 2 + 1) * 256)
                P.op("dve", lambda e, h=h, po=po, osl=osl: e.tensor_scalar(out=tmp[:, h * 256:(h + 1) * 256], in0=po[:, osl],
                                                                          scalar1=mv[:, h, 0:1], scalar2=hr[:, h:h + 1],
                                                                          op0=ALU.subtract, op1=ALU.mult),
                     [po.r, mv.r, hr.r], [tmp.r])
            P.op("pool", lambda e: e.tensor_tensor(out=og[:], in0=tmp[:], in1=sr[:], op=ALU.mult), [tmp.r, sr.r], [og.r])
            yield
            for ec in range(8):
                P.op("pe", lambda e, ec=ec: e.transpose(out=pT[:, ec, :], in_=og[:, ec * 128:(ec + 1) * 128], identity=idt[:]),
                     [og.r, idt.r], [pT.r])
            P.op("act", lambda e: e.activation(out=ogT[:], in_=pT[:], func=AF.Copy), [pT.r], [ogT.r])
            for hf in range(2):
                for ec in range(8):
                    P.op("pe", lambda e, hf=hf, ec=ec: e.matmul(out=pP[hf][:], lhsT=ogT[:, ec, :],
                                                               rhs=w_out[:, ec, hf * 512:(hf + 1) * 512],
                                                               start=(ec == 0), stop=(ec == 7)), [ogT.r, w_out.r], [pP[hf].r])
                P.op("dve", lambda e, hf=hf, b=b: e.tensor_tensor(out=xo[:, hf * 512:(hf + 1) * 512], in0=pP[hf][:],
                                                                  in1=xt[b][:, hf * 512:(hf + 1) * 512], op=ALU.add),
                     [pP[hf].r, xt[b].r], [xo.r])
            P.dma("sp", X[t * 128:(t + 1) * 128, :], xo[:], [xo.r], [xr], ("xo", k_))
        run_interleaved([body(t) for t in range(cx.NT)], lead=GLA_LEAD)
        barrier(cx, tiles)


RET_G = [1.0 - 2.0 ** (-5.0 - h) for h in range(4)]
DIL_PATTERNS = ((128, 1), (512, 4), (2048, 16))


def make_consts_even(S):
    tok = np.arange(128)
    cf = {}
    lg = [math.log(g) for g in RET_G]
    dm = np.zeros((128, 4, 128))
    gq = np.zeros((128, 4, 128))
    gk = np.zeros((128, 4, 128))
    for h in range(4):
        rel = tok[None, :] - tok[:, None]
        dm[:, h, :] = np.where(rel >= 0, np.exp(lg[h] * np.maximum(rel, 0)), 0.0)
        gq[:, h, :] = np.exp(lg[h] * (tok + 1.0))[:, None]
        gk[:, h, :] = np.exp(lg[h] * (127.0 - tok))[:, None]
    cf["dmT"] = dm.reshape(128, 512)
    cf["gq"] = gq.reshape(128, 512)
    cf["gk"] = gk.reshape(128, 512)
    prev = np.where(tok[:, None] >= tok[None, :], 1.0, 0.0)
    same = np.where(tok[None, :] >= tok[:, None], 1.0, 0.0)
    cf["dilm"] = np.stack([prev, same, prev, same], 1).reshape(128, 512)
    off = {}
    cols = []
    c0 = 0
    for k, v in cf.items():
        v = np.asarray(v, np.float32).reshape(128, -1)
        off[k] = (c0, v.shape[1])
        cols.append(v)
        c0 += v.shape[1]
    ce = np.ascontiguousarray(np.concatenate(cols, 1))
    pos = np.arange(S, dtype=np.float32)
    fr = (np.float32(10000.0) ** (-np.linspace(0.0, 1.0, 64, dtype=np.float32))).astype(np.float32)
    ang = pos[:, None] * fr[None, :]
    ropeR = np.concatenate([np.tile(np.cos(ang), (1, 4)), np.tile(np.sin(ang), (1, 4))], 1).astype(np.float32)
    fd = (np.float32(500000.0) ** (-np.arange(0, 16, 2, dtype=np.float32) / np.float32(16))).astype(np.float32)
    angd = pos[:, None] * fd[None, :]
    ropeD = np.concatenate([np.tile(np.cos(angd), (1, 8)), np.tile(np.sin(angd), (1, 8))], 1).astype(np.float32)
    return ce, off, np.ascontiguousarray(ropeR), np.ascontiguousarray(ropeD)


CE32, CE_OFF, _, _ = make_consts_even(128)


def phase_even(cx, X, g_row, w_in_d, w_out_d, ident, ce_d, ropeR_d, ropeD_d, scr):
    nc, P, S = cx.nc, cx.P, cx.S
    NT = cx.NT
    EIN = 3584
    QD, KD, VD, ORD, UB = scr["QD"], scr["KD"], scr["VD"], scr["ORD"], scr["UB"]
    def stageA():
        with ExitStack() as es:
            sb, ps = phase_alloc(cx, es)
            tiles = []

            def SB(name, shape, dt):
                t = sb(name, shape, dt)
                tiles.append(t)
                return t

            def PS(name, shape, dt):
                t = ps(name, shape, dt)
                tiles.append(t)
                return t

            w_in = SB("w_in", [128, 8, EIN], BF16)
            ce = SB("ce", [128, CE32.shape[1]], F32)
            idt = SB("idt", [128, 128], BF16)
            g_bc = SB("g_bc", [128, D_MODEL], F32)
            xt = [SB("xt%d" % b, [128, D_MODEL], F32) for b in range(4)]
            rR = [SB("rR%d" % b, [128, 512], F32) for b in range(4)]
            rD = [SB("rD%d" % b, [128, 128], F32) for b in range(4)]
            junk = SB("junk", [128, D_MODEL], BF16)
            ssq_2 = [SB("ssq_%d" % i_, [128, 1], F32) for i_ in range(2)]
            rstd_2 = [SB("rstd_%d" % i_, [128, 1], F32) for i_ in range(2)]
            xn_2 = [SB("xn_%d" % i_, [128, D_MODEL], BF16) for i_ in range(2)]
            xnT_2 = [SB("xnT_%d" % i_, [128, 8, 128], BF16) for i_ in range(2)]
            ta_2 = [SB("ta_%d" % i_, [128, 256], F32) for i_ in range(2)]
            tb_2 = [SB("tb_%d" % i_, [128, 256], F32) for i_ in range(2)]
            qr_2 = [SB("qr_%d" % i_, [128, 512], F32) for i_ in range(2)]
            kr_2 = [SB("kr_%d" % i_, [128, 512], F32) for i_ in range(2)]
            qb_2 = [SB("qb_%d" % i_, [128, 3, 512], BF16) for i_ in range(2)]
            kdec_2 = [SB("kdec_%d" % i_, [128, 512], BF16) for i_ in range(2)]
            vb_2 = [SB("vb_%d" % i_, [128, 512], BF16) for i_ in range(2)]
            sg_2 = [SB("sg_%d" % i_, [128, 512], F32) for i_ in range(2)]
            qkT_2 = [SB("qkT_%d" % i_, [128, 3, 4, 128], BF16) for i_ in range(2)]
            sT_2 = [SB("sT_%d" % i_, [128, 4, 128], BF16) for i_ in range(2)]
            st = SB("st", [128, 4, 128], F32)
            sbf = [SB("sbf%d" % i, [128, 4, 128], BF16) for i in range(2)]
            bst_2 = [SB("bst_%d" % i_, [128, 4, 6], F32) for i_ in range(2)]
            mv_2 = [SB("mv_%d" % i_, [128, 4, 2], F32) for i_ in range(2)]
            hr_2 = [SB("hr_%d" % i_, [128, 4], F32) for i_ in range(2)]
            tmp_2 = [SB("tmp_%d" % i_, [128, 512], F32) for i_ in range(2)]
            orb_2 = [SB("orb_%d" % i_, [128, 512], BF16) for i_ in range(2)]
            dq_2 = [SB("dq_%d" % i_, [128, 512], BF16) for i_ in range(2)]
            dk_2 = [SB("dk_%d" % i_, [128, 512], BF16) for i_ in range(2)]
            va_2 = [SB("va_%d" % i_, [128, 8, 65], BF16) for i_ in range(2)]
            da_2 = [SB("da_%d" % i_, [128, 64], F32) for i_ in range(2)]
            db_2 = [SB("db_%d" % i_, [128, 64], F32) for i_ in range(2)]
            prq_2 = [SB("prq_%d" % i_, [128, 512], F32) for i_ in range(2)]
            prk_2 = [SB("prk_%d" % i_, [128, 512], F32) for i_ in range(2)]
            drq_2 = [SB("drq_%d" % i_, [128, 128], F32) for i_ in range(2)]
            drk_2 = [SB("drk_%d" % i_, [128, 128], F32) for i_ in range(2)]

            pT = PS("pT", [128, 8, 128], BF16)
            pP = [PS("pP%d" % i, [128, 512], F32) for i in range(3)]
            pT3 = [PS("pT3%d" % i, [128, 8, 128], BF16) for i in range(1)]
            pS = PS("pS", [128, 4, 128], F32)
            pKV = PS("pKV", [128, 4, 128], F32)
            pO = PS("pO", [128, 4, 128], F32)

            def cev(name):
                o, w = CE_OFF[name]
                return ce[:, o:o + w]

            P.dma("sp", idt[:], ident, [], [idt.r], ("id", 0))
            P.dma("sp", ce[:], ce_d, [], [ce.r], ("ce", 0))
            load_gain_bc(cx, g_bc, g_row)
            wv = w_in_d.rearrange("(kc p) f -> p kc f", p=128)
            for kh in range(2):
                for (c0, c1) in ((0, 1792), (1792, EIN)):
                    P.dma("pool", w_in[:, kh * 4:(kh + 1) * 4, c0:c1], wv[:, kh * 4:(kh + 1) * 4, c0:c1], [], [w_in.r],
                          ("w", "w_in"))
            P.op("dve", lambda e: e.memset(st[:], 0.0), [], [st.r])
            P.op("dve", lambda e: e.memset(sbf[0][:], 0.0), [], [sbf[0].r])
            for va in va_2:
                P.op("pool", lambda e, va=va: e.memset(va[:], 1.0), [], [va.r])
            scK = 128 ** -0.5

            def proj(gi, pp, xnT):
                for kc in range(8):
                    P.op("pe", lambda e, kc=kc, gi=gi, pp=pp, xnT=xnT: e.matmul(out=pp[:], lhsT=xnT[:, kc, :],
                                                                       rhs=w_in[:, kc, gi * 512:(gi + 1) * 512],
                                                                       start=(kc == 0), stop=(kc == 7)),
                         [xnT.r, w_in.r], [pp.r])

            def rot(pp, rt, nh, hd, half, dst, scale, scratch):
                pv = pp[:].rearrange("p (h d) -> p h d", h=nh)
                x1 = pv[:, :, 0:half]
                x2 = pv[:, :, half:2 * half]
                n = nh * half
                cosv = rt[:, 0:n].rearrange("p (h d) -> p h d", h=nh)
                sinv = rt[:, n:2 * n].rearrange("p (h d) -> p h d", h=nh)
                A, B = scratch
                Av = A[:, 0:n].rearrange("p (h d) -> p h d", h=nh)
                Bv = B[:, 0:n].rearrange("p (h d) -> p h d", h=nh)
                dv = dst.rearrange("p (h d) -> p h d", h=nh)
                for (u, w_, sgn, lo) in ((x1, x2, ALU.subtract, 0), (x2, x1, ALU.add, half)):
                    P.op("dve", lambda e, u=u: e.scalar_tensor_tensor(out=Av, in0=u, scalar=scale, in1=cosv, op0=ALU.mult,
                                                                      op1=ALU.mult), [pp.r, rt_res[0]], [A.r])
                    P.op("dve", lambda e, w_=w_: e.scalar_tensor_tensor(out=Bv, in0=w_, scalar=scale, in1=sinv, op0=ALU.mult,
                                                                        op1=ALU.mult), [pp.r, rt_res[0]], [B.r])
                    P.op("pool", lambda e, sgn=sgn, lo=lo: e.tensor_tensor(out=dv[:, :, lo:lo + half], in0=Av, in1=Bv, op=sgn),
                         [A.r, B.r], [dst_res[0]])

            rt_res = [None]
            dst_res = [None]
            def loads(tt):
                bb = tt % 4
                P.dma("sp", xt[bb][:], X[tt * 128:(tt + 1) * 128, :], [cx.R("X%d" % tt)], [xt[bb].r], ("xt", bb))
                P.dma("sp", rR[bb][:], ropeR_d[tt * 128:(tt + 1) * 128, :], [], [rR[bb].r], ("rR", bb))
                P.dma("sp", rD[bb][:], ropeD_d[tt * 128:(tt + 1) * 128, :], [], [rD[bb].r], ("rD", bb))

            def body(t):
                b = t % 4
                k_ = t % 2
                ssq = ssq_2[k_]
                rstd = rstd_2[k_]
                xn = xn_2[k_]
                xnT = xnT_2[k_]
                ta = ta_2[k_]
                tb = tb_2[k_]
                qr = qr_2[k_]
                kr = kr_2[k_]
                qb = qb_2[k_]
                kdec = kdec_2[k_]
                vb = vb_2[k_]
                sg = sg_2[k_]
                qkT = qkT_2[k_]
                sT = sT_2[k_]
                bst = bst_2[k_]
                mv = mv_2[k_]
                hr = hr_2[k_]
                tmp = tmp_2[k_]
                orb = orb_2[k_]
                dq = dq_2[k_]
                dk = dk_2[k_]
                va = va_2[k_]
                da = da_2[k_]
                db = db_2[k_]
                prq = prq_2[k_]
                prk = prk_2[k_]
                drq = drq_2[k_]
                drk = drk_2[k_]
                if t == 0:
                    loads(0)
                    loads(1)
                if t + 2 < NT:
                    loads(t + 2)
                rms_prep(cx, xt[b], junk, ssq, rstd, xn, g_bc)
                for kc in range(8):
                    P.op("pe", lambda e, kc=kc: e.transpose(out=pT[:, kc, :], in_=xn[:, kc * 128:(kc + 1) * 128],
                                                            identity=idt[:]), [xn.r, idt.r], [pT.r])
                P.op("act", lambda e: e.activation(out=xnT[:], in_=pT[:], func=AF.Copy), [pT.r], [xnT.r])
                yield
                proj(0, pP[0], xnT)
                rt_res[0], dst_res[0] = rR[b].r, qr.r
                P.op("act", lambda e: e.activation(out=prq[:], in_=pP[0][:], func=AF.Copy), [pP[0].r], [prq.r])
                rot(prq, rR[b], 4, 128, 64, qr[:], 1.0, (ta, tb))
                P.op("act", lambda e: e.activation(out=qb[:, 0, :], in_=qr[:], func=AF.Copy), [qr.r], [qb.r])
                P.op("dve", lambda e: e.tensor_tensor(out=qb[:, 1, :], in0=qr[:], in1=cev("gq"), op=ALU.mult), [qr.r, ce.r], [qb.r])
                yield
                proj(1, pP[1], xnT)
                rt_res[0], dst_res[0] = rR[b].r, kr.r
                P.op("act", lambda e: e.activation(out=prk[:], in_=pP[1][:], func=AF.Copy), [pP[1].r], [prk.r])
                rot(prk, rR[b], 4, 128, 64, kr[:], scK, (ta, tb))
                P.op("act", lambda e: e.activation(out=qb[:, 2, :], in_=kr[:], func=AF.Copy), [kr.r], [qb.r])
                P.op("dve", lambda e: e.tensor_tensor(out=kdec[:], in0=kr[:], in1=cev("gk"), op=ALU.mult), [kr.r, ce.r], [kdec.r])
                yield
                proj(2, pP[2], xnT)
                P.op("act", lambda e: e.activation(out=vb[:], in_=pP[2][:], func=AF.Copy), [pP[2].r], [vb.r])
                proj(3, pP[0], xnT)
                P.op("act", lambda e: e.activation(out=sg[:], in_=pP[0][:], func=AF.Silu), [pP[0].r], [sg.r])
                yield
                for (gi, pp, dst, dram) in ((4, pP[1], dq, QD), (5, pP[2], dk, KD)):
                    if gi == 5:
                        yield
                    proj(gi, pp, xnT)
                    P.op("act", lambda e, pp=pp, dst=dst: e.activation(out=dst[:], in_=pp[:], func=AF.Copy), [pp.r], [dst.r])
                    rt_res[0], dst_res[0] = rD[b].r, dst.r
                    drw = drq if gi == 4 else drk
                    P.op("act", lambda e, pp=pp, drw=drw: e.activation(
                        out=drw[:].rearrange("p (h d) -> p h d", h=8),
                        in_=pp[:].rearrange("p (h d) -> p h d", h=8)[:, :, 0:16], func=AF.Copy), [pp.r], [drw.r])
                    rot(drw, rD[b], 8, 64, 8, dst[:], 1.0, (da, db))
                    P.dma("sp", dram[t * 128:(t + 1) * 128, :], dst[:], [dst.r], [cx.R("%s%d" % (dram.tensor.name, t))],
                          ("st", dst.r.name, k_))
                yield
                proj(6, pP[0], xnT)
                P.op("act", lambda e: e.activation(out=va[:, :, 0:64], in_=pP[0][:].rearrange("p (h d) -> p h d", h=8),
                                                   func=AF.Copy), [pP[0].r], [va.r])
                P.dma("sp", VD[t * 128:(t + 1) * 128, :], va[:].rearrange("p h d -> p (h d)"), [va.r], [cx.R("VD%d" % t)],
                      ("st", "va", k_))
                yield
                for j in range(3):
                    for h in range(4):
                        P.op("pe", lambda e, j=j, h=h: e.transpose(out=pT3[0][:, h, :], in_=qb[:, j, h * 128:(h + 1) * 128],
                                                                   identity=idt[:]), [qb.r, idt.r], [pT3[0].r])
                    P.op("act", lambda e, j=j: e.activation(out=qkT[:, j, :, :], in_=pT3[0][:, 0:4, :], func=AF.Copy), [pT3[0].r], [qkT.r])
                for h in range(4):
                    P.op("pe", lambda e, h=h: e.matmul(out=pS[:, h, :], lhsT=qkT[:, 2, h, :], rhs=qkT[:, 0, h, :], start=True,
                                                       stop=True), [qkT.r], [pS.r])
                P.op("dve", lambda e: e.tensor_tensor(out=sT[:].rearrange("p h i -> p (h i)"),
                                                      in0=pS[:].rearrange("p h i -> p (h i)"), in1=cev("dmT"), op=ALU.mult),
                     [pS.r, ce.r], [sT.r])
                yield
                sp, sn = sbf[t % 2], sbf[(t + 1) % 2]
                for h in range(4):
                    P.op("pe", lambda e, h=h: e.matmul(out=pKV[:, h, :], lhsT=kdec[:, h * 128:(h + 1) * 128],
                                                       rhs=vb[:, h * 128:(h + 1) * 128], start=True, stop=True),
                         [kdec.r, vb.r], [pKV.r])
                for h in range(4):
                    P.op("pe", lambda e, h=h: e.matmul(out=pO[:, h, :], lhsT=sT[:, h, :], rhs=vb[:, h * 128:(h + 1) * 128],
                                                       start=True, stop=False), [sT.r, vb.r], [pO.r])
                    P.op("pe", lambda e, h=h, sp=sp: e.matmul(out=pO[:, h, :], lhsT=qkT[:, 1, h, :], rhs=sp[:, h, :],
                                                              start=False, stop=True), [qkT.r, sp.r], [pO.r])
                for h in range(4):
                    P.op("dve", lambda e, h=h: e.scalar_tensor_tensor(out=st[:, h, :], in0=st[:, h, :], scalar=RET_G[h] ** 128,
                                                                      in1=pKV[:, h, :], op0=ALU.mult, op1=ALU.add),
                         [st.r, pKV.r], [st.r])
                P.op("pool", lambda e, sn=sn: e.tensor_copy(out=sn[:], in_=st[:]), [st.r], [sn.r])
                for h in range(4):
                    P.op("dve", lambda e, h=h: e.bn_stats(out=bst[:, h, :], in_=pO[:, h, :]), [pO.r], [bst.r])
                    P.op("dve", lambda e, h=h: e.bn_aggr(out=mv[:, h, :], in_=bst[:, h, :]), [bst.r], [mv.r])
                P.op("act", lambda e: e.activation(out=hr[:], in_=mv[:, :, 1], func=AF.Sqrt, bias=EPS), [mv.r], [hr.r])
                P.op("dve", lambda e: e.reciprocal(out=hr[:], in_=hr[:]), [hr.r], [hr.r])
                for h in range(4):
                    P.op("dve", lambda e, h=h: e.tensor_scalar(out=tmp[:, h * 128:(h + 1) * 128], in0=pO[:, h, :],
                                                               scalar1=mv[:, h, 0:1], scalar2=hr[:, h:h + 1],
                                                               op0=ALU.subtract, op1=ALU.mult), [pO.r, mv.r, hr.r], [tmp.r])
                P.op("pool", lambda e: e.tensor_tensor(out=orb[:], in0=tmp[:], in1=sg[:], op=ALU.mult), [tmp.r, sg.r], [orb.r])
                P.dma("sp", ORD[t * 128:(t + 1) * 128, :], orb[:], [orb.r], [cx.R("ORD%d" % t)], ("st", "orb", k_))
            run_interleaved([body(t) for t in range(NT)], lead=5)
            barrier(cx, tiles)

    import os
    _st = os.environ.get('EVEN_STAGES', 'ABC')
    if 'A' in _st:
        stageA()
    def stageB():
        with ExitStack() as es:
            sb, ps = phase_alloc(cx, es)
            tiles = []

            def SB(name, shape, dt):
                t = sb(name, shape, dt)
                tiles.append(t)
                return t

            def PS(name, shape, dt):
                t = ps(name, shape, dt)
                tiles.append(t)
                return t

            ce = SB("ce", [128, CE32.shape[1]], F32)
            idt = SB("idt", [128, 128], BF16)
            mk = SB("mk", [128, 512], BF16)
            qt_ = [SB("qt%d" % i, [128, 512], BF16) for i in range(2)]
            kt_ = [SB("kt%d" % i, [128, 512], BF16) for i in range(2)]
            vt_ = [SB("vt%d" % i, [128, 8, 65], BF16) for i in range(3)]
            qT_2 = [SB("qT_%d" % i_, [128, 4, 128], BF16) for i_ in range(2)]
            kT = [SB("kT%d" % i, [128, 4, 128], BF16) for i in range(3)]
            pe_ = [SB("pe%d" % i, [128, 512], BF16) for i in range(4)]
            pm = [SB("pm%d" % i, [128, 4, 128], BF16) for i in range(4)]
            us = [SB("us%d" % i, [128, 520], F32) for i in range(2)]
            pTq = PS("pTq", [128, 8, 128], BF16)
            pTk = PS("pTk", [128, 8, 128], BF16)
            pS = [PS("pS%d" % i, [128, 4, 128], F32) for i in range(4)]
            pU = [PS("pU%d" % i, [128, 512], F32) for i in range(2)]

            P.dma("sp", idt[:], ident, [], [idt.r], ("id", 0))
            P.dma("sp", ce[:], ce_d, [], [ce.r], ("ce", 0))
            o_, w_ = CE_OFF["dilm"]
            P.op("act", lambda e: e.activation(out=mk[:], in_=ce[:, o_:o_ + w_], func=AF.Copy), [ce.r], [mk.r])
            mk4 = mk[:].rearrange("p (a c i) -> p a c i", a=2, c=2)
            sc = 64 ** -0.5
            def body(bi, r, rho, n, cnt):
                Qv = QD.rearrange("(l r) f -> r l f", r=r)
                Kv = KD.rearrange("(l r) f -> r l f", r=r)
                Vv = VD.rearrange("(l r) f -> r l f", r=r)
                Uv = UB[bi].rearrange("(l r) f -> r l f", r=r)
                i2 = cnt % 2
                i3 = cnt % 3
                ip = (cnt - 1) % 3
                qT = qT_2[i2]
                rows = slice(n * 128, (n + 1) * 128)
                src_q = [cx.R("%s%d" % (QD.tensor.name, t)) for t in range(NT)]
                src_k = [cx.R("%s%d" % (KD.tensor.name, t)) for t in range(NT)]
                src_v = [cx.R("VD%d" % t) for t in range(NT)]
                P.dma("sp", qt_[i2][:], Qv[rho, rows, :], src_q, [qt_[i2].r], ("ld", "q", i2))
                P.dma("sp", kt_[i2][:], Kv[rho, rows, :], src_k, [kt_[i2].r], ("ld", "k", i2))
                P.dma("sp", vt_[i3][:].rearrange("p h d -> p (h d)"), Vv[rho, rows, :], src_v, [vt_[i3].r], ("ld", "v", i3))
                for hp in range(4):
                    P.op("pe", lambda e, hp=hp, i2=i2: e.transpose(out=pTq[:, hp, :], in_=qt_[i2][:, hp * 128:(hp + 1) * 128],
                                                                   identity=idt[:]), [qt_[i2].r, idt.r], [pTq.r])
                P.op("act", lambda e: e.activation(out=qT[:], in_=pTq[:, 0:4, :], func=AF.Copy), [pTq.r], [qT.r])
                for hp in range(4):
                    P.op("pe", lambda e, hp=hp, i2=i2: e.transpose(out=pTk[:, hp, :], in_=kt_[i2][:, hp * 128:(hp + 1) * 128],
                                                                   identity=idt[:]), [kt_[i2].r, idt.r], [pTk.r])
                P.op("dve", lambda e, i3=i3: e.tensor_copy(out=kT[i3][:], in_=pTk[:, 0:4, :]), [pTk.r], [kT[i3].r])
                yield
                for hq in range(2):
                    psb = [pS[hq * 2 + 0], pS[hq * 2 + 1]]
                    peb = [pe_[hq * 2 + 0], pe_[hq * 2 + 1]]
                    pmb = [pm[hq * 2 + 0], pm[hq * 2 + 1]]
                    for hpi in range(2):
                        hp = hq * 2 + hpi
                        for hh in range(2):
                            prt = slice(hh * 64, (hh + 1) * 64)
                            ps_ = psb[hh]
                            if n > 0:
                                P.op("pe", lambda e, hp=hp, hpi=hpi, prt=prt, ps_=ps_, ip=ip: e.matmul(
                                    out=ps_[:, hpi * 2 + 0, :], lhsT=kT[ip][prt, hp, :], rhs=qT[prt, hp, :], start=True,
                                    stop=True), [kT[ip].r, qT.r], [ps_.r])
                            P.op("pe", lambda e, hp=hp, hpi=hpi, prt=prt, ps_=ps_, i3=i3: e.matmul(
                                out=ps_[:, hpi * 2 + 1, :], lhsT=kT[i3][prt, hp, :], rhs=qT[prt, hp, :], start=True,
                                stop=True), [kT[i3].r, qT.r], [ps_.r])
                    for hh in range(2):
                        ps_, pe__, pm_ = psb[hh], peb[hh], pmb[hh]
                        pe4 = pe__[:].rearrange("p (a c i) -> p a c i", a=2, c=2)
                        ps4 = ps_[:].rearrange("p (a c) i -> p a c i", a=2)
                        pm4 = pm_[:].rearrange("p (a c) i -> p a c i", a=2)
                        if n > 0:
                            P.op("act", lambda e, ps_=ps_, pe__=pe__: e.activation(
                                out=pe__[:], in_=ps_[:].rearrange("p a i -> p (a i)"), func=AF.Exp, scale=sc),
                                [ps_.r], [pe__.r])
                            P.op("pool", lambda e, pe__=pe__, pm_=pm_: e.tensor_tensor(
                                out=pm_[:].rearrange("p a i -> p (a i)"), in0=pe__[:], in1=mk[:], op=ALU.mult),
                                [pe__.r, mk.r], [pm_.r])
                        else:
                            P.op("act", lambda e, pe4=pe4, ps4=ps4: e.activation(
                                out=pe4[:, :, 1, :], in_=ps4[:, :, 1, :], func=AF.Exp, scale=sc), [ps_.r], [pe__.r])
                            P.op("pool", lambda e, pe4=pe4, pm4=pm4: e.tensor_tensor(
                                out=pm4[:, :, 1, :], in0=pe4[:, :, 1, :], in1=mk4[:, :, 1, :], op=ALU.mult),
                                [pe__.r, mk.r], [pm_.r])
                    for hpi in range(2):
                        hp = hq * 2 + hpi
                        for hh in range(2):
                            h = hp * 2 + hh
                            pu = pU[h // 4]
                            pm_ = pmb[hh]
                            usl = slice((h % 4) * 65, (h % 4 + 1) * 65)
                            if n > 0:
                                P.op("pe", lambda e, h=h, hpi=hpi, pu=pu, pm_=pm_, ip=ip, usl=usl: e.matmul(
                                    out=pu[:, usl], lhsT=pm_[:, hpi * 2 + 0, :], rhs=vt_[ip][:, h, :], start=True,
                                    stop=False), [pm_.r, vt_[ip].r], [pu.r])
                            P.op("pe", lambda e, h=h, hpi=hpi, pu=pu, pm_=pm_, i3=i3, n=n, usl=usl: e.matmul(
                                out=pu[:, usl], lhsT=pm_[:, hpi * 2 + 1, :], rhs=vt_[i3][:, h, :], start=(n == 0),
                                stop=True), [pm_.r, vt_[i3].r], [pu.r])
                    P.op("act", lambda e, hq=hq, i2=i2: e.activation(out=us[i2][:, hq * 260:(hq + 1) * 260],
                                                                     in_=pU[hq][:, 0:260], func=AF.Copy),
                         [pU[hq].r], [us[i2].r])
                    yield
                dst_res = []
                for tt in range(NT):
                    dst_res.append(cx.R("UB%d_%d" % (bi, tt)))
                P.dma("sp", Uv[rho, rows, :], us[i2][:], [us[i2].r], dst_res, ("st", "us", i2))
            bodies = []
            cnt = 0
            for bi, (window, r) in enumerate(DIL_PATTERNS):
                nb = (S // r) // 128
                for rho in range(r):
                    for n in range(nb):
                        bodies.append(body(bi, r, rho, n, cnt))
                        cnt += 1
            run_interleaved(bodies, lead=2)
            barrier(cx, tiles)

    if 'B' in _st:
        stageB()
    def stageC():
        with ExitStack() as es:
            sb, ps = phase_alloc(cx, es)
            tiles = []

            def SB(name, shape, dt):
                t = sb(name, shape, dt)
                tiles.append(t)
                return t

            def PS(name, shape, dt):
                t = ps(name, shape, dt)
                tiles.append(t)
                return t

            w_out = SB("w_out", [128, 8, D_MODEL], BF16)
            idt = SB("idt", [128, 128], BF16)
            xt = [SB("xt%d" % b, [128, D_MODEL], F32) for b in range(4)]
            u = [[SB("u%d%d" % (b, i), [128, 8, 65], F32) for i in range(3)] for b in range(4)]
            cat = [SB("cat%d" % b, [128, 1024], BF16) for b in range(4)]
            rc_2 = [SB("rc_%d" % i_, [128, 8], F32) for i_ in range(2)]
            catT_2 = [SB("catT_%d" % i_, [128, 8, 128], BF16) for i_ in range(2)]
            xo = [SB("xo%d" % b, [128, D_MODEL], F32) for b in range(2)]
            pT = PS("pT", [128, 8, 128], BF16)
            pY = [PS("pY%d" % i, [128, 512], F32) for i in range(2)]
            P.dma("sp", idt[:], ident, [], [idt.r], ("id", 0))
            wov = w_out_d.rearrange("(kc p) f -> p kc f", p=128)
            for kh in range(2):
                P.dma("pool", w_out[:, kh * 4:(kh + 1) * 4, :], wov[:, kh * 4:(kh + 1) * 4, :], [], [w_out.r], ("w", "w_out"))
            def loads(tt):
                bb = tt % 4
                rws = slice(tt * 128, (tt + 1) * 128)
                P.dma("sp", xt[bb][:], X[rws, :], [cx.R("X%d" % tt)], [xt[bb].r], ("xt", bb))
                P.dma("sp", cat[bb][:, 0:512], ORD[rws, :], [cx.R("ORD%d" % tt)], [cat[bb].r], ("ld", "cat", bb))
                for i in range(3):
                    P.dma("sp", u[bb][i][:].rearrange("p h d -> p (h d)"), UB[i][rws, :], [cx.R("UB%d_%d" % (i, tt))], [u[bb][i].r],
                          ("ld", "u", bb, i))

            def body(t):
                b = t % 4
                k_ = t % 2
                rc = rc_2[k_]
                catT = catT_2[k_]
                xr = cx.R("X%d" % t)
                rows = slice(t * 128, (t + 1) * 128)
                if t == 0:
                    loads(0)
                    loads(1)
                if t + 2 < NT:
                    loads(t + 2)
                P.op("dve", lambda e, b=b: e.tensor_tensor(out=u[b][0][:], in0=u[b][0][:], in1=u[b][1][:], op=ALU.add),
                     [u[b][0].r, u[b][1].r], [u[b][0].r])
                P.op("dve", lambda e, b=b: e.tensor_tensor(out=u[b][0][:], in0=u[b][0][:], in1=u[b][2][:], op=ALU.add),
                     [u[b][0].r, u[b][2].r], [u[b][0].r])
                P.op("dve", lambda e, b=b: e.reciprocal(out=rc[:], in_=u[b][0][:, :, 64]), [u[b][0].r], [rc.r])
                for h in range(8):
                    P.op("dve", lambda e, b=b, h=h: e.tensor_scalar(out=cat[b][:, 512 + h * 64:512 + (h + 1) * 64],
                                                                    in0=u[b][0][:, h, 0:64], scalar1=rc[:, h:h + 1], scalar2=None,
                                                                    op0=ALU.mult), [u[b][0].r, rc.r], [cat[b].r])
                yield
                for ec in range(8):
                    P.op("pe", lambda e, ec=ec, b=b: e.transpose(out=pT[:, ec, :], in_=cat[b][:, ec * 128:(ec + 1) * 128],
                                                                 identity=idt[:]), [cat[b].r, idt.r], [pT.r])
                P.op("act", lambda e: e.activation(out=catT[:], in_=pT[:], func=AF.Copy), [pT.r], [catT.r])
                for hf in range(2):
                    for ec in range(8):
                        P.op("pe", lambda e, hf=hf, ec=ec: e.matmul(out=pY[hf][:], lhsT=catT[:, ec, :],
                                                                   rhs=w_out[:, ec, hf * 512:(hf + 1) * 512],
                                                                   start=(ec == 0), stop=(ec == 7)), [catT.r, w_out.r], [pY[hf].r])
                    P.op("dve", lambda e, hf=hf, b=b, k_=k_: e.tensor_tensor(out=xo[k_][:, hf * 512:(hf + 1) * 512], in0=pY[hf][:],
                                                                      in1=xt[b][:, hf * 512:(hf + 1) * 512], op=ALU.add),
                         [pY[hf].r, xt[b].r], [xo[k_].r])
                P.dma("sp", X[rows, :], xo[k_][:], [xo[k_].r], [xr], ("xo", k_))
            run_interleaved([body(t) for t in range(NT)], lead=1)
            barrier(cx, tiles)

    if 'C' in _st:
        stageC()

def extra_inputs(S=4096):
    ce, off, ropeR, ropeD = make_consts_even(S)
    return {"ident": np.eye(128).astype(ml_dtypes.bfloat16), "cf32": CF32, "ce32": ce, "ropeR": ropeR, "ropeD": ropeD}


ALL_NAMES = list(INPUT_SHAPES.keys())


def kernel(**inputs):
    S = 4096
    phases = []
    for l in range(DEPTH):
        phases.append(("ffn", l, "pre"))
        phases.append(("mixA", l // 2) if l % 2 == 0 else ("gla", l // 2))
        phases.append(("ffn", l, "post"))
    phases.append(("final",))
    nc = build_program(S, phases, ALL_NAMES)
    x = np.ascontiguousarray(np.asarray(inputs["x"], dtype=np.float32))
    B = x.shape[0]
    shared = {n: np.ascontiguousarray(np.asarray(inputs[n], dtype=np.float32)) for n in ALL_NAMES}
    shared.update(extra_inputs(S))
    in_maps = []
    for b in range(B):
        m = {"x": x[b]}
        m.update(shared)
        in_maps.append(m)
    res = run_bass_kernel_spmd(nc, in_maps, core_ids=list(range(B)))
    return np.stack([np.asarray(r["out"], dtype=np.float32) for r in res.results], axis=0)
```

```python
import math
from contextlib import ExitStack

import numpy as np
import ml_dtypes
import concourse.bass as bass
import concourse.mybir as mybir
from concourse.bass_utils import run_bass_kernel_spmd

F32 = mybir.dt.float32
BF16 = mybir.dt.bfloat16
AF = mybir.ActivationFunctionType
ALU = mybir.AluOpType
AX = mybir.AxisListType

D_MODEL = 1024
FFN_HIDDEN = 2816
EPS = 1e-6
DEPTH = 4


class Res:
    __slots__ = ("name", "last_w", "readers", "dma_readers", "excl")

    def __init__(self, name, excl=False):
        self.name = name
        self.excl = excl
        self.last_w = None
        self.readers = {}
        self.dma_readers = []


class Op:
    __slots__ = ("eng", "fn", "deps", "sig", "val", "is_dma", "chan", "sem")

    def __init__(self, eng, fn, is_dma=False, chan=None):
        self.eng = eng
        self.fn = fn
        self.deps = []
        self.sig = False
        self.val = None
        self.is_dma = is_dma
        self.chan = chan
        self.sem = None


class Prog:
    ENGS = ("pe", "act", "dve", "pool", "sp")

    def __init__(self, nc):
        self.nc = nc
        self.ops = {e: [] for e in self.ENGS}
        self.chan_count = {}
        self.n_ops = 0

    def _add_dep(self, op, d, kind):
        if d is None or d is op:
            return
        if not d.is_dma and not op.is_dma and d.eng == op.eng:
            if op.eng == "pe":
                return
        if d.is_dma and op.is_dma and d.eng == op.eng and kind == "WAR":
            pass
        d.sig = True
        op.deps.append(d)

    def op(self, eng, fn, reads=(), writes=(), is_dma=False, chan=None):
        o = Op(eng, fn, is_dma, chan)
        writes = list(writes) + [r for r in reads if r.excl and r not in writes]
        reads = [r for r in reads if not r.excl]
        for r in reads:
            self._add_dep(o, r.last_w, "RAW")
        for w in writes:
            self._add_dep(o, w.last_w, "WAW")
            for rd in w.readers.values():
                self._add_dep(o, rd, "WAR")
            for rd in w.dma_readers:
                self._add_dep(o, rd, "WAR")
        for r in reads:
            if is_dma:
                r.dma_readers.append(o)
            else:
                r.readers[eng] = o
        for w in writes:
            w.last_w = o
            w.readers = {}
            w.dma_readers = []
        if is_dma:
            c = self.chan_count.get(chan, 0) + 1
            self.chan_count[chan] = c
            o.val = 16 * c
        self.ops[eng].append(o)
        self.n_ops += 1
        return o

    def wait_all(self, eng, resources):
        o = Op(eng, None)
        for w in resources:
            for d in [w.last_w] + list(w.readers.values()) + list(w.dma_readers):
                if d is None or (not d.is_dma and d.eng == eng and eng == "pe"):
                    continue
                d.sig = True
                o.deps.append(d)
        self.ops[eng].append(o)
        self.n_ops += 1
        return o

    def dma(self, queue, out_ap, in_ap, reads, writes, chan):
        def fn(e, out_ap=out_ap, in_ap=in_ap):
            return e.dma_start(out=out_ap, in_=in_ap)

        return self.op(queue, fn, reads, writes, is_dma=True, chan=chan)

    def emit(self):
        nc = self.nc
        with ExitStack() as es:
            eng_sem = {e: es.enter_context(nc.semaphore("s_" + e)) for e in self.ENGS}
            chan_sem = {}
            for c in self.chan_count:
                chan_sem[c] = es.enter_context(nc.semaphore("c_%d" % len(chan_sem)))
            for e in self.ENGS:
                cnt = 0
                for o in self.ops[e]:
                    if o.is_dma:
                        o.sem = chan_sem[o.chan]
                    else:
                        o.sem = eng_sem[e]
                        if o.sig:
                            cnt += 1
                            o.val = cnt
            block = es.enter_context(nc.Block())

            def emit_eng(eng_name, e):
                waited = {}
                for o in self.ops[eng_name]:
                    need = {}
                    for d in o.deps:
                        k = id(d.sem)
                        if waited.get(k, 0) >= d.val:
                            continue
                        if k not in need or need[k][1] < d.val:
                            need[k] = (d.sem, d.val)
                    for k, (sem, val) in need.items():
                        e.wait_ge(sem, val)
                        waited[k] = val
                    if o.fn is None:
                        assert not o.sig
                    else:
                        ins = o.fn(e)
                        if o.is_dma:
                            ins.then_inc(o.sem, 16)
                        elif o.sig:
                            ins.then_inc(o.sem, 1)

            @block.tensor
            def _(e):
                emit_eng("pe", e)

            @block.scalar
            def _(e):
                emit_eng("act", e)

            @block.vector
            def _(e):
                emit_eng("dve", e)

            @block.gpsimd
            def _(e):
                emit_eng("pool", e)

            @block.sync
            def _(e):
                emit_eng("sp", e)


class Ctx:
    def __init__(self, nc, S):
        self.nc = nc
        self.S = S
        self.P = Prog(nc)
        self.NT = S // 128
        self.res_cache = {}
        self.dbg32 = None
        self.dbg16 = None
        self.dbg_off = {False: 0, True: 0}
        self.dbg_map = {}

    def dbg(self, tile, ap2d, name):
        if self.dbg32 is None:
            return
        is16 = ap2d.dtype == BF16
        d = self.dbg16 if is16 else self.dbg32
        off = self.dbg_off[is16]
        p, n = ap2d.shape
        self.dbg_map[name] = (is16, off, p, n)
        self.dbg_off[is16] = off + n
        self.P.dma("sp", d[0:p, off:off + n], ap2d, [tile.r], [self.R("dbgout")], ("dbg", 0))

    def R(self, name):
        r = self.res_cache.get(name)
        if r is None:
            r = Res(name)
            self.res_cache[name] = r
        return r


class Tile:
    def __init__(self, t, name, excl=False):
        self.t = t
        self.r = Res(name, excl)

    def __getitem__(self, k):
        return self.t[k]


def phase_alloc(cx, es):
    nc = cx.nc
    cnt = [0]

    def sb(name, shape, dt):
        cnt[0] += 1
        return Tile(es.enter_context(nc.sbuf_tensor("%s_%d" % (name, cx.P.n_ops), shape, dt)), name)

    def ps(name, shape, dt):
        cnt[0] += 1
        return Tile(es.enter_context(nc.psum_tensor("%s_%d" % (name, cx.P.n_ops), shape, dt)), name, excl=True)

    return sb, ps


def barrier(cx, tiles):
    for eng in ("pe", "act", "dve", "pool", "sp"):
        cx.P.wait_all(eng, [t.r for t in tiles])


def load_gain_bc(cx, g_bc, g_row_ap):
    cx.P.dma("sp", g_bc[:], g_row_ap.partition_broadcast(128), [], [g_bc.r], ("g", g_bc.r.name))


def rms_prep(cx, xt, junk, ssq, rstd, xn, g_bc):
    P = cx.P
    P.op("act", lambda e: e.activation(out=junk[:], in_=xt[:], func=AF.Square, accum_out=ssq[:]),
         [xt.r], [junk.r, ssq.r])
    P.op("act", lambda e: e.activation(out=rstd[:], in_=ssq[:], func=AF.Sqrt, scale=1.0 / D_MODEL, bias=EPS),
         [ssq.r], [rstd.r])
    P.op("dve", lambda e: e.reciprocal(out=rstd[:], in_=rstd[:]), [rstd.r], [rstd.r])
    P.op("dve", lambda e: e.scalar_tensor_tensor(out=xn[:], in0=xt[:], scalar=rstd[:], in1=g_bc[:],
                                                 op0=ALU.mult, op1=ALU.mult),
         [xt.r, rstd.r, g_bc.r], [xn.r])


def phase_ffn(cx, X, g_row, wg_d, wu_d, wd_d, ident, Xin=None):
    nc, P, S = cx.nc, cx.P, cx.S
    if Xin is None:
        Xin = X
    TT = 256
    nt = S // TT
    NFC = FFN_HIDDEN // 128
    with ExitStack() as es:
        sb, ps = phase_alloc(cx, es)
        wg = sb("wg", [128, 8, FFN_HIDDEN], BF16)
        wu = sb("wu", [128, 8, FFN_HIDDEN], BF16)
        wd = sb("wd", [128, NFC, D_MODEL], BF16)
        g_bc = sb("g_bc", [128, D_MODEL], F32)
        xt = [[sb("xt%d%d" % (b, a), [128, D_MODEL], F32) for a in range(2)] for b in range(2)]
        junk = sb("junk", [128, D_MODEL], BF16)
        ssq = [[sb("ssq%d%d" % (b, a), [128, 1], F32) for a in range(2)] for b in range(2)]
        rstd = [[sb("rstd%d%d" % (b, a), [128, 1], F32) for a in range(2)] for b in range(2)]
        xn = [sb("xn%d" % a, [128, D_MODEL], BF16) for a in range(2)]
        xnT = [sb("xnT%d" % b, [128, 8, TT], BF16) for b in range(2)]
        sg = [sb("sg%d" % i, [128, TT], F32) for i in range(2)]
        hT = [sb("hT%d" % i, [128, TT], BF16) for i in range(3)]
        xo = [sb("xo%d" % a, [128, D_MODEL], F32) for a in range(2)]
        pT = [ps("pT%d" % a, [128, 8, 128], BF16) for a in range(2)]
        pGU = [ps("pGU%d" % i, [128, 512], F32) for i in range(2)]
        pO = [[ps("pO%d%d" % (a, h), [128, 512], F32) for h in range(2)] for a in range(2)]
        idt = sb("idt", [128, 128], BF16)
        tiles_extra = []
        all_tiles = ([wg, wu, wd, g_bc, junk, idt] + sum(xt, []) + sum(ssq, []) + sum(rstd, []) + xn + xnT + sg
                     + hT + xo + pT + pGU + sum(pO, []))

        P.dma("sp", idt[:], ident, [], [idt.r], ("id", 0))
        load_gain_bc(cx, g_bc, g_row)
        wgv = wg_d.rearrange("(kc p) f -> p kc f", p=128)
        wuv = wu_d.rearrange("(kc p) f -> p kc f", p=128)
        wdv = wd_d.rearrange("(fc p) d -> p fc d", p=128)
        FH = FFN_HIDDEN // 2
        wres = {}
        for fh in range(2):
            for (dst, src, nm) in ((wg, wgv, "wg"), (wu, wuv, "wu")):
                for kh in range(2):
                    r_ = Res("%s_%d_%d" % (nm, fh, kh))
                    wres[(nm, fh, kh)] = r_
                    tiles_extra.append(r_)
                    P.dma("pool", dst[:, kh * 4:(kh + 1) * 4, fh * FH:(fh + 1) * FH],
                          src[:, kh * 4:(kh + 1) * 4, fh * FH:(fh + 1) * FH], [], [r_], ("w", nm, fh, kh))
            r_ = Res("wd_%d" % fh)
            wres[("wd", fh)] = r_
            tiles_extra.append(r_)
            P.dma("pool", wd[:, fh * 11:(fh + 1) * 11, :], wdv[:, fh * 11:(fh + 1) * 11, :], [], [r_], ("w", "wd", fh))

        def xres(t, a):
            return cx.R("X%d" % (t * 2 + a))

        def prep_load(t):
            b = t % 2
            for a in range(2):
                r0 = t * TT + a * 128
                P.dma("sp", xt[b][a][:], Xin[r0:r0 + 128, :], [xres(t, a)], [xt[b][a].r], ("xt", b, a))

        def prep_norm(t):
            b = t % 2
            for a in range(2):
                rms_prep(cx, xt[b][a], junk, ssq[b][a], rstd[b][a], xn[a], g_bc)

        def prep_T(t):
            b = t % 2
            for a in range(2):
                for kc in range(8):
                    P.op("pe", lambda e, a=a, kc=kc: e.transpose(out=pT[a][:, kc, :],
                                                                 in_=xn[a][:, kc * 128:(kc + 1) * 128],
                                                                 identity=idt[:]),
                         [xn[a].r, idt.r], [pT[a].r])
                P.op("act", lambda e, a=a, b=b: e.activation(out=xnT[b][:, :, a * 128:(a + 1) * 128], in_=pT[a][:],
                                                             func=AF.Copy),
                     [pT[a].r], [xnT[b].r])

        def gu(t, fc):
            b = t % 2
            p = pGU[fc % 2]
            for (w_, off, nm) in ((wg, 0, "wg"), (wu, TT, "wu")):
                for kc in range(8):
                    P.op("pe", lambda e, w_=w_, off=off, kc=kc, p=p, b=b, fc=fc: e.matmul(
                        out=p[:, off:off + TT], lhsT=w_[:, kc, fc * 128:(fc + 1) * 128], rhs=xnT[b][:, kc, :],
                        start=(kc == 0), stop=(kc == 7)), [wres[(nm, fc // 11, kc // 4)], xnT[b].r], [p.r])

        def actmul(t, fc):
            p = pGU[fc % 2]
            s_ = sg[fc % 2]
            h_ = hT[fc % 3]
            P.op("act", lambda e, p=p, s_=s_: e.activation(out=s_[:], in_=p[:, 0:TT], func=AF.Silu), [p.r], [s_.r])
            P.op("dve", lambda e, p=p, s_=s_, h_=h_: e.tensor_tensor(out=h_[:], in0=s_[:], in1=p[:, TT:2 * TT],
                                                                    op=ALU.mult), [s_.r, p.r], [h_.r])

        def down(t, fc):
            h_ = hT[fc % 3]
            for a in range(2):
                for hf in range(2):
                    P.op("pe", lambda e, a=a, hf=hf, h_=h_, fc=fc: e.matmul(
                        out=pO[a][hf][:], lhsT=h_[:, a * 128:(a + 1) * 128], rhs=wd[:, fc, hf * 512:(hf + 1) * 512],
                        start=(fc == 0), stop=(fc == NFC - 1)), [h_.r, wres[("wd", fc // 11)]], [pO[a][hf].r])

        def finish(t):
            b = t % 2
            for a in range(2):
                for hf in range(2):
                    P.op("dve", lambda e, a=a, hf=hf, b=b: e.scalar_tensor_tensor(
                        out=xo[a][:, hf * 512:(hf + 1) * 512], in0=pO[a][hf][:], scalar=0.5,
                        in1=xt[b][a][:, hf * 512:(hf + 1) * 512], op0=ALU.mult, op1=ALU.add),
                        [pO[a][hf].r, xt[b][a].r], [xo[a].r])
                r0 = t * TT + a * 128
                P.dma("sp", X[r0:r0 + 128, :], xo[a][:], [xo[a].r], [xres(t, a)], ("xo", a))

        prep_load(0)
        prep_norm(0)
        prep_T(0)
        for t in range(nt):
            for fc in range(NFC):
                gu(t, fc)
                if t + 1 < nt:
                    if fc == 0:
                        prep_load(t + 1)
                    if fc == 5:
                        prep_norm(t + 1)
                    if fc == 13:
                        prep_T(t + 1)
                actmul(t, fc)
                if fc >= 1:
                    down(t, fc - 1)
            down(t, NFC - 1)
            finish(t)
        for eng in ("pe", "act", "dve", "pool", "sp"):
            cx.P.wait_all(eng, [t_.r for t_ in all_tiles] + tiles_extra)


def phase_final(cx, X, g_row, OUT):
    nc, P, S = cx.nc, cx.P, cx.S
    with ExitStack() as es:
        sb, ps = phase_alloc(cx, es)
        g_bc = sb("g_bc", [128, D_MODEL], F32)
        xt = [sb("xt%d" % b, [128, D_MODEL], F32) for b in range(4)]
        junk = sb("junk", [128, D_MODEL], BF16)
        ssq = [sb("ssq%d" % b, [128, 1], F32) for b in range(2)]
        rstd = [sb("rstd%d" % b, [128, 1], F32) for b in range(2)]
        xo = [sb("xo%d" % b, [128, D_MODEL], F32) for b in range(2)]
        all_tiles = [g_bc, junk] + xt + ssq + rstd + xo
        load_gain_bc(cx, g_bc, g_row)
        outs = []
        def loads(tt):
            P.dma("sp", xt[tt % 4][:], X[tt * 128:(tt + 1) * 128, :], [cx.R("X%d" % tt)], [xt[tt % 4].r], ("xt", tt % 4))

        for t in range(cx.NT):
            b = t % 2
            if t == 0:
                loads(0)
                loads(1)
            if t + 2 < cx.NT:
                loads(t + 2)
            rms_prep(cx, xt[t % 4], junk, ssq[b], rstd[b], xo[b], g_bc)
            ro = cx.R("OUT%d" % t)
            outs.append(ro)
            P.dma("sp", OUT[t * 128:(t + 1) * 128, :], xo[b][:], [xo[b].r], [ro], ("xo", b))
        P.op("sp", None, outs, [])
        barrier(cx, all_tiles)


INPUT_SHAPES = {
    "ffn_pre_norm": [4, 1024], "ffn_pre_w_gate": [4, 1024, 2816], "ffn_pre_w_up": [4, 1024, 2816],
    "ffn_pre_w_down": [4, 2816, 1024], "mix_norm": [4, 1024], "ab_w_in": [2, 1024, 3584],
    "ab_w_out": [2, 1024, 1024], "gla_w_in": [2, 1024, 3088], "gla_w_a2": [2, 16, 512], "gla_b_a": [2, 512],
    "gla_w_out": [2, 1024, 1024], "ffn_post_norm": [4, 1024], "ffn_post_w_gate": [4, 1024, 2816],
    "ffn_post_w_up": [4, 1024, 2816], "ffn_post_w_down": [4, 2816, 1024], "final_norm": [1024],
}


def build_program(S, phases, names):
    return _build_program(S, phases, names)[0]


def _build_program(S, phases, names):
    nc = bass.Bass("TRN2", target_bir_lowering=False)
    x_in = nc.dram_tensor("x", [S, D_MODEL], F32, kind="ExternalInput").ap()
    ident = nc.dram_tensor("ident", [128, 128], BF16, kind="ExternalInput").ap()
    cf_d = nc.dram_tensor("cf32", list(CF32.shape), F32, kind="ExternalInput").ap()
    ce_d = nc.dram_tensor("ce32", list(CE32.shape), F32, kind="ExternalInput").ap()
    ropeR_d = nc.dram_tensor("ropeR", [S, 512], F32, kind="ExternalInput").ap()
    ropeD_d = nc.dram_tensor("ropeD", [S, 128], F32, kind="ExternalInput").ap()
    scr = {
        "QD": nc.dram_tensor("qd", [S, 512], BF16, kind="Internal").ap(),
        "KD": nc.dram_tensor("kd", [S, 512], BF16, kind="Internal").ap(),
        "VD": nc.dram_tensor("vd", [S, 520], BF16, kind="Internal").ap(),
        "ORD": nc.dram_tensor("ord", [S, 512], BF16, kind="Internal").ap(),
        "UB": [nc.dram_tensor("ub%d" % i, [S, 520], F32, kind="Internal").ap() for i in range(3)],
    }
    W = {n: nc.dram_tensor(n, INPUT_SHAPES[n], F32, kind="ExternalInput").ap() for n in names}
    OUT = nc.dram_tensor("out", [S, D_MODEL], F32, kind="ExternalOutput").ap()
    cx = Ctx(nc, S)
    import os
    if os.environ.get("KDEBUG"):
        cx.dbg32 = nc.dram_tensor("dbg32", [128, 16384], F32, kind="ExternalOutput").ap()
        cx.dbg16 = nc.dram_tensor("dbg16", [128, 16384], BF16, kind="ExternalOutput").ap()
    X = x_in
    Xs = nc.dram_tensor("xs", [S, D_MODEL], F32, kind="Internal").ap()
    X = Xs
    first = [True]
    for ph in phases:
        if ph[0] == "ffn":
            l, which = ph[1], ph[2]
            phase_ffn(cx, X, W["ffn_%s_norm" % which][l], W["ffn_%s_w_gate" % which][l],
                      W["ffn_%s_w_up" % which][l], W["ffn_%s_w_down" % which][l], ident,
                      Xin=(x_in if first[0] else None))
            first[0] = False
        elif ph[0] == "copy":
            prev = None
            for t in range(cx.NT):
                cx.P.dma("sp", Xs[t * 128:(t + 1) * 128, :], x_in[t * 128:(t + 1) * 128, :],
                         [cx.R("xcopy_chain")], [cx.R("X%d" % t), cx.R("xcopy_chain")], ("xcopy", 0))
        elif ph[0] == "mixA":
            i = ph[1]
            phase_even(cx, X, W["mix_norm"][2 * i], W["ab_w_in"][i], W["ab_w_out"][i], ident, ce_d, ropeR_d, ropeD_d, scr)
        elif ph[0] == "gla":
            i = ph[1]
            l = 2 * i + 1
            phase_gla(cx, X, W["mix_norm"][l], W["gla_w_in"][i], W["gla_w_a2"][i], W["gla_b_a"][i], W["gla_w_out"][i],
                      ident, cf_d)
        elif ph[0] == "final":
            phase_final(cx, X, W["final_norm"], OUT)
        else:
            raise ValueError(ph)
    cx.P.emit()
    return nc, cx


def make_consts():
    cf = {}
    tok = np.arange(128)
    same64 = (tok[:, None] // 64) == (tok[None, :] // 64)
    cf["mc"] = np.where(same64 & (tok[:, None] <= tok[None, :]), -1.0 / 16, 0.0)
    cf["ms"] = np.where(same64 & (tok[:, None] > tok[None, :]), -1.0 / 16, 0.0)
    cf["mch"] = np.stack([np.where(tok < 64, -1.0 / 16, 0.0), np.where(tok >= 64, -1.0 / 16, 0.0)], 1)
    mT = np.where(same64 & (tok[None, :] >= tok[:, None]), 1.0, 0.0)
    cf["gmaskT"] = np.repeat(mT[:, None, :], 4, axis=1).reshape(128, 512)
    cf["mab"] = np.stack([np.where(tok < 64, 1.0, 0.0), np.where(tok >= 64, 1.0, 0.0)], 1)
    off = {}
    cols = []
    c0 = 0
    for k, v in cf.items():
        v = np.asarray(v, np.float32).reshape(128, -1)
        off[k] = (c0, v.shape[1])
        cols.append(v)
        c0 += v.shape[1]
    return np.ascontiguousarray(np.concatenate(cols, 1)), off


CF32, CF_OFF = make_consts()


GLA_LEAD = 10 ** 9


def run_interleaved(bodies, lead):
    active = []
    it = iter(bodies)
    nxt = next(it, None)
    while active or nxt is not None:
        if nxt is not None and (len(active) == 0 or (len(active) == 1 and active[0][1] >= lead)):
            active.append([nxt, 0])
            nxt = next(it, None)
        for a_ in list(active):
            try:
                next(a_[0])
                a_[1] += 1
            except StopIteration:
                active.remove(a_)


def phase_gla(cx, X, g_row, w_in_d, w_a2_d, b_a_d, w_out_d, ident, cf_d):
    nc, P, S = cx.nc, cx.P, cx.S
    H, DK, DV = 4, 128, 256
    GIN = 3088
    with ExitStack() as es:
        sb, ps = phase_alloc(cx, es)
        tiles = []

        def SB(name, shape, dt):
            t = sb(name, shape, dt)
            tiles.append(t)
            return t

        def PS(name, shape, dt):
            t = ps(name, shape, dt)
            tiles.append(t)
            return t

        w_in = SB("w_in", [128, 8, GIN], BF16)
        w_out = SB("w_out", [128, 8, D_MODEL], BF16)
        wa2 = SB("wa2", [17, 512], F32)
        cf = SB("cf", [128, CF32.shape[1]], F32)
        idt = SB("idt", [128, 128], BF16)
        g_bc = SB("g_bc", [128, D_MODEL], F32)
        xt = [SB("xt%d" % b, [128, D_MODEL], F32) for b in range(4)]
        junk = SB("junk", [128, D_MODEL], BF16)
        ssq_2 = [SB("ssq_%d" % i_, [128, 1], F32) for i_ in range(2)]
        rstd_2 = [SB("rstd_%d" % i_, [128, 1], F32) for i_ in range(2)]
        xn_2 = [SB("xn_%d" % i_, [128, D_MODEL], BF16) for i_ in range(2)]
        xnT_2 = [SB("xnT_%d" % i_, [128, 8, 128], BF16) for i_ in range(2)]
        alT_2 = [SB("alT_%d" % i_, [17, 128], F32) for i_ in range(2)]
        e1_2 = [SB("e1_%d" % i_, [128, 512], F32) for i_ in range(2)]
        l1_2 = [SB("l1_%d" % i_, [128, 512], F32) for i_ in range(2)]
        E1_2 = [SB("E1_%d" % i_, [128, 512], F32) for i_ in range(2)]
        E2_2 = [SB("E2_%d" % i_, [128, 512], F32) for i_ in range(2)]
        E3_2 = [SB("E3_%d" % i_, [128, 512], F32) for i_ in range(2)]
        qt_2 = [SB("qt_%d" % i_, [128, 512], BF16) for i_ in range(2)]
        kt_2 = [SB("kt_%d" % i_, [128, 512], BF16) for i_ in range(2)]
        kd_2 = [[SB("kd%d_%d" % (c, i_), [128, 512], BF16) for c in range(2)] for i_ in range(2)]
        vb_2 = [SB("vb_%d" % i_, [128, 1024], BF16) for i_ in range(2)]
        sr_2 = [SB("sr_%d" % i_, [128, 1024], F32) for i_ in range(2)]
        qTf_2 = [SB("qTf_%d" % i_, [128, 4, 128], BF16) for i_ in range(2)]
        qT2_2 = [SB("qT2_%d" % i_, [128, 4, 2, 128], BF16) for i_ in range(2)]
        kT_2 = [SB("kT_%d" % i_, [128, 4, 128], BF16) for i_ in range(2)]
        sT_2 = [SB("sT_%d" % i_, [128, 4, 128], BF16) for i_ in range(2)]
        st = [SB("st%d" % i, [128, 4, 256], F32) for i in range(2)]
        sbf = [SB("sbf%d" % i, [128, 4, 256], BF16) for i in range(3)]
        dec_2 = [SB("dec_%d" % i_, [128, 4, 2], F32) for i_ in range(2)]
        bst_2 = [SB("bst_%d" % i_, [128, 4, 6], F32) for i_ in range(2)]
        mv_2 = [SB("mv_%d" % i_, [128, 4, 2], F32) for i_ in range(2)]
        hr_2 = [SB("hr_%d" % i_, [128, 4], F32) for i_ in range(2)]
        tmp_2 = [SB("tmp_%d" % i_, [128, 1024], F32) for i_ in range(2)]
        og_2 = [SB("og_%d" % i_, [128, 1024], BF16) for i_ in range(2)]
        ogT_2 = [SB("ogT_%d" % i_, [128, 8, 128], BF16) for i_ in range(2)]
        xo_2 = [SB("xo_%d" % i_, [128, D_MODEL], F32) for i_ in range(2)]

        pT = PS("pT", [128, 8, 128], BF16)
        pP = [PS("pP%d" % i, [128, 512], F32) for i in range(2)]
        pB = [PS("pB%d" % i, [128, 512], F32) for i in range(2)]
        pSm = PS("pSm", [128, 512], F32)
        pTq = PS("pTq", [128, 2, 4, 128], BF16)
        pS = PS("pS", [128, 4, 128], F32)

        def cfv(name):
            o, w = CF_OFF[name]
            return cf[:, o:o + w]

        P.dma("sp", idt[:], ident, [], [idt.r], ("id", 0))
        P.dma("sp", cf[:], cf_d, [], [cf.r], ("cf", 0))
        load_gain_bc(cx, g_bc, g_row)
        P.dma("sp", wa2[0:16, :], w_a2_d, [], [wa2.r], ("wa2", 0))
        P.dma("sp", wa2[16:17, :], b_a_d.rearrange("(o f) -> o f", o=1), [], [wa2.r], ("wa2", 0))
        wv = w_in_d.rearrange("(kc p) f -> p kc f", p=128)
        for kh in range(2):
            for (c0, c1) in ((0, 1544), (1544, GIN)):
                P.dma("pool", w_in[:, kh * 4:(kh + 1) * 4, c0:c1], wv[:, kh * 4:(kh + 1) * 4, c0:c1], [], [w_in.r],
                      ("w", "w_in"))
        wov = w_out_d.rearrange("(kc p) f -> p kc f", p=128)
        for kh in range(2):
            P.dma("pool", w_out[:, kh * 4:(kh + 1) * 4, :], wov[:, kh * 4:(kh + 1) * 4, :], [], [w_out.r], ("w", "w_out"))
        for alT in alT_2:
            P.op("dve", lambda e, alT=alT: e.memset(alT[:], 1.0), [], [alT.r])
        P.op("dve", lambda e: e.memset(st[0][:], 0.0), [], [st[0].r])
        P.op("dve", lambda e: e.memset(sbf[0][:], 0.0), [], [sbf[0].r])
        for qT2 in qT2_2:
            P.op("pool", lambda e, qT2=qT2: e.memset(qT2[:], 0.0), [], [qT2.r])

        sc = DK ** -0.5
        def loads(tt):
            bb = tt % 4
            P.dma("sp", xt[bb][:], X[tt * 128:(tt + 1) * 128, :], [cx.R("X%d" % tt)], [xt[bb].r], ("xt", bb))

        def body(t):
            b = t % 4
            k_ = t % 2
            ssq = ssq_2[k_]
            rstd = rstd_2[k_]
            xn = xn_2[k_]
            xnT = xnT_2[k_]
            alT = alT_2[k_]
            e1 = e1_2[k_]
            l1 = l1_2[k_]
            E1 = E1_2[k_]
            E2 = E2_2[k_]
            E3 = E3_2[k_]
            qt = qt_2[k_]
            kt = kt_2[k_]
            vb = vb_2[k_]
            sr = sr_2[k_]
            qTf = qTf_2[k_]
            qT2 = qT2_2[k_]
            kT = kT_2[k_]
            sT = sT_2[k_]
            dec = dec_2[k_]
            bst = bst_2[k_]
            mv = mv_2[k_]
            hr = hr_2[k_]
            tmp = tmp_2[k_]
            og = og_2[k_]
            ogT = ogT_2[k_]
            xo = xo_2[k_]
            kd = kd_2[k_]
            xr = cx.R("X%d" % t)
            if t == 0:
                loads(0)
                loads(1)
            if t + 2 < cx.NT:
                loads(t + 2)
            rms_prep(cx, xt[b], junk, ssq, rstd, xn, g_bc)
            for kc in range(8):
                P.op("pe", lambda e, kc=kc: e.transpose(out=pT[:, kc, :], in_=xn[:, kc * 128:(kc + 1) * 128],
                                                        identity=idt[:]), [xn.r, idt.r], [pT.r])
            P.op("act", lambda e: e.activation(out=xnT[:], in_=pT[:], func=AF.Copy), [pT.r], [xnT.r])
            yield
            for kc in range(8):
                P.op("pe", lambda e, kc=kc: e.matmul(out=pSm[0:16, 0:128], lhsT=w_in[:, kc, 3072:3088], rhs=xnT[:, kc, :],
                                                     start=(kc == 0), stop=(kc == 7)), [w_in.r, xnT.r], [pSm.r])
            P.op("act", lambda e: e.activation(out=alT[0:16, :], in_=pSm[0:16, 0:128], func=AF.Copy), [pSm.r], [alT.r])
            yield
            P.op("pe", lambda e: e.matmul(out=pB[0][:], lhsT=alT[:], rhs=wa2[:], start=True, stop=True),
                 [alT.r, wa2.r], [pB[0].r])
            P.op("act", lambda e: e.activation(out=e1[:], in_=pB[0][:], func=AF.Exp, scale=-1.0), [pB[0].r], [e1.r])
            P.op("act", lambda e: e.activation(out=l1[:], in_=e1[:], func=AF.Ln, bias=1.0), [e1.r], [l1.r])
            yield
            P.op("pe", lambda e: e.matmul(out=pB[0][:], lhsT=cfv("mc"), rhs=l1[:], start=True, stop=True),
                 [cf.r, l1.r], [pB[0].r])
            P.op("pe", lambda e: e.matmul(out=pB[1][:], lhsT=cfv("ms"), rhs=l1[:], start=True, stop=True),
                 [cf.r, l1.r], [pB[1].r])
            for h in range(H):
                P.op("pe", lambda e, h=h: e.matmul(out=pSm[:, 128 + 2 * h:130 + 2 * h], lhsT=l1[:, h * 128:(h + 1) * 128],
                                                   rhs=cfv("mch"), start=True, stop=True), [cf.r, l1.r], [pSm.r])
            P.op("act", lambda e: e.activation(out=E1[:], in_=pB[0][:], func=AF.Exp), [pB[0].r], [E1.r])
            P.op("act", lambda e: e.activation(out=E2[:], in_=pB[0][:], func=AF.Exp, scale=-1.0), [pB[0].r], [E2.r])
            P.op("act", lambda e: e.activation(out=E3[:], in_=pB[1][:], func=AF.Exp), [pB[1].r], [E3.r])
            P.op("act", lambda e: e.activation(out=dec[:].rearrange("p h c -> p (h c)"), in_=pSm[:, 128:136], func=AF.Exp),
                 [pSm.r], [dec.r])
            yield
            def proj(gi, pp):
                for kc in range(8):
                    P.op("pe", lambda e, kc=kc, gi=gi, pp=pp: e.matmul(out=pp[:], lhsT=xnT[:, kc, :],
                                                                       rhs=w_in[:, kc, gi * 512:(gi + 1) * 512],
                                                                       start=(kc == 0), stop=(kc == 7)),
                         [xnT.r, w_in.r], [pp.r])
            proj(0, pP[0])
            P.op("dve", lambda e: e.scalar_tensor_tensor(out=qt[:], in0=pP[0][:], scalar=sc, in1=E1[:], op0=ALU.mult,
                                                         op1=ALU.mult), [pP[0].r, E1.r], [qt.r])
            yield
            proj(1, pP[1])
            P.op("dve", lambda e: e.tensor_tensor(out=kt[:], in0=pP[1][:], in1=E2[:], op=ALU.mult), [pP[1].r, E2.r], [kt.r])
            for c in range(2):
                P.op("dve", lambda e, c=c: e.scalar_tensor_tensor(out=kd[c][:], in0=pP[1][:], scalar=cfv("mab")[:, c:c + 1],
                                                                  in1=E3[:], op0=ALU.mult, op1=ALU.mult),
                     [pP[1].r, E3.r, cf.r], [kd[c].r])
            for gi in (2, 3):
                yield
                pp = pP[gi % 2]
                proj(gi, pp)
                P.op("act", lambda e, gi=gi, pp=pp: e.activation(out=vb[:, (gi - 2) * 512:(gi - 1) * 512], in_=pp[:],
                                                                 func=AF.Copy), [pp.r], [vb.r])
            for gi in (4, 5):
                yield
                pp = pP[gi % 2]
                proj(gi, pp)
                P.op("act", lambda e, gi=gi, pp=pp: e.activation(out=sr[:, (gi - 4) * 512:(gi - 3) * 512], in_=pp[:],
                                                                 func=AF.Silu), [pp.r], [sr.r])
            yield
            for h in range(H):
                P.op("pe", lambda e, h=h: e.transpose(out=pTq[:, 0, h, :], in_=qt[:, h * 128:(h + 1) * 128], identity=idt[:]),
                     [qt.r, idt.r], [pTq.r])
            for h in range(H):
                P.op("pe", lambda e, h=h: e.transpose(out=pTq[:, 1, h, :], in_=kt[:, h * 128:(h + 1) * 128], identity=idt[:]),
                     [kt.r, idt.r], [pTq.r])
            P.op("act", lambda e: e.activation(out=qTf[:], in_=pTq[:, 0, :, :], func=AF.Copy), [pTq.r], [qTf.r])
            P.op("dve", lambda e: e.tensor_copy(out=kT[:], in_=pTq[:, 1, :, :]), [pTq.r], [kT.r])
            P.op("pool", lambda e: e.tensor_copy(out=qT2[:, :, 0, 0:64], in_=qTf[:, :, 0:64]), [qTf.r], [qT2.r])
            P.op("pool", lambda e: e.tensor_copy(out=qT2[:, :, 1, 64:128], in_=qTf[:, :, 64:128]), [qTf.r], [qT2.r])
            yield
            for h in range(H):
                P.op("pe", lambda e, h=h: e.matmul(out=pS[:, h, :], lhsT=kT[:, h, :], rhs=qTf[:, h, :], start=True, stop=True),
                     [kT.r, qTf.r], [pS.r])
            P.op("dve", lambda e: e.tensor_tensor(out=sT[:].rearrange("p h i -> p (h i)"),
                                                  in0=pS[:].rearrange("p h i -> p (h i)"), in1=cfv("gmaskT"), op=ALU.mult),
                 [pS.r, cf.r], [sT.r])
            yield
            b0, b1, b2 = (2 * t) % 3, (2 * t + 1) % 3, (2 * t + 2) % 3
            s0, s1 = 0, 1
            for h in range(H):
                pk = pB[h % 2]
                for c in range(2):
                    P.op("pe", lambda e, h=h, c=c, pk=pk: e.matmul(out=pk[:, c * 256:(c + 1) * 256],
                                                                  lhsT=kd[c][:, h * 128:(h + 1) * 128],
                                                                  rhs=vb[:, h * 256:(h + 1) * 256], start=True, stop=True),
                         [kd[c].r, vb.r], [pk.r])
                P.op("dve", lambda e, h=h, pk=pk, s0=s0, s1=s1: e.scalar_tensor_tensor(out=st[s1][:, h, :], in0=st[s0][:, h, :],
                                                                         scalar=dec[:, h, 0:1], in1=pk[:, 0:256],
                                                                         op0=ALU.mult, op1=ALU.add),
                     [st[s0].r, dec.r, pk.r], [st[s1].r])
                P.op("pool", lambda e, h=h, b1=b1, s1=s1: e.tensor_copy(out=sbf[b1][:, h, :], in_=st[s1][:, h, :]), [st[s1].r], [sbf[b1].r])
                P.op("dve", lambda e, h=h, pk=pk, s0=s0, s1=s1: e.scalar_tensor_tensor(out=st[s0][:, h, :], in0=st[s1][:, h, :],
                                                                         scalar=dec[:, h, 1:2], in1=pk[:, 256:512],
                                                                         op0=ALU.mult, op1=ALU.add),
                     [st[s1].r, dec.r, pk.r], [st[s0].r])
                P.op("pool", lambda e, h=h, b2=b2, s0=s0: e.tensor_copy(out=sbf[b2][:, h, :], in_=st[s0][:, h, :]), [st[s0].r], [sbf[b2].r])
            for h in range(H):
                po = pP[h // 2]
                osl = slice((h % 2) * 256, (h % 2 + 1) * 256)
                P.op("pe", lambda e, h=h, po=po, osl=osl, b0=b0, b1=b1: e.matmul(out=po[:, osl], lhsT=sT[:, h, :],
                                                                  rhs=vb[:, h * 256:(h + 1) * 256], start=True, stop=False),
                     [sT.r, vb.r], [po.r])
                P.op("pe", lambda e, h=h, po=po, osl=osl, b0=b0, b1=b1: e.matmul(out=po[:, osl], lhsT=qT2[:, h, 0, :],
                                                                  rhs=sbf[b0][:, h, :], start=False, stop=False),
                     [qT2.r, sbf[b0].r], [po.r])
                P.op("pe", lambda e, h=h, po=po, osl=osl, b0=b0, b1=b1: e.matmul(out=po[:, osl], lhsT=qT2[:, h, 1, :],
                                                                  rhs=sbf[b1][:, h, :], start=False, stop=True),
                     [qT2.r, sbf[b1].r], [po.r])
            for h in range(H):
                po = pP[h // 2]
                osl = slice((h % 2) * 256, (h % 2 + 1) * 256)
                P.op("dve", lambda e, h=h, po=po, osl=osl: e.bn_stats(out=bst[:, h, :], in_=po[:, osl]), [po.r], [bst.r])
                P.op("dve", lambda e, h=h: e.bn_aggr(out=mv[:, h, :], in_=bst[:, h, :]), [bst.r], [mv.r])
            P.op("act", lambda e: e.activation(out=hr[:], in_=mv[:, :, 1], func=AF.Sqrt, bias=EPS), [mv.r], [hr.r])
            P.op("dve", lambda e: e.reciprocal(out=hr[:], in_=hr[:]), [hr.r], [hr.r])
            for h in range(H):
                po = pP[h // 2]
                osl = slice((h % 2) * 256, (h % 2 + 1) * 256)
                P.op("dve", lambda e, h=h, po=po, osl=osl: e.tensor_scalar(out=tmp[:, h * 256:(h + 1) * 256], in0=po[:, osl],
                                                                          scalar1=mv[:, h, 0:1], scalar2=hr[:, h:h + 1],
                                                                          op0=ALU.subtract, op1=ALU.mult),
                     [po.r, mv.r, hr.r], [tmp.r])
            P.op("pool", lambda e: e.tensor_tensor(out=og[:], in0=tmp[:], in1=sr[:], op=ALU.mult), [tmp.r, sr.r], [og.r])
            yield
            for ec in range(8):
                P.op("pe", lambda e, ec=ec: e.transpose(out=pT[:, ec, :], in_=og[:, ec * 128:(ec + 1) * 128], identity=idt[:]),
                     [og.r, idt.r], [pT.r])
            P.op("act", lambda e: e.activation(out=ogT[:], in_=pT[:], func=AF.Copy), [pT.r], [ogT.r])
            for hf in range(2):
                for ec in range(8):
                    P.op("pe", lambda e, hf=hf, ec=ec: e.matmul(out=pP[hf][:], lhsT=ogT[:, ec, :],
                                                               rhs=w_out[:, ec, hf * 512:(hf + 1) * 512],
                                                               start=(ec == 0), stop=(ec == 7)), [ogT.r, w_out.r], [pP[hf].r])
                P.op("dve", lambda e, hf=hf, b=b: e.tensor_tensor(out=xo[:, hf * 512:(hf + 1) * 512], in0=pP[hf][:],
                                                                  in1=xt[b][:, hf * 512:(hf + 1) * 512], op=ALU.add),
                     [pP[hf].r, xt[b].r], [xo.r])
            P.dma("sp", X[t * 128:(t + 1) * 128, :], xo[:], [xo.r], [xr], ("xo", k_))
        run_interleaved([body(t) for t in range(cx.NT)], lead=GLA_LEAD)
        barrier(cx, tiles)


RET_G = [1.0 - 2.0 ** (-5.0 - h) for h in range(4)]
DIL_PATTERNS = ((128, 1), (512, 4), (2048, 16))


def make_consts_even(S):
    tok = np.arange(128)
    cf = {}
    lg = [math.log(g) for g in RET_G]
    dm = np.zeros((128, 4, 128))
    gq = np.zeros((128, 4, 128))
    gk = np.zeros((128, 4, 128))
    for h in range(4):
        rel = tok[None, :] - tok[:, None]
        dm[:, h, :] = np.where(rel >= 0, np.exp(lg[h] * np.maximum(rel, 0)), 0.0)
        gq[:, h, :] = np.exp(lg[h] * (tok + 1.0))[:, None]
        gk[:, h, :] = np.exp(lg[h] * (127.0 - tok))[:, None]
    cf["dmT"] = dm.reshape(128, 512)
    cf["gq"] = gq.reshape(128, 512)
    cf["gk"] = gk.reshape(128, 512)
    prev = np.where(tok[:, None] >= tok[None, :], 1.0, 0.0)
    same = np.where(tok[None, :] >= tok[:, None], 1.0, 0.0)
    cf["dilm"] = np.stack([prev, same, prev, same], 1).reshape(128, 512)
    off = {}
    cols = []
    c0 = 0
    for k, v in cf.items():
        v = np.asarray(v, np.float32).reshape(128, -1)
        off[k] = (c0, v.shape[1])
        cols.append(v)
        c0 += v.shape[1]
    ce = np.ascontiguousarray(np.concatenate(cols, 1))
    pos = np.arange(S, dtype=np.float32)
    fr = (np.float32(10000.0) ** (-np.linspace(0.0, 1.0, 64, dtype=np.float32))).astype(np.float32)
    ang = pos[:, None] * fr[None, :]
    ropeR = np.concatenate([np.tile(np.cos(ang), (1, 4)), np.tile(np.sin(ang), (1, 4))], 1).astype(np.float32)
    fd = (np.float32(500000.0) ** (-np.arange(0, 16, 2, dtype=np.float32) / np.float32(16))).astype(np.float32)
    angd = pos[:, None] * fd[None, :]
    ropeD = np.concatenate([np.tile(np.cos(angd), (1, 8)), np.tile(np.sin(angd), (1, 8))], 1).astype(np.float32)
    return ce, off, np.ascontiguousarray(ropeR), np.ascontiguousarray(ropeD)


CE32, CE_OFF, _, _ = make_consts_even(128)


def phase_even(cx, X, g_row, w_in_d, w_out_d, ident, ce_d, ropeR_d, ropeD_d, scr):
    nc, P, S = cx.nc, cx.P, cx.S
    NT = cx.NT
    EIN = 3584
    QD, KD, VD, ORD, UB = scr["QD"], scr["KD"], scr["VD"], scr["ORD"], scr["UB"]
    def stageA():
        with ExitStack() as es:
            sb, ps = phase_alloc(cx, es)
            tiles = []

            def SB(name, shape, dt):
                t = sb(name, shape, dt)
                tiles.append(t)
                return t

            def PS(name, shape, dt):
                t = ps(name, shape, dt)
                tiles.append(t)
                return t

            w_in = SB("w_in", [128, 8, EIN], BF16)
            ce = SB("ce", [128, CE32.shape[1]], F32)
            idt = SB("idt", [128, 128], BF16)
            g_bc = SB("g_bc", [128, D_MODEL], F32)
            xt = [SB("xt%d" % b, [128, D_MODEL], F32) for b in range(4)]
            rR = [SB("rR%d" % b, [128, 512], F32) for b in range(4)]
            rD = [SB("rD%d" % b, [128, 128], F32) for b in range(4)]
            junk = SB("junk", [128, D_MODEL], BF16)
            ssq_2 = [SB("ssq_%d" % i_, [128, 1], F32) for i_ in range(2)]
            rstd_2 = [SB("rstd_%d" % i_, [128, 1], F32) for i_ in range(2)]
            xn_2 = [SB("xn_%d" % i_, [128, D_MODEL], BF16) for i_ in range(2)]
            xnT_2 = [SB("xnT_%d" % i_, [128, 8, 128], BF16) for i_ in range(2)]
            ta_2 = [SB("ta_%d" % i_, [128, 256], F32) for i_ in range(2)]
            tb_2 = [SB("tb_%d" % i_, [128, 256], F32) for i_ in range(2)]
            qr_2 = [SB("qr_%d" % i_, [128, 512], F32) for i_ in range(2)]
            kr_2 = [SB("kr_%d" % i_, [128, 512], F32) for i_ in range(2)]
            qb_2 = [SB("qb_%d" % i_, [128, 3, 512], BF16) for i_ in range(2)]
            kdec_2 = [SB("kdec_%d" % i_, [128, 512], BF16) for i_ in range(2)]
            vb_2 = [SB("vb_%d" % i_, [128, 512], BF16) for i_ in range(2)]
            sg_2 = [SB("sg_%d" % i_, [128, 512], F32) for i_ in range(2)]
            qkT_2 = [SB("qkT_%d" % i_, [128, 3, 4, 128], BF16) for i_ in range(2)]
            sT_2 = [SB("sT_%d" % i_, [128, 4, 128], BF16) for i_ in range(2)]
            st = SB("st", [128, 4, 128], F32)
            sbf = [SB("sbf%d" % i, [128, 4, 128], BF16) for i in range(2)]
            bst_2 = [SB("bst_%d" % i_, [128, 4, 6], F32) for i_ in range(2)]
            mv_2 = [SB("mv_%d" % i_, [128, 4, 2], F32) for i_ in range(2)]
            hr_2 = [SB("hr_%d" % i_, [128, 4], F32) for i_ in range(2)]
            tmp_2 = [SB("tmp_%d" % i_, [128, 512], F32) for i_ in range(2)]
            orb_2 = [SB("orb_%d" % i_, [128, 512], BF16) for i_ in range(2)]
            dq_2 = [SB("dq_%d" % i_, [128, 512], BF16) for i_ in range(2)]
            dk_2 = [SB("dk_%d" % i_, [128, 512], BF16) for i_ in range(2)]
            va_2 = [SB("va_%d" % i_, [128, 8, 65], BF16) for i_ in range(2)]
            da_2 = [SB("da_%d" % i_, [128, 64], F32) for i_ in range(2)]
            db_2 = [SB("db_%d" % i_, [128, 64], F32) for i_ in range(2)]
            prq_2 = [SB("prq_%d" % i_, [128, 512], F32) for i_ in range(2)]
            prk_2 = [SB("prk_%d" % i_, [128, 512], F32) for i_ in range(2)]
            drq_2 = [SB("drq_%d" % i_, [128, 128], F32) for i_ in range(2)]
            drk_2 = [SB("drk_%d" % i_, [128, 128], F32) for i_ in range(2)]

            pT = PS("pT", [128, 8, 128], BF16)
            pP = [PS("pP%d" % i, [128, 512], F32) for i in range(3)]
            pT3 = [PS("pT3%d" % i, [128, 8, 128], BF16) for i in range(1)]
            pS = PS("pS", [128, 4, 128], F32)
            pKV = PS("pKV", [128, 4, 128], F32)
            pO = PS("pO", [128, 4, 128], F32)

            def cev(name):
                o, w = CE_OFF[name]
                return ce[:, o:o + w]

            P.dma("sp", idt[:], ident, [], [idt.r], ("id", 0))
            P.dma("sp", ce[:], ce_d, [], [ce.r], ("ce", 0))
            load_gain_bc(cx, g_bc, g_row)
            wv = w_in_d.rearrange("(kc p) f -> p kc f", p=128)
            for kh in range(2):
                for (c0, c1) in ((0, 1792), (1792, EIN)):
                    P.dma("pool", w_in[:, kh * 4:(kh + 1) * 4, c0:c1], wv[:, kh * 4:(kh + 1) * 4, c0:c1], [], [w_in.r],
                          ("w", "w_in"))
            P.op("dve", lambda e: e.memset(st[:], 0.0), [], [st.r])
            P.op("dve", lambda e: e.memset(sbf[0][:], 0.0), [], [sbf[0].r])
            for va in va_2:
                P.op("pool", lambda e, va=va: e.memset(va[:], 1.0), [], [va.r])
            scK = 128 ** -0.5

            def proj(gi, pp, xnT):
                for kc in range(8):
                    P.op("pe", lambda e, kc=kc, gi=gi, pp=pp, xnT=xnT: e.matmul(out=pp[:], lhsT=xnT[:, kc, :],
                                                                       rhs=w_in[:, kc, gi * 512:(gi + 1) * 512],
                                                                       start=(kc == 0), stop=(kc == 7)),
                         [xnT.r, w_in.r], [pp.r])

            def rot(pp, rt, nh, hd, half, dst, scale, scratch):
                pv = pp[:].rearrange("p (h d) -> p h d", h=nh)
                x1 = pv[:, :, 0:half]
                x2 = pv[:, :, half:2 * half]
                n = nh * half
                cosv = rt[:, 0:n].rearrange("p (h d) -> p h d", h=nh)
                sinv = rt[:, n:2 * n].rearrange("p (h d) -> p h d", h=nh)
                A, B = scratch
                Av = A[:, 0:n].rearrange("p (h d) -> p h d", h=nh)
                Bv = B[:, 0:n].rearrange("p (h d) -> p h d", h=nh)
                dv = dst.rearrange("p (h d) -> p h d", h=nh)
                for (u, w_, sgn, lo) in ((x1, x2, ALU.subtract, 0), (x2, x1, ALU.add, half)):
                    P.op("dve", lambda e, u=u: e.scalar_tensor_tensor(out=Av, in0=u, scalar=scale, in1=cosv, op0=ALU.mult,
                                                                      op1=ALU.mult), [pp.r, rt_res[0]], [A.r])
                    P.op("dve", lambda e, w_=w_: e.scalar_tensor_tensor(out=Bv, in0=w_, scalar=scale, in1=sinv, op0=ALU.mult,
                                                                        op1=ALU.mult), [pp.r, rt_res[0]], [B.r])
                    P.op("pool", lambda e, sgn=sgn, lo=lo: e.tensor_tensor(out=dv[:, :, lo:lo + half], in0=Av, in1=Bv, op=sgn),
                         [A.r, B.r], [dst_res[0]])

            rt_res = [None]
            dst_res = [None]
            def loads(tt):
                bb = tt % 4
                P.dma("sp", xt[bb][:], X[tt * 128:(tt + 1) * 128, :], [cx.R("X%d" % tt)], [xt[bb].r], ("xt", bb))
                P.dma("sp", rR[bb][:], ropeR_d[tt * 128:(tt + 1) * 128, :], [], [rR[bb].r], ("rR", bb))
                P.dma("sp", rD[bb][:], ropeD_d[tt * 128:(tt + 1) * 128, :], [], [rD[bb].r], ("rD", bb))

            def body(t):
                b = t % 4
                k_ = t % 2
                ssq = ssq_2[k_]
                rstd = rstd_2[k_]
                xn = xn_2[k_]
                xnT = xnT_2[k_]
                ta = ta_2[k_]
                tb = tb_2[k_]
                qr = qr_2[k_]
                kr = kr_2[k_]
                qb = qb_2[k_]
                kdec = kdec_2[k_]
                vb = vb_2[k_]
                sg = sg_2[k_]
                qkT = qkT_2[k_]
                sT = sT_2[k_]
                bst = bst_2[k_]
                mv = mv_2[k_]
                hr = hr_2[k_]
                tmp = tmp_2[k_]
                orb = orb_2[k_]
                dq = dq_2[k_]
                dk = dk_2[k_]
                va = va_2[k_]
                da = da_2[k_]
                db = db_2[k_]
                prq = prq_2[k_]
                prk = prk_2[k_]
                drq = drq_2[k_]
                drk = drk_2[k_]
                if t == 0:
                    loads(0)
                    loads(1)
                if t + 2 < NT:
                    loads(t + 2)
                rms_prep(cx, xt[b], junk, ssq, rstd, xn, g_bc)
                for kc in range(8):
                    P.op("pe", lambda e, kc=kc: e.transpose(out=pT[:, kc, :], in_=xn[:, kc * 128:(kc + 1) * 128],
                                                            identity=idt[:]), [xn.r, idt.r], [pT.r])
                P.op("act", lambda e: e.activation(out=xnT[:], in_=pT[:], func=AF.Copy), [pT.r], [xnT.r])
                yield
                proj(0, pP[0], xnT)
                rt_res[0], dst_res[0] = rR[b].r, qr.r
                P.op("act", lambda e: e.activation(out=prq[:], in_=pP[0][:], func=AF.Copy), [pP[0].r], [prq.r])
                rot(prq, rR[b], 4, 128, 64, qr[:], 1.0, (ta, tb))
                P.op("act", lambda e: e.activation(out=qb[:, 0, :], in_=qr[:], func=AF.Copy), [qr.r], [qb.r])
                P.op("dve", lambda e: e.tensor_tensor(out=qb[:, 1, :], in0=qr[:], in1=cev("gq"), op=ALU.mult), [qr.r, ce.r], [qb.r])
                yield
                proj(1, pP[1], xnT)
                rt_res[0], dst_res[0] = rR[b].r, kr.r
                P.op("act", lambda e: e.activation(out=prk[:], in_=pP[1][:], func=AF.Copy), [pP[1].r], [prk.r])
                rot(prk, rR[b], 4, 128, 64, kr[:], scK, (ta, tb))
                P.op("act", lambda e: e.activation(out=qb[:, 2, :], in_=kr[:], func=AF.Copy), [kr.r], [qb.r])
                P.op("dve", lambda e: e.tensor_tensor(out=kdec[:], in0=kr[:], in1=cev("gk"), op=ALU.mult), [kr.r, ce.r], [kdec.r])
                yield
                proj(2, pP[2], xnT)
                P.op("act", lambda e: e.activation(out=vb[:], in_=pP[2][:], func=AF.Copy), [pP[2].r], [vb.r])
                proj(3, pP[0], xnT)
                P.op("act", lambda e: e.activation(out=sg[:], in_=pP[0][:], func=AF.Silu), [pP[0].r], [sg.r])
                yield
                for (gi, pp, dst, dram) in ((4, pP[1], dq, QD), (5, pP[2], dk, KD)):
                    if gi == 5:
                        yield
                    proj(gi, pp, xnT)
                    P.op("act", lambda e, pp=pp, dst=dst: e.activation(out=dst[:], in_=pp[:], func=AF.Copy), [pp.r], [dst.r])
                    rt_res[0], dst_res[0] = rD[b].r, dst.r
                    drw = drq if gi == 4 else drk
                    P.op("act", lambda e, pp=pp, drw=drw: e.activation(
                        out=drw[:].rearrange("p (h d) -> p h d", h=8),
                        in_=pp[:].rearrange("p (h d) -> p h d", h=8)[:, :, 0:16], func=AF.Copy), [pp.r], [drw.r])
                    rot(drw, rD[b], 8, 64, 8, dst[:], 1.0, (da, db))
                    P.dma("sp", dram[t * 128:(t + 1) * 128, :], dst[:], [dst.r], [cx.R("%s%d" % (dram.tensor.name, t))],
                          ("st", dst.r.name, k_))
                yield
                proj(6, pP[0], xnT)
                P.op("act", lambda e: e.activation(out=va[:, :, 0:64], in_=pP[0][:].rearrange("p (h d) -> p h d", h=8),
                                                   func=AF.Copy), [pP[0].r], [va.r])
                P.dma("sp", VD[t * 128:(t + 1) * 128, :], va[:].rearrange("p h d -> p (h d)"), [va.r], [cx.R("VD%d" % t)],
                      ("st", "va", k_))
                yield
                for j in range(3):
                    for h in range(4):
                        P.op("pe", lambda e, j=j, h=h: e.transpose(out=pT3[0][:, h, :], in_=qb[:, j, h * 128:(h + 1) * 128],
                                                                   identity=idt[:]), [qb.r, idt.r], [pT3[0].r])
                    P.op("act", lambda e, j=j: e.activation(out=qkT[:, j, :, :], in_=pT3[0][:, 0:4, :], func=AF.Copy), [pT3[0].r], [qkT.r])
                for h in range(4):
                    P.op("pe", lambda e, h=h: e.matmul(out=pS[:, h, :], lhsT=qkT[:, 2, h, :], rhs=qkT[:, 0, h, :], start=True,
                                                       stop=True), [qkT.r], [pS.r])
                P.op("dve", lambda e: e.tensor_tensor(out=sT[:].rearrange("p h i -> p (h i)"),
                                                      in0=pS[:].rearrange("p h i -> p (h i)"), in1=cev("dmT"), op=ALU.mult),
                     [pS.r, ce.r], [sT.r])
                yield
                sp, sn = sbf[t % 2], sbf[(t + 1) % 2]
                for h in range(4):
                    P.op("pe", lambda e, h=h: e.matmul(out=pKV[:, h, :], lhsT=kdec[:, h * 128:(h + 1) * 128],
                                                       rhs=vb[:, h * 128:(h + 1) * 128], start=True, stop=True),
                         [kdec.r, vb.r], [pKV.r])
                for h in range(4):
                    P.op("pe", lambda e, h=h: e.matmul(out=pO[:, h, :], lhsT=sT[:, h, :], rhs=vb[:, h * 128:(h + 1) * 128],
                                                       start=True, stop=False), [sT.r, vb.r], [pO.r])
                    P.op("pe", lambda e, h=h, sp=sp: e.matmul(out=pO[:, h, :], lhsT=qkT[:, 1, h, :], rhs=sp[:, h, :],
                                                              start=False, stop=True), [qkT.r, sp.r], [pO.r])
                for h in range(4):
                    P.op("dve", lambda e, h=h: e.scalar_tensor_tensor(out=st[:, h, :], in0=st[:, h, :], scalar=RET_G[h] ** 128,
                                                                      in1=pKV[:, h, :], op0=ALU.mult, op1=ALU.add),
                         [st.r, pKV.r], [st.r])
                P.op("pool", lambda e, sn=sn: e.tensor_copy(out=sn[:], in_=st[:]), [st.r], [sn.r])
                for h in range(4):
                    P.op("dve", lambda e, h=h: e.bn_stats(out=bst[:, h, :], in_=pO[:, h, :]), [pO.r], [bst.r])
                    P.op("dve", lambda e, h=h: e.bn_aggr(out=mv[:, h, :], in_=bst[:, h, :]), [bst.r], [mv.r])
                P.op("act", lambda e: e.activation(out=hr[:], in_=mv[:, :, 1], func=AF.Sqrt, bias=EPS), [mv.r], [hr.r])
                P.op("dve", lambda e: e.reciprocal(out=hr[:], in_=hr[:]), [hr.r], [hr.r])
                for h in range(4):
                    P.op("dve", lambda e, h=h: e.tensor_scalar(out=tmp[:, h * 128:(h + 1) * 128], in0=pO[:, h, :],
                                                               scalar1=mv[:, h, 0:1], scalar2=hr[:, h:h + 1],
                                                               op0=ALU.subtract, op1=ALU.mult), [pO.r, mv.r, hr.r], [tmp.r])
                P.op("pool", lambda e: e.tensor_tensor(out=orb[:], in0=tmp[:], in1=sg[:], op=ALU.mult), [tmp.r, sg.r], [orb.r])
                P.dma("sp", ORD[t * 128:(t + 1) * 128, :], orb[:], [orb.r], [cx.R("ORD%d" % t)], ("st", "orb", k_))
            run_interleaved([body(t) for t in range(NT)], lead=5)
            barrier(cx, tiles)

    import os
    _st = os.environ.get('EVEN_STAGES', 'ABC')
    if 'A' in _st:
        stageA()
    def stageB():
        with ExitStack() as es:
            sb, ps = phase_alloc(cx, es)
            tiles = []

            def SB(name, shape, dt):
                t = sb(name, shape, dt)
                tiles.append(t)
                return t

            def PS(name, shape, dt):
                t = ps(name, shape, dt)
                tiles.append(t)
                return t

            ce = SB("ce", [128, CE32.shape[1]], F32)
            idt = SB("idt", [128, 128], BF16)
            mk = SB("mk", [128, 512], BF16)
            qt_ = [SB("qt%d" % i, [128, 512], BF16) for i in range(2)]
            kt_ = [SB("kt%d" % i, [128, 512], BF16) for i in range(2)]
            vt_ = [SB("vt%d" % i, [128, 8, 65], BF16) for i in range(3)]
            qT_2 = [SB("qT_%d" % i_, [128, 4, 128], BF16) for i_ in range(2)]
            kT = [SB("kT%d" % i, [128, 4, 128], BF16) for i in range(3)]
            pe_ = [SB("pe%d" % i, [128, 512], BF16) for i in range(4)]
            pm = [SB("pm%d" % i, [128, 4, 128], BF16) for i in range(4)]
            us = [SB("us%d" % i, [128, 520], F32) for i in range(2)]
            pTq = PS("pTq", [128, 8, 128], BF16)
            pTk = PS("pTk", [128, 8, 128], BF16)
            pS = [PS("pS%d" % i, [128, 4, 128], F32) for i in range(4)]
            pU = [PS("pU%d" % i, [128, 512], F32) for i in range(2)]

            P.dma("sp", idt[:], ident, [], [idt.r], ("id", 0))
            P.dma("sp", ce[:], ce_d, [], [ce.r], ("ce", 0))
            o_, w_ = CE_OFF["dilm"]
            P.op("act", lambda e: e.activation(out=mk[:], in_=ce[:, o_:o_ + w_], func=AF.Copy), [ce.r], [mk.r])
            mk4 = mk[:].rearrange("p (a c i) -> p a c i", a=2, c=2)
            sc = 64 ** -0.5
            def body(bi, r, rho, n, cnt):
                Qv = QD.rearrange("(l r) f -> r l f", r=r)
                Kv = KD.rearrange("(l r) f -> r l f", r=r)
                Vv = VD.rearrange("(l r) f -> r l f", r=r)
                Uv = UB[bi].rearrange("(l r) f -> r l f", r=r)
                i2 = cnt % 2
                i3 = cnt % 3
                ip = (cnt - 1) % 3
                qT = qT_2[i2]
                rows = slice(n * 128, (n + 1) * 128)
                src_q = [cx.R("%s%d" % (QD.tensor.name, t)) for t in range(NT)]
                src_k = [cx.R("%s%d" % (KD.tensor.name, t)) for t in range(NT)]
                src_v = [cx.R("VD%d" % t) for t in range(NT)]
                P.dma("sp", qt_[i2][:], Qv[rho, rows, :], src_q, [qt_[i2].r], ("ld", "q", i2))
                P.dma("sp", kt_[i2][:], Kv[rho, rows, :], src_k, [kt_[i2].r], ("ld", "k", i2))
                P.dma("sp", vt_[i3][:].rearrange("p h d -> p (h d)"), Vv[rho, rows, :], src_v, [vt_[i3].r], ("ld", "v", i3))
                for hp in range(4):
                    P.op("pe", lambda e, hp=hp, i2=i2: e.transpose(out=pTq[:, hp, :], in_=qt_[i2][:, hp * 128:(hp + 1) * 128],
                                                                   identity=idt[:]), [qt_[i2].r, idt.r], [pTq.r])
                P.op("act", lambda e: e.activation(out=qT[:], in_=pTq[:, 0:4, :], func=AF.Copy), [pTq.r], [qT.r])
                for hp in range(4):
                    P.op("pe", lambda e, hp=hp, i2=i2: e.transpose(out=pTk[:, hp, :], in_=kt_[i2][:, hp * 128:(hp + 1) * 128],
                                                                   identity=idt[:]), [kt_[i2].r, idt.r], [pTk.r])
                P.op("dve", lambda e, i3=i3: e.tensor_copy(out=kT[i3][:], in_=pTk[:, 0:4, :]), [pTk.r], [kT[i3].r])
                yield
                for hq in range(2):
                    psb = [pS[hq * 2 + 0], pS[hq * 2 + 1]]
                    peb = [pe_[hq * 2 + 0], pe_[hq * 2 + 1]]
                    pmb = [pm[hq * 2 + 0], pm[hq * 2 + 1]]
                    for hpi in range(2):
                        hp = hq * 2 + hpi
                        for hh in range(2):
                            prt = slice(hh * 64, (hh + 1) * 64)
                            ps_ = psb[hh]
                            if n > 0:
                                P.op("pe", lambda e, hp=hp, hpi=hpi, prt=prt, ps_=ps_, ip=ip: e.matmul(
                                    out=ps_[:, hpi * 2 + 0, :], lhsT=kT[ip][prt, hp, :], rhs=qT[prt, hp, :], start=True,
                                    stop=True), [kT[ip].r, qT.r], [ps_.r])
                            P.op("pe", lambda e, hp=hp, hpi=hpi, prt=prt, ps_=ps_, i3=i3: e.matmul(
                                out=ps_[:, hpi * 2 + 1, :], lhsT=kT[i3][prt, hp, :], rhs=qT[prt, hp, :], start=True,
                                stop=True), [kT[i3].r, qT.r], [ps_.r])
                    for hh in range(2):
                        ps_, pe__, pm_ = psb[hh], peb[hh], pmb[hh]
                        pe4 = pe__[:].rearrange("p (a c i) -> p a c i", a=2, c=2)
                        ps4 = ps_[:].rearrange("p (a c) i -> p a c i", a=2)
                        pm4 = pm_[:].rearrange("p (a c) i -> p a c i", a=2)
                        if n > 0:
                            P.op("act", lambda e, ps_=ps_, pe__=pe__: e.activation(
                                out=pe__[:], in_=ps_[:].rearrange("p a i -> p (a i)"), func=AF.Exp, scale=sc),
                                [ps_.r], [pe__.r])
                            P.op("pool", lambda e, pe__=pe__, pm_=pm_: e.tensor_tensor(
                                out=pm_[:].rearrange("p a i -> p (a i)"), in0=pe__[:], in1=mk[:], op=ALU.mult),
                                [pe__.r, mk.r], [pm_.r])
                        else:
                            P.op("act", lambda e, pe4=pe4, ps4=ps4: e.activation(
                                out=pe4[:, :, 1, :], in_=ps4[:, :, 1, :], func=AF.Exp, scale=sc), [ps_.r], [pe__.r])
                            P.op("pool", lambda e, pe4=pe4, pm4=pm4: e.tensor_tensor(
                                out=pm4[:, :, 1, :], in0=pe4[:, :, 1, :], in1=mk4[:, :, 1, :], op=ALU.mult),
                                [pe__.r, mk.r], [pm_.r])
                    for hpi in range(2):
                        hp = hq * 2 + hpi
                        for hh in range(2):
                            h = hp * 2 + hh
                            pu = pU[h // 4]
                            pm_ = pmb[hh]
                            usl = slice((h % 4) * 65, (h % 4 + 1) * 65)
                            if n > 0:
                                P.op("pe", lambda e, h=h, hpi=hpi, pu=pu, pm_=pm_, ip=ip, usl=usl: e.matmul(
                                    out=pu[:, usl], lhsT=pm_[:, hpi * 2 + 0, :], rhs=vt_[ip][:, h, :], start=True,
                                    stop=False), [pm_.r, vt_[ip].r], [pu.r])
                            P.op("pe", lambda e, h=h, hpi=hpi, pu=pu, pm_=pm_, i3=i3, n=n, usl=usl: e.matmul(
                                out=pu[:, usl], lhsT=pm_[:, hpi * 2 + 1, :], rhs=vt_[i3][:, h, :], start=(n == 0),
                                stop=True), [pm_.r, vt_[i3].r], [pu.r])
                    P.op("act", lambda e, hq=hq, i2=i2: e.activation(out=us[i2][:, hq * 260:(hq + 1) * 260],
                                                                     in_=pU[hq][:, 0:260], func=AF.Copy),
                         [pU[hq].r], [us[i2].r])
                    yield
                dst_res = []
                for tt in range(NT):
                    dst_res.append(cx.R("UB%d_%d" % (bi, tt)))
                P.dma("sp", Uv[rho, rows, :], us[i2][:], [us[i2].r], dst_res, ("st", "us", i2))
            bodies = []
            cnt = 0
            for bi, (window, r) in enumerate(DIL_PATTERNS):
                nb = (S // r) // 128
                for rho in range(r):
                    for n in range(nb):
                        bodies.append(body(bi, r, rho, n, cnt))
                        cnt += 1
            run_interleaved(bodies, lead=2)
            barrier(cx, tiles)

    if 'B' in _st:
        stageB()
    def stageC():
        with ExitStack() as es:
            sb, ps = phase_alloc(cx, es)
            tiles = []

            def SB(name, shape, dt):
                t = sb(name, shape, dt)
                tiles.append(t)
                return t

            def PS(name, shape, dt):
                t = ps(name, shape, dt)
                tiles.append(t)
                return t

            w_out = SB("w_out", [128, 8, D_MODEL], BF16)
            idt = SB("idt", [128, 128], BF16)
            xt = [SB("xt%d" % b, [128, D_MODEL], F32) for b in range(4)]
            u = [[SB("u%d%d" % (b, i), [128, 8, 65], F32) for i in range(3)] for b in range(4)]
            cat = [SB("cat%d" % b, [128, 1024], BF16) for b in range(4)]
            rc_2 = [SB("rc_%d" % i_, [128, 8], F32) for i_ in range(2)]
            catT_2 = [SB("catT_%d" % i_, [128, 8, 128], BF16) for i_ in range(2)]
            xo = [SB("xo%d" % b, [128, D_MODEL], F32) for b in range(2)]
            pT = PS("pT", [128, 8, 128], BF16)
            pY = [PS("pY%d" % i, [128, 512], F32) for i in range(2)]
            P.dma("sp", idt[:], ident, [], [idt.r], ("id", 0))
            wov = w_out_d.rearrange("(kc p) f -> p kc f", p=128)
            for kh in range(2):
                P.dma("pool", w_out[:, kh * 4:(kh + 1) * 4, :], wov[:, kh * 4:(kh + 1) * 4, :], [], [w_out.r], ("w", "w_out"))
            def loads(tt):
                bb = tt % 4
                rws = slice(tt * 128, (tt + 1) * 128)
                P.dma("sp", xt[bb][:], X[rws, :], [cx.R("X%d" % tt)], [xt[bb].r], ("xt", bb))
                P.dma("sp", cat[bb][:, 0:512], ORD[rws, :], [cx.R("ORD%d" % tt)], [cat[bb].r], ("ld", "cat", bb))
                for i in range(3):
                    P.dma("sp", u[bb][i][:].rearrange("p h d -> p (h d)"), UB[i][rws, :], [cx.R("UB%d_%d" % (i, tt))], [u[bb][i].r],
                          ("ld", "u", bb, i))

            def body(t):
                b = t % 4
                k_ = t % 2
                rc = rc_2[k_]
                catT = catT_2[k_]
                xr = cx.R("X%d" % t)
                rows = slice(t * 128, (t + 1) * 128)
                if t == 0:
                    loads(0)
                    loads(1)
                if t + 2 < NT:
                    loads(t + 2)
                P.op("dve", lambda e, b=b: e.tensor_tensor(out=u[b][0][:], in0=u[b][0][:], in1=u[b][1][:], op=ALU.add),
                     [u[b][0].r, u[b][1].r], [u[b][0].r])
                P.op("dve", lambda e, b=b: e.tensor_tensor(out=u[b][0][:], in0=u[b][0][:], in1=u[b][2][:], op=ALU.add),
                     [u[b][0].r, u[b][2].r], [u[b][0].r])
                P.op("dve", lambda e, b=b: e.reciprocal(out=rc[:], in_=u[b][0][:, :, 64]), [u[b][0].r], [rc.r])
                P.op("dve", lambda e, b=b: e.tensor_tensor(
                    out=cat[b][:, 512:1024].rearrange("p (h d) -> p h d", h=8), in0=u[b][0][:, :, 0:64],
                    in1=rc[:].unsqueeze(2).to_broadcast([128, 8, 64]), op=ALU.mult), [u[b][0].r, rc.r], [cat[b].r])
                yield
                for ec in range(8):
                    P.op("pe", lambda e, ec=ec, b=b: e.transpose(out=pT[:, ec, :], in_=cat[b][:, ec * 128:(ec + 1) * 128],
                                                                 identity=idt[:]), [cat[b].r, idt.r], [pT.r])
                P.op("act", lambda e: e.activation(out=catT[:], in_=pT[:], func=AF.Copy), [pT.r], [catT.r])
                for hf in range(2):
                    for ec in range(8):
                        P.op("pe", lambda e, hf=hf, ec=ec: e.matmul(out=pY[hf][:], lhsT=catT[:, ec, :],
                                                                   rhs=w_out[:, ec, hf * 512:(hf + 1) * 512],
                                                                   start=(ec == 0), stop=(ec == 7)), [catT.r, w_out.r], [pY[hf].r])
                    P.op("dve", lambda e, hf=hf, b=b, k_=k_: e.tensor_tensor(out=xo[k_][:, hf * 512:(hf + 1) * 512], in0=pY[hf][:],
                                                                      in1=xt[b][:, hf * 512:(hf + 1) * 512], op=ALU.add),
                         [pY[hf].r, xt[b].r], [xo[k_].r])
                P.dma("sp", X[rows, :], xo[k_][:], [xo[k_].r], [xr], ("xo", k_))
            run_interleaved([body(t) for t in range(NT)], lead=1)
            barrier(cx, tiles)

    if 'C' in _st:
        stageC()

def extra_inputs(S=4096):
    ce, off, ropeR, ropeD = make_consts_even(S)
    return {"ident": np.eye(128).astype(ml_dtypes.bfloat16), "cf32": CF32, "ce32": ce, "ropeR": ropeR, "ropeD": ropeD}


ALL_NAMES = list(INPUT_SHAPES.keys())


def kernel(**inputs):
    S = 4096
    phases = []
    for l in range(DEPTH):
        phases.append(("ffn", l, "pre"))
        phases.append(("mixA", l // 2) if l % 2 == 0 else ("gla", l // 2))
        phases.append(("ffn", l, "post"))
    phases.append(("final",))
    nc = build_program(S, phases, ALL_NAMES)
    x = np.ascontiguousarray(np.asarray(inputs["x"], dtype=np.float32))
    B = x.shape[0]
    shared = {n: np.ascontiguousarray(np.asarray(inputs[n], dtype=np.float32)) for n in ALL_NAMES}
    shared.update(extra_inputs(S))
    in_maps = []
    for b in range(B):
        m = {"x": x[b]}
        m.update(shared)
        in_maps.append(m)
    res = run_bass_kernel_spmd(nc, in_maps, core_ids=list(range(B)))
    return np.stack([np.asarray(r["out"], dtype=np.float32) for r in res.results], axis=0)
```
